# Optimizing a Trainium2 kernel written in Bass

```python
import jax
import jax.numpy as jnp
from jax import lax
import numpy as np

D_MODEL = 1024
BATCH = 4
SEQ = 4096
DEPTH = 2

CTX_LEN = 256
GRID_W = 64
F32 = jnp.float32

MLSTM_H = 4
MLSTM_DH = 64
MLSTM_W = MLSTM_H * MLSTM_DH
MLSTM_CHUNK = 64
CONV_W = 3
RWKV_H = 4
RWKV_N = 64
RWKV_W = RWKV_H * RWKV_N
DECAY_LORA = 64
AAA_LORA = 64
GATE_LORA = 128
RWKV_GN_EPS = 64e-5
MLA_H = 8
MLA_NOPE = 64
MLA_ROPE = 32
MLA_V = 64
MLA_W = MLA_H * MLA_V
Q_RANK = 256
KV_RANK = 128
ROPE_THETA = 10000.0
Q_BLOCK = 128
D_MIX = MLSTM_W + RWKV_W + MLA_W
MLSTM_COLS = (2 * MLSTM_W, MLSTM_W, MLSTM_W, 4 * MLSTM_H)
RWKV_COLS = (RWKV_W, RWKV_W, RWKV_W, 2 * DECAY_LORA, 2 * AAA_LORA, GATE_LORA)
MLA_COLS = (Q_RANK, KV_RANK, MLA_ROPE)
N_MLSTM_IN = 4 * MLSTM_W + 4 * MLSTM_H
N_RWKV_IN = 3 * RWKV_W + 2 * DECAY_LORA + 2 * AAA_LORA + GATE_LORA
N_MLA_IN = Q_RANK + KV_RANK + MLA_ROPE
N_IN = N_MLSTM_IN + N_RWKV_IN + N_MLA_IN
N_EXPERTS = 64
TOP_K = 8
N_GROUPS = 8
TOPK_GROUPS = 4
D_EXPERT = 256
ROUTED_SCALE = 2.5
MOE_BLOCK = 256
DEEPNORM_ALPHA = (2 * DEPTH) ** 0.25
DEEPNORM_BETA = (8 * DEPTH) ** -0.25
LN_EPS = 1e-6

kernel_name = 'hybrid_mlstm_rwkv7_mla_moe_dit_block'


def _split(z, sizes):
    return jnp.split(z, np.cumsum(sizes)[:-1].tolist(), axis=-1)


def _ln(x):
    xf = x.astype(F32)
    xc = xf - xf.mean(-1, keepdims=True)
    return xc * lax.rsqrt(jnp.mean(xc * xc, -1, keepdims=True) + LN_EPS)


def _modulate(x, shift, scale):
    return (_ln(x) * (1.0 + scale) + shift).astype(x.dtype)


def _post_norm(x_res, y, gate, w, b):
    return (_ln(DEEPNORM_ALPHA * x_res + gate * y) * w + b).astype(x_res.dtype)


def _rms(x, w):
    xf = x.astype(F32)
    return (xf * lax.rsqrt(jnp.mean(xf * xf, -1, keepdims=True) + 1e-6) * w).astype(x.dtype)


def _head_normalize(x, eps):
    xf = x.astype(F32)
    xc = xf - xf.mean(-1, keepdims=True)
    return xc * lax.rsqrt(jnp.mean(xc * xc, -1, keepdims=True) + eps)


def _centred_conv(z, w):
    t = z.shape[1]
    zp = jnp.pad(z, ((0, 0), (CONV_W // 2, CONV_W // 2), (0, 0)))
    return sum(w[j] * zp[:, j:j + t] for j in range(CONV_W))


def _bidir_token_shift(z, mu):
    zp = jnp.pad(z, ((0, 0), (1, 1), (0, 0)))
    return z + mu * (0.5 * (zp[:, :-2] + zp[:, 2:]) - z)


def _rope_tables(n):
    ROWS = n // GRID_W
    row = jnp.repeat(jnp.arange(ROWS), GRID_W).astype(F32)
    col = jnp.tile(jnp.arange(GRID_W), ROWS).astype(F32)
    n_freq = MLA_ROPE // 4
    freq = ROPE_THETA ** (-jnp.arange(n_freq, dtype=F32) / n_freq)
    ang = jnp.concatenate([row[:, None] * freq, col[:, None] * freq], -1)
    return jnp.cos(ang), jnp.sin(ang)


def _rope(x, cos, sin):
    xf = x.astype(F32).reshape(x.shape[:-1] + (-1, 2))
    x0, x1 = xf[..., 0], xf[..., 1]
    out = jnp.stack([x0 * cos - x1 * sin, x0 * sin + x1 * cos], -1)
    return out.reshape(x.shape).astype(x.dtype)


def _mlstm_chunked(q, k, v, log_i, log_f, state, emit):
    b2, h, t, dh = q.shape
    cl = MLSTM_CHUNK
    nc = t // cl
    q, k, v = (a.reshape(b2, h, nc, cl, dh) for a in (q, k, v))
    li = log_i.reshape(b2, h, nc, cl)
    bcum = jnp.cumsum(log_f.reshape(b2, h, nc, cl), axis=-1)
    b_end = bcum[..., -1]
    w_end = b_end[..., None] - bcum + li
    m_loc = w_end.max(-1)
    e_end = jnp.exp(w_end - m_loc[..., None])
    c_loc = jnp.einsum('bhcl,bhclv,bhclk->bhcvk', e_end, v, k)
    n_loc = jnp.einsum('bhcl,bhclk->bhck', e_end, k)

    def step(carry, xs):
        c_st, n_st, m_st = carry
        c_l, n_l, m_l, b_l = xs
        m_new = jnp.maximum(b_l + m_st, m_l)
        a_old = jnp.exp(b_l + m_st - m_new)
        a_loc = jnp.exp(m_l - m_new)
        c_new = a_old[..., None, None] * c_st + a_loc[..., None, None] * c_l
        n_new = a_old[..., None] * n_st + a_loc[..., None] * n_l
        return (c_new, n_new, m_new), (c_st, n_st, m_st)

    xs = tuple(jnp.moveaxis(a, 2, 0) for a in (c_loc, n_loc, m_loc, b_end))
    final, prev = lax.scan(step, state, xs)
    if not emit:
        return None, final
    c_prev, n_prev, m_prev = (jnp.moveaxis(a, 0, 2) for a in prev)
    lower = jnp.tril(jnp.ones((cl, cl), dtype=bool))
    d_log = jnp.where(lower, bcum[..., :, None] - bcum[..., None, :] + li[..., None, :], -jnp.inf)
    g_log = bcum + m_prev[..., None]
    m_row = jnp.maximum(g_log, d_log.max(-1))
    w_intra = jnp.exp(d_log - m_row[..., None]) * jnp.einsum('bhcjd,bhcsd->bhcjs', q, k)
    e_inter = jnp.exp(g_log - m_row)
    num = (jnp.einsum('bhcjs,bhcsv->bhcjv', w_intra, v)
           + e_inter[..., None] * jnp.einsum('bhcvk,bhcjk->bhcjv', c_prev, q))
    den = w_intra.sum(-1) + e_inter * jnp.einsum('bhck,bhcjk->bhcj', n_prev, q)
    out = num / jnp.maximum(jnp.abs(den), jnp.exp(-m_row))[..., None]
    return out.reshape(b2, h, t, dh), final


def _mlstm_prepare(z, conv_w, gate_bias):
    b, t, _ = z.shape
    zqk, zv, zo, zg = _split(z, MLSTM_COLS)
    zq, zk = jnp.split(jax.nn.silu(_centred_conv(zqk, conv_w)), 2, axis=-1)
    heads = lambda a: a.astype(F32).reshape(b, t, MLSTM_H, MLSTM_DH).transpose(0, 2, 1, 3)
    q = heads(zq) * MLSTM_DH ** -0.5
    k = heads(zk)
    v = heads(zv)
    g = (zg.astype(F32).reshape(b, t, 4, MLSTM_H) + gate_bias).transpose(2, 0, 3, 1)
    log_i = jnp.concatenate([g[0], jnp.flip(g[2], -1)], 0)
    log_f = jax.nn.log_sigmoid(jnp.concatenate([g[1], jnp.flip(g[3], -1)], 0))
    both = lambda a: jnp.concatenate([a, jnp.flip(a, 2)], 0)
    return (both(q), both(k), both(v), log_i, log_f), zo


def _mlstm_mixer(z_lat, z_ctx, p, emit_ctx):
    b = z_lat.shape[0]
    ctx_in, o_ctx = _mlstm_prepare(z_ctx, p['mlstm_conv'], p['mlstm_gate_bias'])
    lat_in, o_lat = _mlstm_prepare(z_lat, p['mlstm_conv'], p['mlstm_gate_bias'])
    state0 = (jnp.zeros((2 * b, MLSTM_H, MLSTM_DH, MLSTM_DH), F32),
              jnp.zeros((2 * b, MLSTM_H, MLSTM_DH), F32),
              jnp.zeros((2 * b, MLSTM_H), F32))
    h_ctx, state_ctx = _mlstm_chunked(*ctx_in, state0, emit_ctx)
    h_lat, _ = _mlstm_chunked(*lat_in, state_ctx, True)

    def readout(h2, zo):
        hs = (h2[:b] + jnp.flip(h2[b:], 2)).transpose(0, 2, 1, 3)
        hs = _head_normalize(hs, 1e-6) * p['mlstm_norm_w'].reshape(MLSTM_H, MLSTM_DH)
        return (jax.nn.sigmoid(zo.astype(F32)) * hs.reshape(zo.shape)).astype(zo.dtype)

    y_ctx = readout(h_ctx, o_ctx) if emit_ctx else None
    return readout(h_lat, o_lat), y_ctx


def _rwkv_prepare(z, p):
    b, t, _ = z.shape
    z = _bidir_token_shift(z.astype(F32), p['rwkv_mu'])
    zr, zk, zv, zw, za, zg = _split(z, RWKV_COLS)
    heads = lambda a: a.reshape(b, t, RWKV_H, RWKV_N)
    w_raw = p['rwkv_w0'] + jnp.einsum('btdr,drc->btdc', jnp.tanh(zw.reshape(b, t, 2, DECAY_LORA)), p['rwkv_w_up'])
    decay = jnp.exp(-jnp.exp(-jax.nn.softplus(-w_raw) - 0.5))
    a = jax.nn.sigmoid(p['rwkv_a0'] + jnp.einsum('btdr,drc->btdc', za.reshape(b, t, 2, AAA_LORA), p['rwkv_a_up']))
    g = jax.nn.sigmoid(zg) @ p['rwkv_g_up']
    kk = heads(zk * p['rwkv_k_k'])
    kk = kk * lax.rsqrt(jnp.maximum(jnp.sum(kk * kk, -1, keepdims=True), 1e-12))
    k_dir = (zk[:, :, None, :] * (1.0 + (a - 1.0) * p['rwkv_k_a'])).reshape(b, t, 2, RWKV_H, RWKV_N)
    r = heads(zr)
    v = heads(zv)
    bonus = jnp.einsum('bthn,btdhn->bth', r * p['rwkv_r_k'].reshape(RWKV_H, RWKV_N), k_dir)[..., None] * v
    both = lambda f, bw: jnp.concatenate([f, jnp.flip(bw, 1)], 0)
    seqs = (both(r, r),
            both(heads(decay[:, :, 0]), heads(decay[:, :, 1])),
            both(k_dir[:, :, 0], k_dir[:, :, 1]),
            both(v, v),
            both(kk, kk),
            both(heads(a[:, :, 0]), heads(a[:, :, 1])))
    return seqs, g, bonus


def _rwkv_scan(s0, seqs, emit):
    xs = tuple(jnp.moveaxis(a, 1, 0) for a in seqs)

    def step(s, inp):
        r, w, k, v, kk, a = inp
        s = (s * w[:, :, None, :]
             + jnp.einsum('bhvk,bhk->bhv', s, -kk)[..., None] * (kk * a)[:, :, None, :]
             + v[..., None] * k[:, :, None, :])
        return s, (jnp.einsum('bhvk,bhk->bhv', s, r) if emit else None)

    s, out = lax.scan(step, s0, xs)
    return (jnp.moveaxis(out, 0, 1) if emit else None), s


def _rwkv_mixer(z_lat, z_ctx, p, emit_ctx):
    b = z_lat.shape[0]
    seq_c, g_c, bonus_c = _rwkv_prepare(z_ctx, p)
    seq_l, g_l, bonus_l = _rwkv_prepare(z_lat, p)
    s0 = jnp.zeros((2 * b, RWKV_H, RWKV_N, RWKV_N), F32)
    o_c, s_ctx = _rwkv_scan(s0, seq_c, emit_ctx)
    o_l, _ = _rwkv_scan(s_ctx, seq_l, True)

    def readout(o2, g, bonus, dtype):
        o = o2[:b] + jnp.flip(o2[b:], 1)
        o = (_head_normalize(o, RWKV_GN_EPS) * p['rwkv_ln_w'].reshape(RWKV_H, RWKV_N)
             + p['rwkv_ln_b'].reshape(RWKV_H, RWKV_N) + bonus)
        return (o.reshape(g.shape) * g).astype(dtype)

    y_ctx = readout(o_c, g_c, bonus_c, z_ctx.dtype) if emit_ctx else None
    return readout(o_l, g_l, bonus_l, z_lat.dtype), y_ctx


def _mla_project(z, p, cos=None, sin=None):
    b, t, _ = z.shape
    zq, zkv, k_rope = _split(z, MLA_COLS)
    q = (_rms(zq, p['mla_q_norm']) @ p['mla_w_uq']).reshape(b, t, MLA_H, MLA_NOPE + MLA_ROPE)
    kv = (_rms(zkv, p['mla_kv_norm']) @ p['mla_w_ukv']).reshape(b, t, MLA_H, MLA_NOPE + MLA_V)
    q_nope, q_rope = q[..., :MLA_NOPE], q[..., MLA_NOPE:]
    k_nope, v = kv[..., :MLA_NOPE], kv[..., MLA_NOPE:]
    if cos is not None:
        q_rope = _rope(q_rope, cos[:, None], sin[:, None])
        k_rope = _rope(k_rope, cos, sin)
    q = jnp.concatenate([q_nope, q_rope], -1)
    k = jnp.concatenate([k_nope, jnp.broadcast_to(k_rope[:, :, None, :], (b, t, MLA_H, MLA_ROPE))], -1)
    return q.transpose(0, 2, 1, 3), k.transpose(0, 2, 1, 3), v.transpose(0, 2, 1, 3)


def _softmax_attend(q, k, v):
    s = jnp.einsum('bhqd,bhkd->bhqk', q.astype(F32), k.astype(F32)) * (q.shape[-1] ** -0.5)
    return jnp.einsum('bhqk,bhkd->bhqd', jax.nn.softmax(s, axis=-1).astype(v.dtype), v)


def _mla_mixer(z_lat, z_ctx, p, cos, sin, emit_ctx):
    q_c, k_c, v_c = _mla_project(z_ctx, p)
    q_l, k_l, v_l = _mla_project(z_lat, p, cos, sin)
    b, h, t, dk = q_l.shape
    k_all = jnp.concatenate([k_l, k_c], 2)
    v_all = jnp.concatenate([v_l, v_c], 2)
    q_blocks = jnp.moveaxis(q_l.reshape(b, h, t // Q_BLOCK, Q_BLOCK, dk), 2, 0)
    o = lax.map(lambda qb: _softmax_attend(qb, k_all, v_all), q_blocks)
    y_lat = jnp.moveaxis(o, 0, 2).reshape(b, h, t, MLA_V).transpose(0, 2, 1, 3).reshape(b, t, MLA_W)
    y_ctx = None
    if emit_ctx:
        y_ctx = _softmax_attend(q_c, k_c, v_c).transpose(0, 2, 1, 3).reshape(b, z_ctx.shape[1], MLA_W)
    return y_lat, y_ctx


def _token_mixers(h_lat, h_ctx, p, cos, sin, emit_ctx):
    z_lat = h_lat @ p['w_in']
    z_ctx = h_ctx @ p['w_in']
    zl_m, zl_r, zl_a = _split(z_lat, (N_MLSTM_IN, N_RWKV_IN, N_MLA_IN))
    zc_m, zc_r, zc_a = _split(z_ctx, (N_MLSTM_IN, N_RWKV_IN, N_MLA_IN))
    m_l, m_c = _mlstm_mixer(zl_m, zc_m, p, emit_ctx)
    r_l, r_c = _rwkv_mixer(zl_r, zc_r, p, emit_ctx)
    a_l, a_c = _mla_mixer(zl_a, zc_a, p, cos, sin, emit_ctx)
    y_lat = jnp.concatenate([m_l, r_l, a_l], -1) @ p['w_out']
    y_ctx = jnp.concatenate([m_c, r_c, a_c], -1) @ p['w_out'] if emit_ctx else None
    return y_lat, y_ctx


def _moe(h, p):
    t, d = h.shape
    e_n = N_EXPERTS
    scores = jax.nn.sigmoid(h.astype(F32) @ p['router_w'].astype(F32))
    biased = scores + p['router_bias'].astype(F32)
    group_score = lax.top_k(biased.reshape(t, N_GROUPS, e_n // N_GROUPS), 2)[0].sum(-1)
    _, top_groups = lax.top_k(group_score, TOPK_GROUPS)
    group_mask = jax.nn.one_hot(top_groups, N_GROUPS, dtype=F32).sum(1)
    allowed = jnp.repeat(group_mask, e_n // N_GROUPS, axis=1) > 0
    _, eidx = lax.top_k(jnp.where(allowed, biased, -jnp.inf), TOP_K)
    sel = jnp.take_along_axis(scores, eidx, 1)
    gates = ROUTED_SCALE * sel / sel.sum(-1, keepdims=True)
    n_assign = t * TOP_K
    flat_e = eidx.reshape(n_assign)
    flat_t = jnp.repeat(jnp.arange(t, dtype=jnp.int32), TOP_K)
    flat_g = gates.reshape(n_assign)
    order = jnp.argsort(flat_e)
    sorted_e = flat_e[order]
    counts = jnp.bincount(flat_e, length=e_n)
    padded = (counts + MOE_BLOCK - 1) // MOE_BLOCK * MOE_BLOCK
    ends = jnp.cumsum(padded)
    dest = (ends - padded)[sorted_e] + jnp.arange(n_assign) - (jnp.cumsum(counts) - counts)[sorted_e]
    n_blocks = -(-(n_assign + e_n * (MOE_BLOCK - 1)) // MOE_BLOCK)
    n_rows = n_blocks * MOE_BLOCK
    tok = jnp.full((n_rows,), t, jnp.int32).at[dest].set(flat_t[order])
    gate = jnp.zeros((n_rows,), h.dtype).at[dest].set(flat_g[order].astype(h.dtype))
    block_e = jnp.minimum(jnp.searchsorted(ends, jnp.arange(n_blocks) * MOE_BLOCK, side='right'), e_n - 1)
    h_pad = jnp.concatenate([h, jnp.zeros((1, d), h.dtype)], 0)

    def expert_block(args):
        tb, gb, e = args
        xb = h_pad[tb]
        y = (jax.nn.silu(xb @ p['exp_w_gate'][e]) * (xb @ p['exp_w_up'][e])) @ p['exp_w_down'][e]
        return y * gb[:, None]

    ys = lax.map(expert_block, (tok.reshape(n_blocks, MOE_BLOCK), gate.reshape(n_blocks, MOE_BLOCK), block_e))
    routed = jnp.zeros((t + 1, d), h.dtype).at[tok].add(ys.reshape(n_rows, d))[:t]
    shared = (jax.nn.silu(h @ p['sh_w_gate']) * (h @ p['sh_w_up'])) @ p['sh_w_down']
    return routed + shared


def setup_inputs(seed: int = 0) -> dict:
    key = jax.random.key(seed)
    ks = iter(jax.random.split(key, 48))
    nrm = lambda shape, scale: jax.random.normal(next(ks), shape, F32) * scale
    L, D = DEPTH, D_MODEL
    gate_i = nrm((L, 2, MLSTM_H), 0.1)
    gate_f = jax.random.uniform(next(ks), (L, 2, MLSTM_H), F32, 3.0, 6.0)
    mlstm_gate_bias = jnp.stack([gate_i[:, 0], gate_f[:, 0], gate_i[:, 1], gate_f[:, 1]], 1)
    return {
        'x': nrm((BATCH, SEQ, D), 1.0),
        'c': nrm((BATCH, D), 1.0),
        'ctx': nrm((BATCH, CTX_LEN, D), 1.0),
        'c_ctx': nrm((D,), 1.0),
        'w_mod': nrm((L, D, 6 * D), 0.5 * D ** -0.5),
        'b_mod': nrm((L, 6 * D), 0.01),
        'w_in': nrm((L, D, N_IN), D ** -0.5),
        'mlstm_conv': nrm((L, CONV_W, 2 * MLSTM_W), CONV_W ** -0.5),
        'mlstm_gate_bias': mlstm_gate_bias,
        'mlstm_norm_w': 1.0 + nrm((L, MLSTM_W), 0.1),
        'rwkv_mu': jax.random.uniform(next(ks), (L, N_RWKV_IN), F32),
        'rwkv_w0': jax.random.uniform(next(ks), (L, 2, RWKV_W), F32, -3.0, 1.0),
        'rwkv_w_up': nrm((L, 2, DECAY_LORA, RWKV_W), 0.5 * DECAY_LORA ** -0.5),
        'rwkv_a0': nrm((L, 2, RWKV_W), 0.5),
        'rwkv_a_up': nrm((L, 2, AAA_LORA, RWKV_W), 0.5 * AAA_LORA ** -0.5),
        'rwkv_g_up': nrm((L, GATE_LORA, RWKV_W), GATE_LORA ** -0.5),
        'rwkv_k_k': 0.85 + nrm((L, RWKV_W), 0.1),
        'rwkv_k_a': 1.0 + nrm((L, RWKV_W), 0.1),
        'rwkv_r_k': nrm((L, RWKV_W), 0.1),
        'rwkv_ln_w': 1.0 + nrm((L, RWKV_W), 0.1),
        'rwkv_ln_b': nrm((L, RWKV_W), 0.01),
        'mla_q_norm': 1.0 + nrm((L, Q_RANK), 0.1),
        'mla_kv_norm': 1.0 + nrm((L, KV_RANK), 0.1),
        'mla_w_uq': nrm((L, Q_RANK, MLA_H * (MLA_NOPE + MLA_ROPE)), Q_RANK ** -0.5),
        'mla_w_ukv': nrm((L, KV_RANK, MLA_H * (MLA_NOPE + MLA_V)), KV_RANK ** -0.5),
        'w_out': nrm((L, D_MIX, D), DEEPNORM_BETA * D_MIX ** -0.5),
        'ln1_w': 1.0 + nrm((L, D), 0.1),
        'ln1_b': nrm((L, D), 0.01),
        'router_w': nrm((L, D, N_EXPERTS), D ** -0.5),
        'router_bias': nrm((L, N_EXPERTS), 0.01),
        'exp_w_gate': nrm((L, N_EXPERTS, D, D_EXPERT), D ** -0.5),
        'exp_w_up': nrm((L, N_EXPERTS, D, D_EXPERT), D ** -0.5),
        'exp_w_down': nrm((L, N_EXPERTS, D_EXPERT, D), DEEPNORM_BETA * D_EXPERT ** -0.5),
        'sh_w_gate': nrm((L, D, D_EXPERT), D ** -0.5),
        'sh_w_up': nrm((L, D, D_EXPERT), D ** -0.5),
        'sh_w_down': nrm((L, D_EXPERT, D), DEEPNORM_BETA * D_EXPERT ** -0.5),
        'ln2_w': 1.0 + nrm((L, D), 0.1),
        'ln2_b': nrm((L, D), 0.01),
    }


def reference(x, c, ctx, c_ctx, w_mod, b_mod, w_in, mlstm_conv, mlstm_gate_bias, mlstm_norm_w,
              rwkv_mu, rwkv_w0, rwkv_w_up, rwkv_a0, rwkv_a_up, rwkv_g_up, rwkv_k_k, rwkv_k_a, rwkv_r_k,
              rwkv_ln_w, rwkv_ln_b, mla_q_norm, mla_kv_norm, mla_w_uq, mla_w_ukv, w_out, ln1_w, ln1_b,
              router_w, router_bias, exp_w_gate, exp_w_up, exp_w_down, sh_w_gate, sh_w_up, sh_w_down,
              ln2_w, ln2_b):
    b, n_lat, d = x.shape
    n_ctx = ctx.shape[1]
    cos, sin = _rope_tables(n_lat)
    xc = ctx
    for l in range(DEPTH):
        last = l == DEPTH - 1
        p = {
            'w_in': w_in[l], 'w_out': w_out[l],
            'mlstm_conv': mlstm_conv[l], 'mlstm_gate_bias': mlstm_gate_bias[l], 'mlstm_norm_w': mlstm_norm_w[l],
            'rwkv_mu': rwkv_mu[l], 'rwkv_w0': rwkv_w0[l], 'rwkv_w_up': rwkv_w_up[l], 'rwkv_a0': rwkv_a0[l],
            'rwkv_a_up': rwkv_a_up[l], 'rwkv_g_up': rwkv_g_up[l], 'rwkv_k_k': rwkv_k_k[l], 'rwkv_k_a': rwkv_k_a[l],
            'rwkv_r_k': rwkv_r_k[l], 'rwkv_ln_w': rwkv_ln_w[l], 'rwkv_ln_b': rwkv_ln_b[l],
            'mla_q_norm': mla_q_norm[l], 'mla_kv_norm': mla_kv_norm[l], 'mla_w_uq': mla_w_uq[l], 'mla_w_ukv': mla_w_ukv[l],
        }
        moe_p = {
            'router_w': router_w[l], 'router_bias': router_bias[l], 'exp_w_gate': exp_w_gate[l],
            'exp_w_up': exp_w_up[l], 'exp_w_down': exp_w_down[l], 'sh_w_gate': sh_w_gate[l],
            'sh_w_up': sh_w_up[l], 'sh_w_down': sh_w_down[l],
        }
        sh1, sc1, g1, sh2, sc2, g2 = [m[:, None, :] for m in jnp.split(jax.nn.silu(c) @ w_mod[l] + b_mod[l], 6, axis=-1)]
        csh1, csc1, cg1, csh2, csc2, cg2 = jnp.split(jax.nn.silu(c_ctx) @ w_mod[l] + b_mod[l], 6, axis=-1)
        y_lat, y_ctx = _token_mixers(_modulate(x, sh1, sc1), _modulate(xc, csh1, csc1), p, cos, sin, not last)
        x = _post_norm(x, y_lat, g1, ln1_w[l], ln1_b[l])
        h2_lat = _modulate(x, sh2, sc2).reshape(b * n_lat, d)
        if last:
            f_lat = _moe(h2_lat, moe_p)
        else:
            xc = _post_norm(xc, y_ctx, cg1, ln1_w[l], ln1_b[l])
            h2_ctx = _modulate(xc, csh2, csc2).reshape(b * n_ctx, d)
            f_all = _moe(jnp.concatenate([h2_lat, h2_ctx], 0), moe_p)
            f_lat = f_all[:b * n_lat]
            xc = _post_norm(xc, f_all[b * n_lat:].reshape(b, n_ctx, d), cg2, ln2_w[l], ln2_b[l])
        x = _post_norm(x, f_lat.reshape(b, n_lat, d), g2, ln2_w[l], ln2_b[l])
    return x
```

```python
import numpy as np
import ml_dtypes
import concourse.bass as bass
import concourse.mybir as mybir
from concourse.bass_utils import run_bass_kernel_spmd

F32 = mybir.dt.float32
BF16 = mybir.dt.bfloat16
AF = mybir.ActivationFunctionType
ALU = mybir.AluOpType
AX = mybir.AxisListType

D = 1024
T = 4352
NT = 34
NCTX = 256
ISQ96 = 96 ** -0.5


class Res:
    __slots__ = ("w", "r")

    def __init__(self):
        self.w = None
        self.r = {}


class Buf:
    def __init__(self, t):
        self.t = t
        self.res = Res()
        self._sub = {}

    def __getitem__(self, k):
        return self.t[k]

    def sub(self, k):
        if k not in self._sub:
            self._sub[k] = Res()
        return self._sub[k]


def _res(x):
    return x.res if hasattr(x, "res") else x


class Prog:
    CE = ("tensor", "vector", "scalar", "gpsimd")

    def __init__(self, nc, ndma=12):
        self.nc = nc
        self.lists = {e: [] for e in self.CE + ("sync",)}
        self.sems = {e: nc.alloc_semaphore("es_" + e) for e in self.CE}
        for i in range(ndma):
            self.sems["d%d" % i] = nc.alloc_semaphore("ds%d" % i)
        self.ndma = ndma
        self.duse = [0] * ndma
        self.dnext = 0
        self.tick = {e: 0 for e in self.CE}
        self.seen = {e: {} for e in self.lists}
        self.n = 0
        self.scoped = False
        self.sp = 16384 + 256
        self.cnt = 0
        self.ncoll = 0

    def sb(self, name, shape, dt):
        if not self.scoped:
            self.cnt += 1
            return Buf(self.nc.alloc_sbuf_tensor("s%d_%s" % (self.cnt, name), list(shape), dt))
        esz = 4 if dt == F32 else 2
        nb = esz
        for d_ in shape[1:]:
            nb *= d_
        nb = (nb + 63) // 64 * 64
        off = self.sp
        self.sp += nb
        assert self.sp <= 229000, ("SBUF overflow", name, self.sp)
        self.cnt += 1
        return Buf(self.nc.alloc_sbuf_tensor_at("s%d_%s" % (self.cnt, name), list(shape), dt, offset=off))

    def ps(self, name, shape, dt=F32):
        return Buf(self.nc.alloc_psum_tensor("p_" + name, list(shape), dt))

    def mark(self):
        return self.sp

    def release(self, mark):
        self.barrier()
        self.sp = mark

    def barrier(self):
        tgt = {e: self.tick[e] for e in self.CE if self.tick[e]}
        for i in range(self.ndma):
            if self.duse[i]:
                tgt["d%d" % i] = 16 * self.duse[i]
        for i in range(self.ncoll):
            tgt["c%d" % i] = 1
        for q in self.lists:
            waits = []
            for k, v in tgt.items():
                if k == q:
                    continue
                if self.seen[q].get(k, 0) >= v:
                    continue
                self.seen[q][k] = v
                waits.append((k, v))
            if waits:
                self.lists[q].append((waits, None, None, 0))

    def coll(self, kind, in_ap, out_ap, groups, r=(), w=()):
        key = "c%d" % self.ncoll
        self.ncoll += 1
        self.sems[key] = self.nc.alloc_semaphore("cs_" + key)
        waits = self._deps("gpsimd", r, w)
        tok = (key, 1)
        self.lists["gpsimd"].append((waits, lambda e: e.collective_compute(
            kind, ALU.bypass, replica_groups=groups, ins=[in_ap], outs=[out_ap]), key, 1))
        self._mark(tok, r, w)
        return tok

    def _deps(self, q, r, w, extra=None):
        deps = dict(extra or {})

        def add(k, v):
            if deps.get(k, 0) < v:
                deps[k] = v
        for x in r:
            x = _res(x)
            if x.w is not None:
                add(*x.w)
        for x in w:
            x = _res(x)
            if x.w is not None:
                add(*x.w)
            for k, v in x.r.items():
                add(k, v)
        waits = []
        seen = self.seen[q]
        for k, v in deps.items():
            if k == q and q == "tensor":
                continue
            if seen.get(k, 0) >= v:
                continue
            seen[k] = v
            waits.append((k, v))
        return waits

    def _mark(self, tok, r, w):
        k, v = tok
        for x in r:
            x = _res(x)
            if x.r.get(k, 0) < v:
                x.r[k] = v
        for x in w:
            x = _res(x)
            x.w = tok
            x.r = {}

    def op(self, eng, fn, r=(), w=()):
        waits = self._deps(eng, r, w)
        self.tick[eng] += 1
        tok = (eng, self.tick[eng])
        self.lists[eng].append((waits, fn, eng, 1))
        self._mark(tok, r, w)
        self.n += 1
        return tok

    def dma(self, q, out, in_, r=(), w=()):
        i = self.dnext
        self.dnext = (i + 1) % self.ndma
        key = "d%d" % i
        extra = {key: 16 * self.duse[i]} if self.duse[i] else {}
        waits = self._deps(q, r, w, extra)
        self.duse[i] += 1
        tok = (key, 16 * self.duse[i])
        self.lists[q].append((waits, lambda e: e.dma_start(out=out, in_=in_), key, 16))
        self._mark(tok, r, w)
        self.n += 1
        return tok

    def call(self, eng, name, *args, r=(), w=(), **kw):
        return self.op(eng, lambda e: getattr(e, name)(*args, **kw), r, w)

    def mm(self, out, lhsT, rhs, start=True, stop=True, r=(), w=(), skip=False):
        return self.op("tensor", lambda e: e.matmul(out, lhsT, rhs, start=start, stop=stop,
                                                     skip_group_check=skip), r, w)

    def tr(self, out, in_, ident, r=(), w=()):
        return self.op("tensor", lambda e: e.transpose(out, in_, ident), r, w)

    def act(self, out, in_, func, r=(), w=(), eng="scalar", **kw):
        return self.op(eng, lambda e: e.activation(out, in_, func, **kw), r, w)

    def tt(self, out, in0, in1, op, r=(), w=(), eng="vector"):
        return self.op(eng, lambda e: e.tensor_tensor(out, in0, in1, op), r, w)

    def ts(self, out, in0, s1, s2, op0, op1=None, r=(), w=(), eng="vector", **kw):
        if op1 is None:
            return self.op(eng, lambda e: e.tensor_scalar(out, in0, s1, s2, op0, **kw), r, w)
        return self.op(eng, lambda e: e.tensor_scalar(out, in0, s1, s2, op0, op1, **kw), r, w)

    def stt(self, out, in0, s, in1, op0, op1, r=(), w=()):
        return self.op("vector", lambda e: e.scalar_tensor_tensor(out, in0, s, in1, op0, op1), r, w)

    def cp(self, out, in_, r=(), w=(), eng="vector"):
        if eng == "scalar":
            return self.op(eng, lambda e: e.activation(out, in_, AF.Copy), r, w)
        return self.op(eng, lambda e: e.tensor_copy(out, in_), r, w)

    def memset(self, ap, val, w=(), eng="vector"):
        return self.op(eng, lambda e: e.memset(ap, val), (), w)

    def finish(self):
        waits = []
        for e in self.CE:
            if self.tick[e]:
                waits.append((e, self.tick[e]))
        for i in range(self.ndma):
            if self.duse[i]:
                waits.append(("d%d" % i, 16 * self.duse[i]))
        for i in range(self.ncoll):
            waits.append(("c%d" % i, 1))
        self.lists["sync"].append((waits, None, None, 0))

    def emit(self):
        self.finish()
        P = self
        with self.nc.Block() as block:
            def mk(name):
                def body(eng):
                    for waits, fn, skey, inc in P.lists[name]:
                        for k, v in waits:
                            eng.wait_ge(P.sems[k], v)
                        if fn is not None:
                            if inc == 1 and skey.startswith("c"):
                                fn(eng).then_inc(P.sems[skey])
                            else:
                                fn(eng).then_inc(P.sems[skey], inc)
                return body
            block.tensor(mk("tensor"))
            block.vector(mk("vector"))
            block.scalar(mk("scalar"))
            block.gpsimd(mk("gpsimd"))
            block.sync(mk("sync"))


KIND_COLS = {
    "mla": (("cq", 256), ("ckv", 128), ("kr", 96), ("krs", 96)),
    "mlstm": (("mq", 128), ("mk", 128), ("mv", 128), ("mo", 128), ("mg", 8)),
    "rwkv": (("rr", 128), ("rk", 128), ("rv", 128), ("rw", 128), ("ra", 128), ("rg", 128)),
}
A_COLS = {}
NCA = {}
for _k, _lst in KIND_COLS.items():
    _o = 0
    for _n, _w in _lst:
        A_COLS[_n] = (_o, _w)
        _o += _w
    NCA[_k] = _o
YW = {"mla": 256, "mlstm": 128, "rwkv": 128}

QBLOCKS = [(0, 256)] + [(256 + 512 * i, 512) for i in range(8)]


def phase_ln(nc, P, ps, ident, xrow_fn, cT, wmodA, bmodA, xres_list):
    csil = P.sb("csil", [128, 8, 2], F32)
    P.dma("sync", csil[:], cT[:, :, :], w=[csil])
    P.act(csil[:], csil[:], AF.Silu, r=[csil], w=[csil])
    bmod = P.sb("bmod", [128, 16], F32)
    P.dma("sync", bmod[:], bmodA[:, :], w=[bmod])
    modc = P.sb("modc", [128, 16, 2], F32)
    wm = [P.sb("wm%d" % i, [128, 8, 128], F32) for i in range(2)]
    for oc in range(16):
        wq = wm[oc % 2]
        P.dma("sync", wq[:], wmodA[:, oc * 128:(oc + 1) * 128].rearrange("(k p) c -> p k c", p=128), w=[wq])
        pt = ps[oc % 2]
        for kc in range(8):
            P.mm(pt[:, 0:2], wq[:, kc, :], csil[:, kc, :],
                 start=(kc == 0), stop=(kc == 7), r=[wq, csil], w=[pt])
        P.act(modc[:, oc, :], pt[:, 0:2], AF.Identity, r=[pt, bmod], w=[modc],
              bias=bmod[:, oc:oc + 1])
    P.ts(modc[:, 8:16, :], modc[:, 8:16, :], 1.0, None, ALU.add, r=[modc], w=[modc])
    hT = P.sb("hT", [128, 8, T], BF16)
    xt = [P.sb("xt%d" % i, [128, D], F32) for i in range(2)]
    st = P.sb("st", [128, 12], F32)
    mv = P.sb("mv", [128, 2], F32)
    rs = P.sb("rs", [128, 1], F32)
    for tt in range(NT):
        x_ = xt[tt % 2]
        n_ = x_
        m = 1 if tt < 2 else 0
        src, sres = xrow_fn(tt)
        P.dma("sync", x_[:], src, r=sres, w=[x_])
        P.call("vector", "bn_stats", st[:, 0:6], x_[:, 0:512], r=[x_], w=[st])
        P.call("vector", "bn_stats", st[:, 6:12], x_[:, 512:1024], r=[x_], w=[st])
        P.call("vector", "bn_aggr", mv[:], st[:], r=[st], w=[mv])
        P.act(rs[:], mv[:, 1:2], AF.Sqrt, r=[mv], w=[rs], bias=1e-6)
        P.call("vector", "reciprocal", rs[:], rs[:], r=[rs], w=[rs])
        P.ts(n_[:], x_[:], mv[:, 0:1], rs[:, 0:1], ALU.subtract, ALU.mult, r=[x_, mv, rs], w=[n_])
        for half in range(2):
            pt = ps[half]
            for j4 in range(4):
                j = half * 4 + j4
                P.tr(pt[:, j4 * 128:(j4 + 1) * 128], n_[:, j * 128:(j + 1) * 128], ident[:],
                     r=[n_, ident], w=[pt])
            for j4 in range(4):
                j = half * 4 + j4
                P.act(hT[:, j, tt * 128:(tt + 1) * 128], pt[:, j4 * 128:(j4 + 1) * 128], AF.Identity,
                      r=[pt, modc], w=[hT.sub(tt)],
                      scale=modc[:, 8 + j, m:m + 1], bias=modc[:, j, m:m + 1])
    hres = [hT.sub(tt) for tt in range(NT)]
    return hT, hres, xt


def make_proj_fm(P, hT, hres, winb):
    def proj_fm(pt, col0, ncols, t0, w, first=True, last=True, src=None, wsrc=None):
        src = src or hT
        wsrc = wsrc or winb
        for kc in range(8):
            P.mm(pt[0:ncols, 0:w], wsrc[:, kc, col0:col0 + ncols], src[:, kc, t0:t0 + w],
                 start=(first and kc == 0), stop=(last and kc == 7),
                 r=[wsrc] + (hres if src is hT else [src]), w=[pt])
    return proj_fm


def build_A(kind, emit_ctx, dbg=()):
    nc = bass.Bass("TRN2", target_bir_lowering=False)
    P = Prog(nc)

    def din(name, shape, dt=F32):
        return nc.dram_tensor(name, list(shape), dt, kind="ExternalInput").ap()

    xin = din("xin", [T, D])
    cT = din("cT", [128, 8, 2])
    wmodA = din("wmodA", [D, 2048])
    bmodA = din("bmodA", [128, 16])
    winA = din("winA", [D, NCA[kind]])
    ident_d = din("ident", [128, 128])
    ymix = nc.dram_tensor("ymix", [T, YW[kind]], F32, kind="ExternalOutput").ap()

    ident = P.sb("ident", [128, 128], F32)
    P.dma("sync", ident[:], ident_d[:, :], w=[ident])
    identb = P.sb("identb", [128, 128], BF16)
    P.cp(identb[:], ident[:], r=[ident], w=[identb])
    onesb = P.sb("onesb", [128, 128], BF16)
    P.memset(onesb[:], 1.0, w=[onesb])
    ps = [P.ps("ps%d" % i, [128, 512]) for i in range(8)]
    winb = P.sb("winb", [128, 8, NCA[kind]], BF16)
    for kc in range(8):
        P.dma("gpsimd", winb[:, kc, :], winA[kc * 128:(kc + 1) * 128, :], w=[winb])
    hT, hres, xt = phase_ln(nc, P, ps, ident, lambda tt: (xin[tt * 128:(tt + 1) * 128, :], []), cT, wmodA, bmodA, None)
    proj_fm = make_proj_fm(P, hT, hres, winb)
    env = dict(nc=nc, P=P, din=din, ps=ps, hT=hT, hres=hres, winb=winb, proj_fm=proj_fm, ident=ident,
               identb=identb, onesb=onesb, ymix=ymix, emit_ctx=emit_ctx, dbg=dbg, xt=xt)
    if kind == "mla":
        build_mla(**env)
    elif kind == "mlstm":
        build_mlstm(**env)
    else:
        build_rwkv(**env)
    P.emit()
    return nc


def build_mla(nc, P, din, ps, hT, hres, winb, proj_fm, ident, identb, onesb, ymix, emit_ctx, dbg, xt):
    wuq_d = din("wuq", [256, 4, 192])
    wukv_d = din("wukv", [128, 512])
    qn_d = din("qnorm", [128, 2])
    kvn_d = din("kvnorm", [128, 1])
    cs_d = din("cossin", [32, 2, 4096], BF16)

    wuqb = P.sb("wuqb", [128, 2, 768], BF16)
    qn = P.sb("qn", [128, 2], F32)
    P.dma("sync", qn[:], qn_d[:, :], w=[qn])
    for ch in range(2):
        P.dma("sync", xt[ch][:, 0:768], wuq_d[ch * 128:(ch + 1) * 128, :, :].rearrange("p h c -> p (h c)"), w=[xt[ch]])
    for ch in range(2):
        P.ts(wuqb[:, ch, :], xt[ch][:, 0:768], qn[:, ch:ch + 1], None, ALU.mult, r=[xt[ch], qn], w=[wuqb])
    wukvb = P.sb("wukvb", [128, 512], BF16)
    kvnw = P.sb("kvnw", [128, 1], F32)
    P.dma("sync", kvnw[:], kvn_d[:, :], w=[kvnw])
    cs = P.sb("cs", [128, 2, 4096], BF16)
    P.dma("sync", cs[64:96, :, :], cs_d[:, :, :], w=[cs])

    KT = [P.sb("KT%d" % h, [96, T], BF16) for h in range(4)]
    Vp = P.sb("Vp", [128, NT, 4, 65], BF16)
    P.memset(Vp[:], 1.0, w=[Vp])
    c32 = P.sb("c32", [128, 2, 512], F32)
    sq = P.sb("sqb", [128, 2, 512], BF16)
    rb = P.sb("rb", [128, 512], F32)
    cn = P.sb("cn", [128, 2, 512], BF16)
    t1 = P.sb("ropet1", [128, 512], F32)
    t2 = P.sb("ropet2", [128, 512], F32)
    P.dma("sync", t1[:], wukv_d[:, :], w=[t1])
    P.ts(wukvb[:], t1[:], kvnw[:, 0:1], None, ALU.mult, r=[t1, kvnw], w=[wukvb])

    def rmsnorm(nch, col0, t0, w, pbase):
        for ch in range(nch):
            pt = ps[pbase + ch]
            proj_fm(pt, col0 + ch * 128, 128, t0, w)
            P.act(c32[:, ch, 0:w], pt[:, 0:w], AF.Copy, r=[pt], w=[c32])
            P.act(sq[:, ch, 0:w], pt[:, 0:w], AF.Square, r=[pt], w=[sq], scale=float((128 * nch) ** -0.5))
        pt = ps[pbase + 2]
        for ch in range(nch):
            P.mm(pt[:, 0:w], onesb[:], sq[:, ch, 0:w], start=(ch == 0), stop=(ch == nch - 1), r=[onesb, sq], w=[pt])
        P.act(rb[:, 0:w], pt[:, 0:w], AF.Sqrt, r=[pt], w=[rb], bias=1e-6)
        P.call("vector", "reciprocal", rb[:, 0:w], rb[:, 0:w], r=[rb], w=[rb])
        for ch in range(nch):
            P.tt(cn[:, ch, 0:w], c32[:, ch, 0:w], rb[:, 0:w], ALU.mult, r=[c32, rb], w=[cn])

    def rope(dst, pq, psw, t0, w, lat):
        if not lat:
            P.cp(dst[64:96, t0:t0 + w], pq[64:96, 0:w], r=[pq], w=[dst])
            return
        l0 = t0 - NCTX
        P.tt(t1[64:96, 0:w], pq[64:96, 0:w], cs[64:96, 0, l0:l0 + w], ALU.mult, r=[pq, cs], w=[t1])
        P.tt(t2[64:96, 0:w], psw[64:96, 0:w], cs[64:96, 1, l0:l0 + w], ALU.mult, r=[psw, cs], w=[t2])
        P.tt(dst[64:96, t0:t0 + w], t1[64:96, 0:w], t2[64:96, 0:w], ALU.add, r=[t1, t2], w=[dst])

    ckv0 = A_COLS["ckv"][0]
    for (t0, w) in QBLOCKS:
        lat = t0 >= NCTX
        rmsnorm(1, ckv0, t0, w, 0)
        proj_fm(ps[3], A_COLS["kr"][0], 96, t0, w)
        if lat:
            proj_fm(ps[4], A_COLS["krs"][0], 96, t0, w)
        rope(KT[0], ps[3], ps[4], t0, w, lat)
        for h in range(4):
            pt = ps[5 + (h % 2)]
            P.mm(pt[0:64, 0:w], wukvb[:, h * 64:(h + 1) * 64], cn[:, 0, 0:w], r=[wukvb, cn], w=[pt])
            P.cp(KT[h][0:64, t0:t0 + w], pt[0:64, 0:w], r=[pt], w=[KT[h]], eng="scalar")
            if h > 0:
                P.cp(KT[h][64:96, t0:t0 + w], KT[0][64:96, t0:t0 + w], r=[KT[0]], w=[KT[h]], eng="gpsimd")
        for ti in range(w // 128):
            tt = t0 // 128 + ti
            pt = ps[7]
            P.mm(pt[:, 0:256], cn[:, 0, ti * 128:(ti + 1) * 128], wukvb[:, 256:512], r=[wukvb, cn], w=[pt])
            P.cp(Vp[:, tt, :, 0:64], pt[:, 0:256].rearrange("p (h c) -> p h c", h=4), r=[pt], w=[Vp])

    QT = [P.sb("QT%d" % h, [96, 512], BF16) for h in range(4)]
    PT = [P.sb("PT%d" % i, [128, 512], BF16) for i in range(3)]
    zerob = P.sb("zerob", [128, 512], BF16)
    P.memset(zerob[:], 0.0, w=[zerob])
    rden = P.sb("rden", [128, 4, 1], F32)
    oT = P.sb("oT", [65, 512], F32)
    yo = [P.sb("yo%d" % i, [128, 4, 64], F32) for i in range(2)]
    cq0 = A_COLS["cq"][0]
    it = 0
    for (t0, w) in QBLOCKS:
        lat = t0 >= NCTX
        if not lat and not emit_ctx:
            continue
        rmsnorm(2, cq0, t0, w, 0)
        for h in range(4):
            pq = ps[3]
            psw = ps[4]
            for ch in range(2):
                P.mm(pq[0:96, 0:w], wuqb[:, ch, h * 192:h * 192 + 96], cn[:, ch, 0:w],
                     start=(ch == 0), stop=(ch == 1), r=[wuqb, cn], w=[pq])
            if lat:
                for ch in range(2):
                    P.mm(psw[0:96, 0:w], wuqb[:, ch, h * 192 + 96:h * 192 + 192], cn[:, ch, 0:w],
                         start=(ch == 0), stop=(ch == 1), r=[wuqb, cn], w=[psw])
            P.cp(QT[h][0:64, 0:w], pq[0:64, 0:w], r=[pq], w=[QT[h]], eng="scalar")
            ropeq(P, QT[h], pq, psw, w, lat, t0, cs, t1, t2)
        nq = w // 128
        kts = range(NT) if lat else range(2)
        for h in range(4):
            accT = ps[5 + (h % 2)]
            units = list(kts)

            def qk(kt, j):
                sp_ = ps[j % 3]
                P.mm(sp_[:, 0:w], KT[h][0:96, kt * 128:(kt + 1) * 128], QT[h][0:96, 0:w], r=[KT[h], QT[h]], w=[sp_])
                return sp_
            spq = [qk(units[j], it + j) for j in range(min(2, len(units)))]
            for idx, kt in enumerate(units):
                sp = spq[idx]
                pt_ = PT[it % 3]
                if idx + 2 < len(units):
                    spq.append(qk(units[idx + 2], it + 2))
                it += 1
                P.act(pt_[:, 0:w], sp[:, 0:w], AF.Exp, r=[sp], w=[pt_], scale=ISQ96)
                P.mm(accT[0:65, 0:w], Vp[:, kt, h, :], pt_[:, 0:w], start=(idx == 0), stop=(idx == len(units) - 1),
                     r=[pt_, Vp], w=[accT])
            P.cp(oT[0:65, 0:w], accT[0:65, 0:w], r=[accT], w=[oT])
            acc = ps[7]
            for qi in range(nq):
                P.tr(acc[:, qi * 65:(qi + 1) * 65], oT[0:65, qi * 128:(qi + 1) * 128], ident[0:65, 0:65],
                     r=[oT, ident], w=[acc])
            y_ = yo[h % 2]
            a3 = acc[:, 0:nq * 65].rearrange("p (q c) -> p q c", c=65)
            P.call("vector", "reciprocal", rden[:, 0:nq, :], a3[:, :, 64:65], r=[acc], w=[rden])
            P.tt(y_[:, 0:nq, :], a3[:, :, 0:64], rden[:, 0:nq, :].to_broadcast([128, nq, 64]), ALU.mult,
                 r=[acc, rden], w=[y_])
            for qi in range(nq):
                r0 = t0 + qi * 128
                P.dma("sync", ymix[r0:r0 + 128, h * 64:(h + 1) * 64], y_[:, qi, :], r=[y_])


def ropeq(P, dst, pq, psw, w, lat, t0, cs, t1, t2):
    if not lat:
        P.cp(dst[64:96, 0:w], pq[64:96, 0:w], r=[pq], w=[dst])
        return
    l0 = t0 - NCTX
    P.tt(t1[64:96, 0:w], pq[64:96, 0:w], cs[64:96, 0, l0:l0 + w], ALU.mult, r=[pq, cs], w=[t1])
    P.tt(t2[64:96, 0:w], psw[64:96, 0:w], cs[64:96, 1, l0:l0 + w], ALU.mult, r=[psw, cs], w=[t2])
    P.tt(dst[64:96, 0:w], t1[64:96, 0:w], t2[64:96, 0:w], ALU.add, r=[t1, t2], w=[dst])


def _cols_A(hp, kind):
    N_M = 1040
    RB = N_M
    MB = N_M + 1152
    hs = [2 * hp, 2 * hp + 1]
    idx = {}
    idx["cq"] = list(range(MB, MB + 256))
    idx["ckv"] = list(range(MB + 256, MB + 384))
    kr = list(range(MB + 384, MB + 416))
    idx["kr"] = [-1] * 64 + kr
    idx["krs"] = [-1] * 64 + [kr[i ^ 1] for i in range(32)]
    idx["mq"] = list(range(128 * hp, 128 * hp + 128))
    idx["mk"] = list(range(256 + 128 * hp, 256 + 128 * hp + 128))
    idx["mv"] = list(range(512 + 128 * hp, 512 + 128 * hp + 128))
    idx["mo"] = list(range(768 + 128 * hp, 768 + 128 * hp + 128))
    idx["mg"] = [1024 + g * 4 + h for g in (0, 2, 1, 3) for h in hs]
    idx["rr"] = list(range(RB + 128 * hp, RB + 128 * hp + 128))
    idx["rk"] = list(range(RB + 256 + 128 * hp, RB + 256 + 128 * hp + 128))
    idx["rv"] = list(range(RB + 512 + 128 * hp, RB + 512 + 128 * hp + 128))
    idx["rw"] = list(range(RB + 768, RB + 896))
    idx["ra"] = list(range(RB + 896, RB + 1024))
    idx["rg"] = list(range(RB + 1024, RB + 1152))
    out = []
    for n, w in KIND_COLS[kind]:
        assert len(idx[n]) == w, n
        out += idx[n]
    return np.array(out)


def _gather_cols(w, cols):
    out = np.zeros((w.shape[0], len(cols)), w.dtype)
    m = cols >= 0
    out[:, m] = w[:, cols[m]]
    return out


def _col_layout(v):
    return np.ascontiguousarray(v.reshape(-1, 128).T)


def _rope_tables():
    n = 4096
    row = np.repeat(np.arange(64), 64).astype(np.float32)
    col = np.tile(np.arange(64), 64).astype(np.float32)
    freq = (np.float32(10000.0) ** (-np.arange(8, dtype=np.float32) / np.float32(8))).astype(np.float32)
    ang = np.concatenate([row[:, None] * freq, col[:, None] * freq], -1)
    cos = np.cos(ang).astype(np.float32)
    sin = np.sin(ang).astype(np.float32)
    tab = np.zeros((32, 2, n), np.float32)
    for r in range(32):
        tab[r, 0] = cos[:, r // 2]
        tab[r, 1] = sin[:, r // 2] * (-1.0 if r % 2 == 0 else 1.0)
    return tab.astype(ml_dtypes.bfloat16)


def prep_A(inp, l, x_cur, xc_cur, kind):
    maps = []
    ident = np.eye(128, dtype=np.float32)
    cs = _rope_tables()
    for core in range(8):
        b, hp = core // 2, core % 2
        m = {}
        m["xin"] = np.ascontiguousarray(np.concatenate([xc_cur[b], x_cur[b]], 0))
        cc = np.stack([inp["c"][b], inp["c_ctx"]], -1)
        m["cT"] = np.ascontiguousarray(cc.reshape(8, 128, 2).transpose(1, 0, 2))
        m["wmodA"] = np.ascontiguousarray(inp["w_mod"][l][:, 0:2048])
        m["bmodA"] = _col_layout(inp["b_mod"][l][0:2048])
        m["winA"] = _gather_cols(inp["w_in"][l], _cols_A(hp, kind))
        m["ident"] = ident
        if kind == "mlstm":
            cw = inp["mlstm_conv"][l]
            cc_ = np.concatenate([cw[:, 128 * hp:128 * hp + 128], cw[:, 256 + 128 * hp:256 + 128 * hp + 128]], 1)
            m["convbc"] = np.ascontiguousarray(np.broadcast_to(cc_[None], (128, 3, 256)))
            gb = inp["mlstm_gate_bias"][l]
            gv = np.array([gb[g, h] for g in (0, 2, 1, 3) for h in (2 * hp, 2 * hp + 1)], np.float32)
            m["gbias"] = np.ascontiguousarray(np.broadcast_to(gv[None], (128, 8)))
            nw = inp["mlstm_norm_w"][l][128 * hp:128 * hp + 128]
            m["normw"] = np.ascontiguousarray(np.broadcast_to(nw[None], (128, 128)))
            s_ = np.arange(128)[:, None]
            j_ = np.arange(512)[None, :]
            mf = np.stack([(s_ + 128 * o <= j_) for o in range(4)], 1)
            mb = np.stack([(s_ + 128 * o >= j_) for o in range(4)], 1)
            m["maskf"] = mf.astype(np.float32).astype(ml_dtypes.bfloat16)
            m["maskb"] = mb.astype(np.float32).astype(ml_dtypes.bfloat16)
            m["triu"] = (s_ <= np.arange(128)[None, :]).astype(np.float32)
        if kind == "rwkv":
            RB = 1040
            cols = _cols_A(hp, "rwkv") - RB
            m["mubc"] = np.ascontiguousarray(np.broadcast_to(inp["rwkv_mu"][l][cols][None], (128, 768)))
            hc = slice(128 * hp, 128 * hp + 128)
            pc = np.zeros((128, 8), np.float32)
            pc[:, 0] = inp["rwkv_w0"][l][0][hc]
            pc[:, 1] = inp["rwkv_w0"][l][1][hc]
            pc[:, 2] = inp["rwkv_a0"][l][0][hc]
            pc[:, 3] = inp["rwkv_a0"][l][1][hc]
            pc[:, 4] = inp["rwkv_k_k"][l][hc]
            pc[:, 5] = inp["rwkv_k_a"][l][hc]
            pc[:, 6] = inp["rwkv_r_k"][l][hc]
            m["pcol"] = pc
            m["wup"] = np.ascontiguousarray(inp["rwkv_w_up"][l][:, :, hc].reshape(128, 128))
            m["aup"] = np.ascontiguousarray(inp["rwkv_a_up"][l][:, :, hc].reshape(128, 128))
            m["gup"] = np.ascontiguousarray(inp["rwkv_g_up"][l][:, hc])
            bo = np.zeros((128, 128), np.float32)
            bo[0:64, 0:64] = 1.0
            bo[64:128, 64:128] = 1.0
            m["bones"] = bo
        if kind != "mla":
            maps.append(m)
            continue
        wuq = inp["mla_w_uq"][l].reshape(256, 8, 96)[:, 4 * hp:4 * hp + 4, :]
        w4 = np.zeros((256, 4, 192), np.float32)
        w4[:, :, 0:96] = wuq
        sw = [64 + (i ^ 1) for i in range(32)]
        w4[:, :, 160:192] = wuq[:, :, sw]
        m["wuq"] = w4
        wukv = inp["mla_w_ukv"][l].reshape(128, 8, 128)[:, 4 * hp:4 * hp + 4, :]
        m["wukv"] = np.ascontiguousarray(np.concatenate(
            [wukv[:, :, 0:64].reshape(128, 256), wukv[:, :, 64:128].reshape(128, 256)], 1))
        m["qnorm"] = _col_layout(inp["mla_q_norm"][l])
        m["kvnorm"] = _col_layout(inp["mla_kv_norm"][l])
        m["cossin"] = cs
        maps.append(m)
    return maps


def build_mlstm(nc, P, din, ps, hT, hres, winb, proj_fm, ident, identb, onesb, ymix, emit_ctx, dbg, xt):
    convbc_d = din("convbc", [128, 3, 256])
    gbias_d = din("gbias", [128, 8])
    normw_d = din("normw", [128, 128])
    maskf_d = din("maskf", [128, 4, 512], BF16)
    maskb_d = din("maskb", [128, 4, 512], BF16)
    triu_d = din("triu", [128, 128])

    convbc = P.sb("convbc", [128, 3, 256], F32)
    P.dma("sync", convbc[:], convbc_d[:, :, :], w=[convbc])
    gbias = P.sb("gbias", [128, 8], F32)
    P.dma("sync", gbias[:], gbias_d[:, :], w=[gbias])
    normw = P.sb("normw", [128, 128], F32)
    P.dma("sync", normw[:], normw_d[:, :], w=[normw])
    maskf = P.sb("maskf", [128, 4, 512], BF16)
    P.dma("sync", maskf[:], maskf_d[:, :, :], w=[maskf])
    maskb = P.sb("maskb", [128, 4, 512], BF16)
    P.dma("sync", maskb[:], maskb_d[:, :, :], w=[maskb])
    triu = P.sb("triu", [128, 128], F32)
    P.dma("sync", triu[:], triu_d[:, :], w=[triu])
    ones32 = P.sb("ones32", [128, 128], F32)
    P.memset(ones32[:], 1.0, w=[ones32])

    wc = P.sb("wc", [128, 8, 3, 256], BF16)
    q0 = A_COLS["mq"][0]
    for kc in range(8):
        for j in range(3):
            P.tt(wc[:, kc, j, :], winb[:, kc, q0:q0 + 256], convbc[:, j, :], ALU.mult, r=[winb, convbc], w=[wc],
                 eng="vector")

    QK = [P.sb("QmT", [128, T], BF16), P.sb("KmT", [128, T], BF16)]
    Vp = P.sb("Vpm", [128, NT, 2, 65], BF16)
    P.memset(Vp[:], 1.0, w=[Vp])
    og = P.sb("og", [128, NT, 128], BF16)
    G = P.sb("G", [128, NT, 8], F32)
    sgt = P.sb("sgt", [128, 128], F32)

    STOP = 9
    for (t0, w) in QBLOCKS:
        seg0 = t0 in (0, NCTX)
        seg1 = (t0 + w) in (NCTX, T)
        for qk in range(2):
            pt = ps[5 + qk]
            first = True
            for j in (1, 0, 2):
                a = 1 if (j == 0 and seg0) else 0
                b = 1 if (j == 2 and seg1) else 0
                for kc in range(8):
                    last = (j == 2 and kc == 7)
                    P.mm(pt[:, a:w - b], wc[:, kc, j, qk * 128:(qk + 1) * 128],
                         hT[:, kc, t0 + a + j - 1:t0 + w - b + j - 1],
                         start=first, stop=last, r=[wc] + hres, w=[pt], skip=True)
                    first = False
            P.act(QK[qk][:, t0:t0 + w], pt[:, 0:w], AF.Silu, r=[pt], w=[QK[qk]])
        v0 = A_COLS["mv"][0]
        if STOP == -1:
            continue
        for ti in range(w // 128):
            tt = t0 // 128 + ti
            pt = ps[7]
            for kc in range(8):
                P.mm(pt[:, 0:264], hT[:, kc, tt * 128:(tt + 1) * 128], winb[:, kc, v0:v0 + 264],
                     start=(kc == 0), stop=(kc == 7), r=[winb] + hres, w=[pt])
            P.cp(Vp[:, tt, :, 0:64], pt[:, 0:128].rearrange("p (h c) -> p h c", h=2), r=[pt], w=[Vp])
            P.cp(sgt[:], pt[:, 128:256], r=[pt], w=[sgt])
            P.act(sgt[:], sgt[:], AF.Exp, r=[sgt], w=[sgt], scale=-1.0)
            P.ts(sgt[:], sgt[:], 1.0, None, ALU.add, r=[sgt], w=[sgt])
            P.call("vector", "reciprocal", sgt[:], sgt[:], r=[sgt], w=[sgt])
            P.cp(og[:, tt, :], sgt[:], r=[sgt], w=[og])
            P.tt(G[:, tt, :], pt[:, 256:264], gbias[:], ALU.add, r=[pt, gbias], w=[G])

    if STOP < 1:
        return
    LF = P.sb("LF", [128, NT, 4], F32)
    P.act(LF[:], G[:, :, 4:8], AF.Exp, r=[G], w=[LF], scale=-1.0)
    P.act(LF[:], LF[:], AF.Ln, r=[LF], w=[LF], bias=1.0)
    P.ts(LF[:], LF[:], -1.0, None, ALU.mult, r=[LF], w=[LF])
    PW = P.sb("PW", [128, NT, 4], F32)
    TOT = P.sb("TOT", [128, NT, 4], F32)
    IP = P.sb("IP", [128, NT, 4], F32)
    BQ = P.sb("BQ", [128, NT, 4], F32)
    UK = P.sb("UK", [128, NT, 4], F32)
    lf2 = LF[:].rearrange("p t c -> p (t c)")
    P.mm(ps[5][:, 0:NT * 4], triu[:], lf2, r=[triu, LF], w=[ps[5]])
    P.cp(PW[:].rearrange("p t c -> p (t c)"), ps[5][:, 0:NT * 4], r=[ps[5]], w=[PW])
    P.mm(ps[6][:, 0:NT * 4], ones32[:], lf2, r=[ones32, LF], w=[ps[6]])
    P.cp(TOT[:].rearrange("p t c -> p (t c)"), ps[6][:, 0:NT * 4], r=[ps[6]], w=[TOT])
    for c in range(4):
        for (a, b) in ((0, 2), (2, NT)):
            P.call("vector", "tensor_tensor_scan", IP[:, a:b, c], ones32[:, 0:b - a], TOT[:, a:b, c], 0.0,
                   ALU.mult, ALU.add, r=[TOT, ones32], w=[IP])
    EX = TOT
    P.tt(EX[:], IP[:], TOT[:], ALU.subtract, r=[IP, TOT], w=[TOT])
    P.tt(BQ[:, :, 0:2], PW[:, :, 0:2], EX[:, :, 0:2], ALU.add, r=[PW, TOT], w=[BQ])
    for c in range(2):
        P.ts(BQ[:, 2:NT, c], BQ[:, 2:NT, c], IP[:, 1, c:c + 1], None, ALU.add, r=[BQ, IP], w=[BQ])
    P.tt(BQ[:, :, 2:4], LF[:, :, 2:4], PW[:, :, 2:4], ALU.subtract, r=[LF, PW], w=[BQ])
    P.tt(BQ[:, :, 2:4], BQ[:, :, 2:4], EX[:, :, 2:4], ALU.subtract, r=[BQ, TOT], w=[BQ])
    for c in range(2, 4):
        P.ts(BQ[:, 2:NT, c], BQ[:, 2:NT, c], IP[:, NT - 1, c:c + 1], None, ALU.add, r=[BQ, IP], w=[BQ])
    P.tt(UK[:], G[:, :, 0:4], BQ[:], ALU.subtract, r=[G, BQ], w=[UK])
    P.ts(UK[:], UK[:], float(-np.log(8.0)), None, ALU.add, r=[UK], w=[UK])

    if STOP < 2:
        return
    BL = [P.sb("BL%d" % i, [128, 128], F32) for i in range(2)]
    BCs2 = [P.sb("BCs", [128, 512], F32) for i in range(2)]
    igrp = 0
    DT = [P.sb("DT%d" % i, [128, 512], F32) for i in range(3)]
    WT = [P.sb("WT%d" % i, [128, 512], BF16) for i in range(3)]
    WM = P.sb("WM", [128, 512], F32)
    zerob = P.sb("zerob", [128, 512], BF16)
    P.memset(zerob[:], 0.0, w=[zerob])
    den = P.sb("den", [128, 4, 1], F32)
    oTm = P.sb("oTm", [65, 512], F32)
    nden = P.sb("nden", [128, 4, 1], F32)
    c60 = P.sb("c60", [128, 1], F32)
    P.memset(c60[:], 60.0, w=[c60])
    hd = [P.sb("hd%d" % i, [128, 4, 64], F32) for i in range(2)]
    st = P.sb("st2", [128, 4, 6], F32)
    mv = P.sb("mv2", [128, 4, 2], F32)
    yo = [P.sb("yom%d" % i, [128, 4, 64], F32) for i in range(2)]
    it = 0
    ib = 0
    for (t0, w) in QBLOCKS:
        lat = t0 >= NCTX
        if not lat and not emit_ctx:
            continue
        nq = w // 128
        tq0 = t0 // 128
        for hl in range(2):
            hp_ = slice(hl * 64, hl * 64 + 64)
            for d in range(2):
                c = d * 2 + hl
                BCs = BCs2[igrp % 2]
                igrp += 1
                for qi in range(nq):
                    bl = BL[ib % 2]
                    ib += 1
                    P.ts(bl[:], ones32[:], BQ[:, tq0 + qi, c:c + 1], None, ALU.mult, r=[ones32, BQ], w=[bl])
                    P.mm(ps[2][:, qi * 128:(qi + 1) * 128], bl[:], ident[:], r=[bl, ident], w=[ps[2]])
                P.cp(BCs[:, 0:w], ps[2][:, 0:w], r=[ps[2]], w=[BCs], eng="scalar")
                if d == 0:
                    kts = [kt for kt in range(NT) if kt * 128 < t0 + w and (lat or kt < 2)]
                else:
                    kts = [kt for kt in range(NT) if (kt < 2 and lat) or ((kt * 128 + 128 > t0) and (lat == (kt >= 2)))]
                accT = ps[3 + d]

                def qk(kt, j):
                    sp_ = (ps[0], ps[1], ps[7])[j % 3]
                    P.mm(sp_[:, 0:w], QK[1][hp_, kt * 128:(kt + 1) * 128], QK[0][hp_, t0:t0 + w], r=QK, w=[sp_])
                    return sp_
                spq = [qk(kts[j], it + j) for j in range(min(2, len(kts)))]
                for idx, kt in enumerate(kts):
                    diag = (kt * 128 >= t0) and (kt * 128 < t0 + w)
                    sp = spq[idx]
                    dt_ = DT[it % 3]
                    wt_ = WT[it % 3]
                    if idx + 2 < len(kts):
                        spq.append(qk(kts[idx + 2], it + 2))
                    it += 1
                    if diag:
                        P.ts(dt_[:, 0:w], BCs[:, 0:w], UK[:, kt, c:c + 1], c60[:, 0:1], ALU.add, ALU.min, r=[BCs, UK, c60], w=[dt_])
                        P.act(dt_[:, 0:w], dt_[:, 0:w], AF.Exp, r=[dt_], w=[dt_])
                        P.tt(WM[:, 0:w], sp[:, 0:w], dt_[:, 0:w], ALU.mult, r=[sp, dt_], w=[WM])
                        mk = (maskf if d == 0 else maskb)
                        P.tt(wt_[:, 0:w], WM[:, 0:w], mk[:, (kt * 128 - t0) // 128, 0:w], ALU.mult, r=[WM, mk], w=[wt_])
                    else:
                        P.act(dt_[:, 0:w], BCs[:, 0:w], AF.Exp, r=[BCs, UK], w=[dt_], bias=UK[:, kt, c:c + 1])
                        P.tt(wt_[:, 0:w], sp[:, 0:w], dt_[:, 0:w], ALU.mult, r=[sp, dt_], w=[wt_])
                    P.mm(accT[0:65, 0:w], Vp[:, kt, hl, :], wt_[:, 0:w], start=(idx == 0), stop=(idx == len(kts) - 1),
                         r=[wt_, Vp], w=[accT])
                P.cp(oTm[0:65, 0:w], accT[0:65, 0:w], r=[accT], w=[oTm])
                acc = ps[5 + d]
                for qi in range(nq):
                    P.tr(acc[:, qi * 65:(qi + 1) * 65], oTm[0:65, qi * 128:(qi + 1) * 128], ident[0:65, 0:65],
                         r=[oTm, ident], w=[acc])
                a3 = acc[:, 0:nq * 65].rearrange("p (q c) -> p q c", c=65)
                P.cp(den[:, 0:nq, :], a3[:, :, 64:65], r=[acc], w=[den])
                P.ts(nden[:, 0:nq, :], den[:, 0:nq, :], -1.0, None, ALU.mult, r=[den], w=[nden])
                P.tt(den[:, 0:nq, :], den[:, 0:nq, :], nden[:, 0:nq, :], ALU.max, r=[den, nden], w=[den])
                P.ts(den[:, 0:nq, :], den[:, 0:nq, :], 1.0, None, ALU.max, r=[den], w=[den])
                P.call("vector", "reciprocal", den[:, 0:nq, :], den[:, 0:nq, :], r=[den], w=[den])
                P.tt(hd[d][:, 0:nq, :], a3[:, :, 0:64], den[:, 0:nq, :].to_broadcast([128, nq, 64]), ALU.mult,
                     r=[acc, den], w=[hd[d]])
            h_ = hd[0]
            P.tt(h_[:, 0:nq, :], hd[0][:, 0:nq, :], hd[1][:, 0:nq, :], ALU.add, r=hd, w=[hd[0]])
            y_ = yo[hl]
            for qi in range(nq):
                P.call("vector", "bn_stats", st[:, qi, :], h_[:, qi, :], r=[h_], w=[st])
                P.call("vector", "bn_aggr", mv[:, qi, :], st[:, qi, :], r=[st], w=[mv])
            P.act(mv[:, 0:nq, 1:2], mv[:, 0:nq, 1:2], AF.Sqrt, r=[mv], w=[mv], bias=1e-6)
            P.call("vector", "reciprocal", mv[:, 0:nq, 1:2], mv[:, 0:nq, 1:2], r=[mv], w=[mv])
            for qi in range(nq):
                P.ts(y_[:, qi, :], h_[:, qi, :], mv[:, qi, 0:1], mv[:, qi, 1:2], ALU.subtract, ALU.mult, r=[h_, mv], w=[y_])
                P.tt(y_[:, qi, :], y_[:, qi, :], normw[:, hl * 64:(hl + 1) * 64], ALU.mult, r=[y_, normw], w=[y_])
                P.tt(y_[:, qi, :], y_[:, qi, :], og[:, tq0 + qi, hl * 64:(hl + 1) * 64], ALU.mult, r=[y_, og], w=[y_])
                r0 = t0 + qi * 128
                P.dma("sync", ymix[r0:r0 + 128, hl * 64:(hl + 1) * 64], y_[:, qi, :], r=[y_])


TB = 2176
NTB = 17
ALPHA = 4.0 ** 0.25
BBLOCKS = [(0, 512), (512, 512), (1024, 512), (1536, 512), (2048, 128)]


def build_B():
    nc = bass.Bass("TRN2", target_bir_lowering=False)
    P = Prog(nc)

    def din(name, shape, dt=F32):
        return nc.dram_tensor(name, list(shape), dt, kind="ExternalInput").ap()
    xres = din("xres", [TB, D])
    ymx = din("ymx", [TB, D])
    ident_d = din("ident", [128, 128])
    out = nc.dram_tensor("xout", [TB, D], F32, kind="ExternalOutput").ap()
    ident = P.sb("ident", [128, 128], F32)
    P.dma("sync", ident[:], ident_d[:, :], w=[ident])
    ones32 = P.sb("ones32", [128, 128], F32)
    P.memset(ones32[:], 1.0, w=[ones32])
    ps = [P.ps("ps%d" % i, [128, 512]) for i in range(8)]

    def xres_fn(tt, buf):
        P.dma("sync", buf[:], xres[tt * 128:(tt + 1) * 128, :], w=[buf])

    def ymx_fn(tt, buf, tmp):
        P.dma("sync", buf[:], ymx[tt * 128:(tt + 1) * 128, :], w=[buf])

    def out_fn(tt):
        return out[tt * 128:(tt + 1) * 128, :], []
    phase_B(nc, P, ps, ident, ones32, din, xres_fn, ymx_fn, out_fn)
    P.emit()
    return nc


def phase_B(nc, P, ps, ident, ones32, din, xres_fn, ymx_fn, out_fn, pre=""):
    cT = din("cT", [128, 8, 2]) if not pre else din.shared("cT", [128, 8, 2])
    wmodB = din("wmodB", [D, 4096])
    bmodrow = din("bmodrow", [128, 4096])
    bmodcol = din("bmodcol", [128, 32])
    wout_d = din("wout", [D, D])
    lnrow = din("lnrow", [128, 4, D])
    rw_d = din("rw", [D, 64])
    rb_d = din("rbias", [128, 64])
    eg_d = din("eg", [65, D, 256])
    eu_d = din("eu", [65, D, 256])
    ed_d = din("ed", [65, 256, D])
    x1d = nc.dram_tensor(pre + "x1scr", [TB, D], F32).ap()

    csil = P.sb("csil", [128, 8, 2], F32)
    P.dma("sync", csil[:], cT[:, :, :], w=[csil])
    P.act(csil[:], csil[:], AF.Silu, r=[csil], w=[csil])
    bmc = P.sb("bmc", [128, 32], F32)
    P.dma("sync", bmc[:], bmodcol[:, :], w=[bmc])
    bigall = P.sb("bigall", [128, 4, D], F32)

    class _V:
        def __init__(self, i):
            self.i = i
            self.res = Res()

        def __getitem__(self, k):
            return bigall[:, self.i, :][k] if not isinstance(k, tuple) else bigall[(k[0], self.i) + tuple(k[1:])]
    big = [_V(i) for i in range(4)]
    bigres = [b_.res for b_ in big]
    bigall2 = P.sb("bigall2", [128, 4, D], F32)

    class _V2(_V):
        def __getitem__(self, k):
            return bigall2[:, self.i, :][k] if not isinstance(k, tuple) else bigall2[(k[0], self.i) + tuple(k[1:])]
    big2 = [_V2(i) for i in range(4)]
    modc = P.sb("modc", [128, 16, 2], F32)
    for oc in range(16):
        wq = bigall[:, oc % 2, :].rearrange("p (k c) -> p k c", c=128)
        wqr = bigres[oc % 2]
        c0 = 1024 + oc * 128
        P.dma("sync", wq, wmodB[:, c0:c0 + 128].rearrange("(k p) c -> p k c", p=128), w=[wqr])
        pt = ps[oc % 2]
        for kc in range(8):
            P.mm(pt[:, 0:2], wq[:, kc, :], csil[:, kc, :], start=(kc == 0), stop=(kc == 7), r=[wqr, csil], w=[pt])
        P.act(modc[:, oc, :], pt[:, 0:2], AF.Identity, r=[pt, bmc], w=[modc], bias=bmc[:, 8 + oc:9 + oc])
    P.ts(modc[:, 8:16, :], modc[:, 8:16, :], 1.0, None, ALU.add, r=[modc], w=[modc])
    crep = P.sb("crep", [128, 8, 128], F32)
    growb = [P.sb("grow%d" % m, [128, D], F32) for m in range(2)]
    grow = [[growb[m], growb[m]] for m in range(2)]
    lnr2 = P.sb("lnr", [128, 2, D], F32)

    class _L:
        res = lnr2.res

        def __getitem__(self, k):
            return lnr2[(k[0], k[1] % 2) + tuple(k[2:])]
    lnr = _L()

    def make_gates(k):
        cbase = (0, 3072)[k]
        wrow = bigall[:, 0:2, :].rearrange("p a (k c) -> p (a k) c", c=512)
        for half in range(2):
            c0 = cbase + half * 512
            for m in range(2):
                pt = ps[2 + m]
                for kh in range(2):
                    P.dma("sync", wrow, wmodB[kh * 512:(kh + 1) * 512, c0:c0 + 512].rearrange("(k p) c -> p k c", p=128),
                          w=bigres[0:2])
                    for k4 in range(4):
                        kc = kh * 4 + k4
                        P.ts(crep[:, kc, :], ones32[:], csil[:, kc, m:m + 1], None, ALU.mult, r=[ones32, csil], w=[crep])
                        P.mm(pt[:, :], crep[:, kc, :], wrow[:, k4, :], start=(kc == 0), stop=(kc == 7),
                             r=[crep] + bigres[0:2], w=[pt])
                P.dma("sync", big[2][:, 0:512], bmodrow[:, c0:c0 + 512], w=[big[2]])
                P.tt(growb[m][:, half * 512:(half + 1) * 512], pt[:, :], big[2][:, 0:512], ALU.add,
                     r=[pt, big[2]], w=[growb[m]])
        P.dma("sync", lnr2[:], lnrow[:, 2 * k:2 * k + 2, :], w=[lnr2])
    make_gates(0)
    h2T = P.sb("h2T", [128, 8, TB], BF16)
    GT = P.sb("GT", [128, NTB, 65], F32)
    P.memset(GT[:], 1.0, w=[GT])
    acc = P.sb("acc", [128, NTB, D], F32)
    msub = P.mark()
    woutb = P.sb("woutb", [128, 8, D], BF16)
    for kc in range(8):
        P.dma("gpsimd", woutb[:, kc, :], wout_d[kc * 128:(kc + 1) * 128, :], w=[woutb])
    rw = P.sb("rw", [128, 8, 64], F32)
    P.dma("sync", rw[:], rw_d.rearrange("(k p) e -> p k e", p=128), w=[rw])
    rbias = P.sb("rbias", [128, 64], F32)
    P.dma("sync", rbias[:], rb_d[:, :], w=[rbias])

    yT = P.sb("yT", [128, 8, 128], BF16)
    h32 = P.sb("h32", [128, 8, 128], F32)
    st = P.sb("st", [128, 12], F32)
    mv = P.sb("mv", [128, 2], F32)
    rs = P.sb("rs", [128, 1], F32)
    sc = P.sb("sc", [128, 64], F32)
    bi = P.sb("bi", [128, 64], F32)
    m8 = P.sb("m8", [128, 8, 8], F32)
    gs = P.sb("gs", [128, 8], F32)
    gm = P.sb("gm", [128, 8], F32)
    t64 = P.sb("t64", [128, 64], F32)
    dsum = P.sb("dsum", [128, 1], F32)

    def layernorm(dst, src, srcres):
        P.call("vector", "bn_stats", st[:, 0:6], src[:, 0:512], r=srcres, w=[st])
        P.call("vector", "bn_stats", st[:, 6:12], src[:, 512:1024], r=srcres, w=[st])
        P.call("vector", "bn_aggr", mv[:], st[:], r=[st], w=[mv])
        P.act(rs[:], mv[:, 1:2], AF.Sqrt, r=[mv], w=[rs], bias=1e-6)
        P.call("vector", "reciprocal", rs[:], rs[:], r=[rs], w=[rs])
        P.ts(dst[:], src[:], mv[:, 0:1], rs[:, 0:1], ALU.subtract, ALU.mult, r=srcres + [mv, rs], w=[dst])

    for tt in range(NTB):
        m = 1 if tt == 0 else 0
        xr, ym, u, x1 = (big if tt % 2 == 0 else big2)
        xres_fn(tt, xr)
        ymx_fn(tt, ym, u)
        for half in range(2):
            pt = ps[half]
            for j4 in range(4):
                j = half * 4 + j4
                P.tr(pt[:, j4 * 128:(j4 + 1) * 128], ym[:, j * 128:(j + 1) * 128], ident[:], r=[ym, ident], w=[pt])
            P.cp(yT[:, half * 4:half * 4 + 4, :], pt[:, :].rearrange("p (j c) -> p j c", j=4), r=[pt], w=[yT])
        for half in range(2):
            pt = ps[2 + half]
            for kc in range(8):
                P.mm(pt[:, :], yT[:, kc, :], woutb[:, kc, half * 512:(half + 1) * 512], start=(kc == 0), stop=(kc == 7),
                     r=[yT, woutb], w=[pt])
            sl = slice(half * 512, (half + 1) * 512)
            P.tt(u[:, sl], pt[:, :], grow[m][0][:, sl], ALU.mult, r=[pt, grow[m][0]], w=[u])
            P.stt(u[:, sl], xr[:, sl], ALPHA, u[:, sl], ALU.mult, ALU.add, r=[xr, u], w=[u])
        layernorm(x1, u, [u])
        P.tt(x1[:], x1[:], lnr[:, 0, :], ALU.mult, r=[x1, lnr], w=[x1])
        P.tt(x1[:], x1[:], lnr[:, 1, :], ALU.add, r=[x1, lnr], w=[x1])
        P.dma("sync", x1d[tt * 128:(tt + 1) * 128, :], x1[:], r=[x1])
        layernorm(u, x1, [x1])
        for half in range(2):
            pt = ps[half]
            for j4 in range(4):
                j = half * 4 + j4
                P.tr(pt[:, j4 * 128:(j4 + 1) * 128], u[:, j * 128:(j + 1) * 128], ident[:], r=[u, ident], w=[pt])
            for j4 in range(4):
                j = half * 4 + j4
                src = pt[:, j4 * 128:(j4 + 1) * 128]
                if j4 > 0:
                    P.cp(h32[:, j, :], src, r=[pt], w=[h32])
                    src = h32[:, j, :]
                P.act(h32[:, j, :], src, AF.Identity, r=[pt, modc, h32], w=[h32],
                      scale=modc[:, 8 + j, m:m + 1], bias=modc[:, j, m:m + 1])
        P.cp(h2T[:, :, tt * 128:(tt + 1) * 128], h32[:], r=[h32], w=[h2T])
        pr = ps[4]
        for kc in range(8):
            P.mm(pr[:, 0:64], h32[:, kc, :], rw[:, kc, :], start=(kc == 0), stop=(kc == 7), r=[h32, rw], w=[pr])
        P.cp(sc[:], pr[:, 0:64], r=[pr], w=[sc])
        P.act(sc[:], sc[:], AF.Exp, r=[sc], w=[sc], scale=-1.0)
        P.ts(sc[:], sc[:], 1.0, None, ALU.add, r=[sc], w=[sc])
        P.call("vector", "reciprocal", sc[:], sc[:], r=[sc], w=[sc])
        P.tt(bi[:], sc[:], rbias[:], ALU.add, r=[sc, rbias], w=[bi])
        for g in range(8):
            P.call("vector", "max", m8[:, g, :], bi[:, g * 8:(g + 1) * 8], r=[bi], w=[m8])
        P.tt(gs[:], m8[:, :, 0], m8[:, :, 1], ALU.add, r=[m8], w=[gs])
        P.call("vector", "max", m8[:, 0, :], gs[:], r=[gs], w=[m8])
        P.ts(gm[:], gs[:], m8[:, 0, 3:4], None, ALU.is_ge, r=[gs, m8], w=[gm])
        b3 = bi[:].rearrange("p (g e) -> p g e", g=8)
        t3 = t64[:].rearrange("p (g e) -> p g e", g=8)
        P.tt(t3, b3, gm[:].unsqueeze(2).to_broadcast([128, 8, 8]), ALU.mult, r=[bi, gm], w=[t64])
        P.ts(gm[:], gm[:], 1.0, 1e9, ALU.subtract, ALU.mult, r=[gm], w=[gm])
        P.tt(t3, t3, gm[:].unsqueeze(2).to_broadcast([128, 8, 8]), ALU.add, r=[t64, gm], w=[t64])
        P.call("vector", "max", m8[:, 1, :], t64[:], r=[t64], w=[m8])
        P.ts(t64[:], t64[:], m8[:, 1, 7:8], None, ALU.is_ge, r=[t64, m8], w=[t64])
        P.tt(t64[:], t64[:], sc[:], ALU.mult, r=[t64, sc], w=[t64])
        P.call("vector", "tensor_reduce", dsum[:], t64[:], AX.X, ALU.add, r=[t64], w=[dsum])
        P.call("vector", "reciprocal", dsum[:], dsum[:], r=[dsum], w=[dsum])
        P.ts(GT[:, tt, 0:64], t64[:], dsum[:, 0:1], 2.5, ALU.mult, ALU.mult, r=[t64, dsum], w=[GT])

    P.release(msub)
    wg = [P.sb("wg%d" % i, [128, 8, 256], BF16) for i in range(2)]
    wu = [P.sb("wu%d" % i, [128, 8, 256], BF16) for i in range(2)]
    wd = [P.sb("wd%d" % i, [128, 2, D], BF16) for i in range(2)]
    sg = [P.sb("sg%d" % i, [128, 512], F32) for i in range(2)]
    aT = [P.sb("aT%d" % i, [128, 2, 512], BF16) for i in range(2)]
    ia = 0
    for e in range(65):
        g_, u_, d_ = wg[e % 2], wu[e % 2], wd[e % 2]
        P.dma("gpsimd", g_[:], eg_d[e].rearrange("(k p) f -> p k f", p=128), w=[g_])
        P.dma("gpsimd", u_[:], eu_d[e].rearrange("(k p) f -> p k f", p=128), w=[u_])
        P.dma("gpsimd", d_[:], ed_d[e].rearrange("(k p) f -> p k f", p=128), w=[d_])
        for (t0, w) in BBLOCKS:
            a_ = aT[ia % 2]
            ia += 1
            for fc in range(2):
                pg, pu = ps[fc * 2], ps[fc * 2 + 1]
                for kc in range(8):
                    P.mm(pg[:, 0:w], g_[:, kc, fc * 128:(fc + 1) * 128], h2T[:, kc, t0:t0 + w],
                         start=(kc == 0), stop=(kc == 7), r=[g_, h2T], w=[pg])
                for kc in range(8):
                    P.mm(pu[:, 0:w], u_[:, kc, fc * 128:(fc + 1) * 128], h2T[:, kc, t0:t0 + w],
                         start=(kc == 0), stop=(kc == 7), r=[u_, h2T], w=[pu])
                s_ = sg[fc]
                P.act(s_[:, 0:w], pg[:, 0:w], AF.Silu, r=[pg], w=[s_])
                P.tt(a_[:, fc, 0:w], pu[:, 0:w], s_[:, 0:w], ALU.mult, r=[pu, s_], w=[a_])
            for ti in range(w // 128):
                tt = t0 // 128 + ti
                for half in range(2):
                    pd = ps[4 + (ti * 2 + half) % 4]
                    for fc in range(2):
                        P.mm(pd[:, :], a_[:, fc, ti * 128:(ti + 1) * 128], d_[:, fc, half * 512:(half + 1) * 512],
                             start=(fc == 0), stop=(fc == 1), r=[a_, d_], w=[pd])
                    sl = slice(half * 512, (half + 1) * 512)
                    if e == 0:
                        P.ts(acc[:, tt, sl], pd[:, :], GT[:, tt, e:e + 1], None, ALU.mult, r=[pd, GT], w=[acc.sub(tt)])
                    else:
                        P.stt(acc[:, tt, sl], pd[:, :], GT[:, tt, e:e + 1], acc[:, tt, sl], ALU.mult, ALU.add,
                              r=[pd, GT, acc.sub(tt)], w=[acc.sub(tt)])

    make_gates(1)
    for tt in range(NTB):
        m = 1 if tt == 0 else 0
        x1, u, o_, _ = (big if tt % 2 == 0 else big2)
        P.dma("sync", x1[:], x1d[tt * 128:(tt + 1) * 128, :], w=[x1])
        P.tt(u[:], acc[:, tt, :], grow[m][1][:], ALU.mult, r=[acc.sub(tt), grow[m][1]], w=[u])
        P.stt(u[:], x1[:], ALPHA, u[:], ALU.mult, ALU.add, r=[x1, u], w=[u])
        layernorm(o_, u, [u])
        P.tt(o_[:], o_[:], lnr[:, 2, :], ALU.mult, r=[o_, lnr], w=[o_])
        P.tt(o_[:], o_[:], lnr[:, 3, :], ALU.add, r=[o_, lnr], w=[o_])
        oap, ores = out_fn(tt)
        P.dma("sync", oap, o_[:], r=[o_], w=ores)


def prep_B(inp, l, x_cur, xc_cur, ymix_full):
    maps = []
    ident = np.eye(128, dtype=np.float32)
    bm = inp["b_mod"][l][2048:6144]
    for core in range(8):
        b, hf = core // 2, core % 2
        rows_c = slice(128 * hf, 128 * hf + 128)
        rows_l = slice(2048 * hf, 2048 * hf + 2048)
        m = {}
        m["xres"] = np.ascontiguousarray(np.concatenate([xc_cur[b][rows_c], x_cur[b][rows_l]], 0))
        ym = ymix_full[b]
        m["ymx"] = np.ascontiguousarray(np.concatenate([ym[0:256][rows_c], ym[256:][rows_l]], 0))
        cc = np.stack([inp["c"][b], inp["c_ctx"]], -1)
        m["cT"] = np.ascontiguousarray(cc.reshape(8, 128, 2).transpose(1, 0, 2))
        m["wmodB"] = np.ascontiguousarray(inp["w_mod"][l][:, 2048:6144])
        m["bmodrow"] = np.ascontiguousarray(np.broadcast_to(bm[None], (128, 4096)))
        m["bmodcol"] = _col_layout(bm)
        m["wout"] = inp["w_out"][l]
        m["lnrow"] = np.ascontiguousarray(np.broadcast_to(
            np.stack([inp["ln1_w"][l], inp["ln1_b"][l], inp["ln2_w"][l], inp["ln2_b"][l]])[None], (128, 4, D)))
        m["rw"] = inp["router_w"][l]
        m["rbias"] = np.ascontiguousarray(np.broadcast_to(inp["router_bias"][l][None], (128, 64)))
        m["eg"] = np.concatenate([inp["exp_w_gate"][l], inp["sh_w_gate"][l][None]], 0)
        m["eu"] = np.concatenate([inp["exp_w_up"][l], inp["sh_w_up"][l][None]], 0)
        m["ed"] = np.concatenate([inp["exp_w_down"][l], inp["sh_w_down"][l][None]], 0)
        m["ident"] = ident
        maps.append(m)
    return maps


R1_OUT = ("WT0", "WT1", "NKK", "B0", "B1", "KD0", "KD1", "R", "V", "G", "BV")


def build_rwkv(nc, P, din, ps, hT, hres, winb, proj_fm, ident, identb, onesb, ymix, emit_ctx, dbg, xt, r1sink=None):
    mubc_d = din("mubc", [128, 768])
    pcol_d = din("pcol", [128, 8])
    wup_d = din("wup", [128, 128])
    aup_d = din("aup", [128, 128])
    gup_d = din("gup", [128, 128])
    bones_d = din("bones", [128, 128])
    outs = {} if r1sink is not None else {n: nc.dram_tensor("o_" + n, [128, T], F32, kind="ExternalOutput").ap() for n in R1_OUT}

    mubc = P.sb("mubc", [128, 768], F32)
    P.dma("sync", mubc[:], mubc_d[:, :], w=[mubc])
    cb = P.sb("cb", [128, 2, 768], F32)
    P.ts(cb[:, 0, :], mubc[:], 0.5, None, ALU.mult, r=[mubc], w=[cb])
    P.ts(cb[:, 1, :], mubc[:], -1.0, None, ALU.mult, r=[mubc], w=[cb])
    P.ts(cb[:, 1, :], cb[:, 1, :], 1.0, None, ALU.add, r=[cb], w=[cb])
    wc = P.sb("wcr", [128, 8, 3, 768], BF16)
    for kc in range(8):
        for j in range(3):
            P.tt(wc[:, kc, j, :], winb[:, kc, :], cb[:, j % 2, :], ALU.mult, r=[winb, cb], w=[wc])
    pcol = P.sb("pcol", [128, 8], F32)
    P.dma("sync", pcol[:], pcol_d[:, :], w=[pcol])
    npc = P.sb("npc", [128, 8], F32)
    P.ts(npc[:], pcol[:], -1.0, None, ALU.mult, r=[pcol], w=[npc])
    wup = P.sb("wup", [128, 128], F32)
    P.dma("sync", wup[:], wup_d[:, :], w=[wup])
    aup = P.sb("aup", [128, 128], F32)
    P.dma("sync", aup[:], aup_d[:, :], w=[aup])
    gup = P.sb("gup", [128, 128], F32)
    P.dma("sync", gup[:], gup_d[:, :], w=[gup])
    bones = P.sb("bones", [128, 128], F32)
    P.dma("sync", bones[:], bones_d[:, :], w=[bones])
    cm1 = P.sb("cm1", [128, 1], F32)
    P.memset(cm1[:], -1.0, w=[cm1])

    Zs = [[P.sb("Z%d" % g, [128, 512], F32) for g in range(6)] for k in range(2)]
    tAs = [[P.sb("tA%d" % i, [128, 512], F32) for i in range(2)] for k in range(2)]
    tKs = [[P.sb("tK%d" % i, [128, 512], F32) for i in range(2)] for k in range(2)]
    t0s = [P.sb("t0", [128, 512], F32) for k in range(2)]
    t1s = [P.sb("t1", [128, 512], F32) for k in range(2)]
    t2s = [P.sb("t2", [128, 512], F32) for k in range(2)]
    ob = [P.sb("ob%d" % i, [128, 512], F32) for i in range(3)]
    io = [0]

    def emit(name, src, t0, w, srcres):
        if r1sink is not None:
            r1sink(name, src, t0, w, srcres)
            return
        P.dma("sync", outs[name][:, t0:t0 + w], src, r=srcres)

    def sigmoid_from_psum(dst, pt, w, negbias):
        if negbias is None:
            P.act(dst[:, 0:w], pt[:, 0:w], AF.Exp, r=[pt], w=[dst], scale=-1.0)
        else:
            P.act(dst[:, 0:w], pt[:, 0:w], AF.Exp, r=[pt, npc], w=[dst], scale=-1.0, bias=negbias)
        P.ts(dst[:, 0:w], dst[:, 0:w], 1.0, None, ALU.add, r=[dst], w=[dst])
        P.call("vector", "reciprocal", dst[:, 0:w], dst[:, 0:w], r=[dst], w=[dst])

    for bi, (t0, w) in enumerate(QBLOCKS):
        Z, tA, tK = Zs[bi % 2], tAs[bi % 2], tKs[bi % 2]
        t0_, t1_, t2_ = t0s[bi % 2], t1s[bi % 2], t2s[bi % 2]
        seg0 = t0 in (0, NCTX)
        seg1 = (t0 + w) in (NCTX, T)
        for g in range(6):
            pt = ps[g % 4]
            first = True
            for j in (1, 0, 2):
                a = 1 if (j == 0 and seg0) else 0
                b = 1 if (j == 2 and seg1) else 0
                for kc in range(8):
                    last = (j == 2 and kc == 7)
                    P.mm(pt[:, a:w - b], wc[:, kc, j, g * 128:(g + 1) * 128],
                         hT[:, kc, t0 + a + j - 1:t0 + w - b + j - 1],
                         start=first, stop=last, r=[wc] + hres, w=[pt], skip=True)
                    first = False
            P.cp(Z[g][:, 0:w], pt[:, 0:w], r=[pt], w=[Z[g]])
        zr, zk, zv, zw, za, zg = Z
        emit("R", zr[:, 0:w], t0, w, [zr])
        emit("V", zv[:, 0:w], t0, w, [zv])
        P.act(zw[:, 0:w], zw[:, 0:w], AF.Tanh, r=[zw], w=[zw])
        for d in range(2):
            pt = ps[4 + d]
            rows = slice(d * 64, d * 64 + 64)
            P.mm(pt[:, 0:w], wup[rows, :], zw[rows, 0:w], r=[wup, zw], w=[pt])
            o_ = ob[io[0] % 3]; io[0] += 1
            sigmoid_from_psum(o_, pt, w, npc[:, d:d + 1])
            P.act(o_[:, 0:w], o_[:, 0:w], AF.Exp, r=[o_], w=[o_], scale=-float(np.exp(-0.5)))
            emit("WT%d" % d, o_[:, 0:w], t0, w, [o_])
        for d in range(2):
            pt = ps[6 + d]
            rows = slice(d * 64, d * 64 + 64)
            P.mm(pt[:, 0:w], aup[rows, :], za[rows, 0:w], r=[aup, za], w=[pt])
            sigmoid_from_psum(tA[d], pt, w, npc[:, 2 + d:3 + d])
        sigmoid_from_psum(t0_, zg, w, None) if False else None
        P.act(t0_[:, 0:w], zg[:, 0:w], AF.Exp, r=[zg], w=[t0_], scale=-1.0)
        P.ts(t0_[:, 0:w], t0_[:, 0:w], 1.0, None, ALU.add, r=[t0_], w=[t0_])
        P.call("vector", "reciprocal", t0_[:, 0:w], t0_[:, 0:w], r=[t0_], w=[t0_])
        pt = ps[4]
        P.mm(pt[:, 0:w], gup[:], t0_[:, 0:w], r=[gup, t0_], w=[pt])
        o_ = ob[io[0] % 3]; io[0] += 1
        P.cp(o_[:, 0:w], pt[:, 0:w], r=[pt], w=[o_])
        emit("G", o_[:, 0:w], t0, w, [o_])
        P.ts(t1_[:, 0:w], zk[:, 0:w], pcol[:, 4:5], None, ALU.mult, r=[zk, pcol], w=[t1_])
        P.tt(t2_[:, 0:w], t1_[:, 0:w], t1_[:, 0:w], ALU.mult, r=[t1_], w=[t2_])
        pt = ps[5]
        P.mm(pt[:, 0:w], bones[:], t2_[:, 0:w], r=[bones, t2_], w=[pt])
        P.ts(t2_[:, 0:w], pt[:, 0:w], 1e-12, None, ALU.max, r=[pt], w=[t2_])
        P.act(t2_[:, 0:w], t2_[:, 0:w], AF.Sqrt, r=[t2_], w=[t2_])
        P.call("vector", "reciprocal", t2_[:, 0:w], t2_[:, 0:w], r=[t2_], w=[t2_])
        P.tt(t1_[:, 0:w], t1_[:, 0:w], t2_[:, 0:w], ALU.mult, r=[t1_, t2_], w=[t1_])
        o_ = ob[io[0] % 3]; io[0] += 1
        P.ts(o_[:, 0:w], t1_[:, 0:w], -1.0, None, ALU.mult, r=[t1_], w=[o_])
        emit("NKK", o_[:, 0:w], t0, w, [o_])
        for d in range(2):
            o_ = ob[io[0] % 3]; io[0] += 1
            P.tt(o_[:, 0:w], t1_[:, 0:w], tA[d][:, 0:w], ALU.mult, r=[t1_, tA[d]], w=[o_])
            emit("B%d" % d, o_[:, 0:w], t0, w, [o_])
            P.ts(tK[d][:, 0:w], tA[d][:, 0:w], cm1[:, 0:1], pcol[:, 5:6], ALU.add, ALU.mult, r=[tA[d], cm1, pcol], w=[tK[d]])
            P.stt(tK[d][:, 0:w], tK[d][:, 0:w], 1.0, zk[:, 0:w], ALU.add, ALU.mult, r=[tK[d], zk], w=[tK[d]])
            emit("KD%d" % d, tK[d][:, 0:w], t0, w, [tK[d]])
        P.tt(t2_[:, 0:w], tK[0][:, 0:w], tK[1][:, 0:w], ALU.add, r=tK, w=[t2_])
        P.stt(t2_[:, 0:w], zr[:, 0:w], pcol[:, 6:7], t2_[:, 0:w], ALU.mult, ALU.mult, r=[zr, pcol, t2_], w=[t2_])
        pt = ps[6]
        P.mm(pt[:, 0:w], bones[:], t2_[:, 0:w], r=[bones, t2_], w=[pt])
        o_ = ob[io[0] % 3]; io[0] += 1
        P.tt(o_[:, 0:w], pt[:, 0:w], zv[:, 0:w], ALU.mult, r=[pt, zv], w=[o_])
        emit("BV", o_[:, 0:w], t0, w, [o_])


def build_R2():
    nc = bass.Bass("TRN2", target_bir_lowering=False)
    P = Prog(nc)

    def din(name, shape, dt=F32):
        return nc.dram_tensor(name, list(shape), dt, kind="ExternalInput").ap()
    I = []
    for d in range(2):
        I.append(dict(nk2=din("nk2_%d" % d, [T, 2, 128]), bt=din("bt_%d" % d, [T, 128]),
                      kd2=din("kd2_%d" % d, [T, 2, 128]), vt=din("vt_%d" % d, [T, 128]),
                      wt=din("wt_%d" % d, [128, T]), rbd=din("rbd_%d" % d, [128, T, 2])))
    TS = 64
    mask_d = din("mask32", [128, 32])
    O = [nc.dram_tensor("O_%d" % d, [64, T, 2], F32, kind="ExternalOutput").ap() for d in range(2)]

    mask32 = P.sb("mask32", [128, 32], F32)
    P.dma("sync", mask32[:], mask_d[:, :], w=[mask32])
    S = [P.sb("S%d" % d, [128, 64], F32) for d in range(2)]
    Sb = [[P.sb("Sb%d%d" % (d, k), [128, 64], BF16) for k in range(2)] for d in range(2)]
    for d in range(2):
        P.memset(S[d][:], 0.0, w=[S[d]])
        P.memset(Sb[d][1][:], 0.0, w=[Sb[d][1]])
    TB_ = [[dict(nk2f=P.sb("nk2f_%d%d" % (d, k), [TS, 2, 128], F32), bt=P.sb("bt_%d%d" % (d, k), [TS, 128], F32),
                 kd2f=P.sb("kd2f_%d%d" % (d, k), [TS, 2, 128], F32), vt=P.sb("vt_%d%d" % (d, k), [TS, 128], F32),
                 rbdf=P.sb("rbdf_%d%d" % (d, k), [128, TS, 2], F32),
                 nk2=P.sb("nk2_%d%d" % (d, k), [TS, 2, 128], BF16), kd2=P.sb("kd2_%d%d" % (d, k), [TS, 2, 128], BF16),
                 wt=P.sb("wt_%d%d" % (d, k), [128, TS], F32), rbd=P.sb("rbd_%d%d" % (d, k), [128, TS, 2], BF16),
                 Bd=P.sb("Bd_%d%d" % (d, k), [TS, 32, 128], BF16), Vd=P.sb("Vd_%d%d" % (d, k), [TS, 32, 128], BF16))
            for k in range(2)] for d in range(2)]
    LS = [[P.sb("LS%d%d" % (d, k), [128, 128], BF16) for k in range(4)] for d in range(2)]
    osb = [P.sb("osb%d" % d, [64, 2 * TS], F32) for d in range(2)]
    LP = [[P.ps("LP%d%d" % (d, k), [128, 512]) for k in range(2)] for d in range(2)]
    SP = [P.ps("SP%d" % d, [128, 512]) for d in range(2)]
    OP = [P.ps("OP%d" % d, [128, 512]) for d in range(2)]

    def load(i):
        for d in range(2):
            tb = TB_[d][i % 2]
            r0 = i * TS
            P.dma("sync", tb["nk2f"][:], I[d]["nk2"][r0:r0 + TS, :, :], w=[tb["nk2f"]])
            P.dma("sync", tb["bt"][:], I[d]["bt"][r0:r0 + TS, :], w=[tb["bt"]])
            P.dma("sync", tb["kd2f"][:], I[d]["kd2"][r0:r0 + TS, :, :], w=[tb["kd2f"]])
            P.dma("sync", tb["vt"][:], I[d]["vt"][r0:r0 + TS, :], w=[tb["vt"]])
            P.dma("sync", tb["wt"][:], I[d]["wt"][:, r0:r0 + TS], w=[tb["wt"]])
            P.dma("sync", tb["rbdf"][:], I[d]["rbd"][:, r0:r0 + TS, :], w=[tb["rbdf"]])
            P.cp(tb["nk2"][:], tb["nk2f"][:], r=[tb["nk2f"]], w=[tb["nk2"]], eng="gpsimd")
            P.cp(tb["kd2"][:], tb["kd2f"][:], r=[tb["kd2f"]], w=[tb["kd2"]], eng="gpsimd")
            P.cp(tb["rbd"][:], tb["rbdf"][:], r=[tb["rbdf"]], w=[tb["rbd"]], eng="gpsimd")
            mb = mask32[0:TS, :].unsqueeze(2).to_broadcast([TS, 32, 128])
            P.tt(tb["Bd"][:], mb, tb["bt"][:].unsqueeze(1).to_broadcast([TS, 32, 128]), ALU.mult,
                 r=[mask32, tb["bt"]], w=[tb["Bd"]])
            P.tt(tb["Vd"][:], mb, tb["vt"][:].unsqueeze(1).to_broadcast([TS, 32, 128]), ALU.mult,
                 r=[mask32, tb["vt"]], w=[tb["Vd"]])

    def lgen(g):
        i, t = g // TS, g % TS
        rows = slice((t // 32) * 32, (t // 32) * 32 + 32)
        tl = t % 32
        for d in range(2):
            tb = TB_[d][i % 2]
            lp = LP[d][g % 2]
            ls = LS[d][g % 4]
            P.mm(lp[:, 0:64], tb["nk2"][rows, 0, :], tb["Bd"][rows, tl, 0:64], r=[tb["nk2"], tb["Bd"]], w=[lp])
            P.mm(lp[:, 64:128], tb["nk2"][rows, 1, :], tb["Bd"][rows, tl, 64:128], r=[tb["nk2"], tb["Bd"]], w=[lp])
            P.cp(ls[:], lp[:, 0:128], r=[lp], w=[ls], eng="scalar")

    def step(g):
        i, t = g // TS, g % TS
        rows = slice((t // 32) * 32, (t // 32) * 32 + 32)
        tl = t % 32
        for d in range(2):
            tb = TB_[d][i % 2]
            ls = LS[d][g % 4]
            sp = SP[d]
            sbo = Sb[d][(g + 1) % 2]
            P.mm(sp[:, 0:64], ls[:], sbo[:], start=True, stop=False, r=[ls, sbo], w=[sp])
            P.mm(sp[:, 0:64], tb["kd2"][rows, 0, :], tb["Vd"][rows, tl, 0:64], start=False, stop=False,
                 r=[tb["kd2"], tb["Vd"]], w=[sp])
            P.mm(sp[:, 0:64], tb["kd2"][rows, 1, :], tb["Vd"][rows, tl, 64:128], start=False, stop=True,
                 r=[tb["kd2"], tb["Vd"]], w=[sp])
        for d in range(2):
            tb = TB_[d][i % 2]
            sbn = Sb[d][g % 2]
            P.stt(sbn[:], S[d][:], tb["wt"][:, t:t + 1], SP[d][:, 0:64], ALU.mult, ALU.add,
                  r=[S[d], tb["wt"], SP[d]], w=[sbn])
        for d in range(2):
            tb = TB_[d][i % 2]
            P.mm(OP[d][0:64, 2 * t:2 * t + 2], Sb[d][g % 2][:], tb["rbd"][:, t, :], r=[Sb[d][g % 2], tb["rbd"]], w=[OP[d]])
        for d in range(2):
            tb = TB_[d][i % 2]
            P.stt(S[d][:], S[d][:], tb["wt"][:, t:t + 1], SP[d][:, 0:64], ALU.mult, ALU.add,
                  r=[S[d], tb["wt"], SP[d]], w=[S[d]])

    LA = 2
    load(0)
    for g in range(LA):
        lgen(g)
    for g in range(T):
        if g % TS == 0 and g // TS + 1 < T // TS:
            load(g // TS + 1)
        if g + LA < T:
            lgen(g + LA)
        step(g)
        if g % TS == TS - 1:
            i = g // TS
            for d in range(2):
                P.cp(osb[d][:], OP[d][0:64, 0:2 * TS], r=[OP[d]], w=[osb[d]])
                P.dma("sync", O[d][:, i * TS:(i + 1) * TS, :], osb[d][:].rearrange("p (t h) -> p t h", h=2), r=[osb[d]])
    P.emit()
    return nc


def build_R3():
    nc = bass.Bass("TRN2", target_bir_lowering=False)
    P = Prog(nc)

    def din(name, shape, dt=F32):
        return nc.dram_tensor(name, list(shape), dt, kind="ExternalInput").ap()
    of_d = din("of", [T, 128])
    ob_d = din("ob", [T, 128])
    g_d = din("g", [T, 128])
    bv_d = din("bv", [T, 128])
    ln_d = din("lnrow", [128, 2, 128])
    y_d = nc.dram_tensor("ymix", [T, 128], F32, kind="ExternalOutput").ap()
    ln = P.sb("ln", [128, 2, 128], F32)
    P.dma("sync", ln[:], ln_d[:, :, :], w=[ln])
    bufs = [[P.sb("r3_%d%d" % (k, i), [128, 128], F32) for i in range(4)] for k in range(2)]
    st = P.sb("st", [128, 2, 6], F32)
    mv = P.sb("mv", [128, 2, 2], F32)
    for tt in range(NT):
        of, ob, g, bv = bufs[tt % 2]
        r0 = tt * 128
        P.dma("sync", of[:], of_d[r0:r0 + 128, :], w=[of])
        P.dma("sync", ob[:], ob_d[r0:r0 + 128, :], w=[ob])
        P.dma("sync", g[:], g_d[r0:r0 + 128, :], w=[g])
        P.dma("sync", bv[:], bv_d[r0:r0 + 128, :], w=[bv])
        P.tt(of[:], of[:], ob[:], ALU.add, r=[of, ob], w=[of])
        for h in range(2):
            P.call("vector", "bn_stats", st[:, h, :], of[:, h * 64:(h + 1) * 64], r=[of], w=[st])
            P.call("vector", "bn_aggr", mv[:, h, :], st[:, h, :], r=[st], w=[mv])
        P.act(mv[:, :, 1:2], mv[:, :, 1:2], AF.Sqrt, r=[mv], w=[mv], bias=64e-5)
        P.call("vector", "reciprocal", mv[:, :, 1:2], mv[:, :, 1:2], r=[mv], w=[mv])
        for h in range(2):
            sl = slice(h * 64, (h + 1) * 64)
            P.ts(of[:, sl], of[:, sl], mv[:, h, 0:1], mv[:, h, 1:2], ALU.subtract, ALU.mult, r=[of, mv], w=[of])
        P.tt(of[:], of[:], ln[:, 0, :], ALU.mult, r=[of, ln], w=[of])
        P.tt(of[:], of[:], ln[:, 1, :], ALU.add, r=[of, ln], w=[of])
        P.tt(of[:], of[:], bv[:], ALU.add, r=[of, bv], w=[of])
        P.tt(of[:], of[:], g[:], ALU.mult, r=[of, g], w=[of])
        P.dma("sync", y_d[r0:r0 + 128, :], of[:], r=[of])
    P.emit()
    return nc


_PERM_B = np.concatenate([np.arange(255, -1, -1), np.arange(T - 1, 255, -1)])


def prep_R2(r1res):
    maps = []
    mask32 = (np.arange(128)[:, None] % 32 == np.arange(32)[None, :]).astype(np.float32)
    for core in range(8):
        o = r1res[core]
        m = {"mask32": mask32}
        for d in range(2):
            perm = np.arange(T) if d == 0 else _PERM_B

            def tm(a):
                return np.ascontiguousarray(a.T[perm])

            def mask2(a_tm):
                out = np.zeros((T, 2, 128), np.float32)
                out[:, 0, 0:64] = a_tm[:, 0:64]
                out[:, 1, 64:128] = a_tm[:, 64:128]
                return out
            m["nk2_%d" % d] = mask2(tm(o["o_NKK"]))
            m["bt_%d" % d] = tm(o["o_B%d" % d])
            m["kd2_%d" % d] = mask2(tm(o["o_KD%d" % d]))
            m["vt_%d" % d] = tm(o["o_V"])
            m["wt_%d" % d] = np.ascontiguousarray(o["o_WT%d" % d][:, perm])
            r = o["o_R"][:, perm]
            rbd = np.zeros((128, T, 2), np.float32)
            rbd[0:64, :, 0] = r[0:64]
            rbd[64:128, :, 1] = r[64:128]
            m["rbd_%d" % d] = rbd
        maps.append(m)
    return maps


def prep_R3(inp, l, r1res, r2res):
    maps = []
    inv = np.argsort(_PERM_B)
    for core in range(8):
        hp = core % 2
        hc = slice(128 * hp, 128 * hp + 128)
        o0 = r2res[core]["O_0"]
        o1 = r2res[core]["O_1"][:, inv, :]
        m = {}
        m["of"] = np.ascontiguousarray(o0.transpose(1, 2, 0).reshape(T, 128))
        m["ob"] = np.ascontiguousarray(o1.transpose(1, 2, 0).reshape(T, 128))
        m["g"] = np.ascontiguousarray(r1res[core]["o_G"].T)
        m["bv"] = np.ascontiguousarray(r1res[core]["o_BV"].T)
        m["lnrow"] = np.ascontiguousarray(np.broadcast_to(
            np.stack([inp["rwkv_ln_w"][l][hc], inp["rwkv_ln_b"][l][hc]])[None], (128, 2, 128)))
        maps.append(m)
    return maps


def run_rwkv(inp, l, x, xc, emit_ctx=True):
    cores = list(range(8))
    r1 = run_bass_kernel_spmd(build_A("rwkv", emit_ctx), prep_A(inp, l, x, xc, "rwkv"), core_ids=cores).results
    r2 = run_bass_kernel_spmd(build_R2(), prep_R2(r1), core_ids=cores).results
    r3 = run_bass_kernel_spmd(build_R3(), prep_R3(inp, l, r1, r2), core_ids=cores).results
    return [r3[c]["ymix"] for c in cores]


def _kernel_unfused(inp, nlayers=2):
    inp = {k: np.asarray(v) for k, v in inp.items()}
    x = inp["x"].astype(np.float32)
    xc = inp["ctx"].astype(np.float32)
    cores = list(range(8))
    for l in range(nlayers):
        emit_ctx = (l == 0)
        ymix = np.zeros((4, T, 1024), np.float32)
        for kind, c0, W in (("mlstm", 0, 128), ("mla", 512, 256)):
            nc = build_A(kind, emit_ctx)
            res = run_bass_kernel_spmd(nc, prep_A(inp, l, x, xc, kind), core_ids=cores)
            for core in cores:
                b, hp = core // 2, core % 2
                ymix[b][:, c0 + hp * W:c0 + (hp + 1) * W] = res.results[core]["ymix"]
        yr = run_rwkv(inp, l, x, xc, emit_ctx)
        for core in cores:
            b, hp = core // 2, core % 2
            ymix[b][:, 256 + hp * 128:256 + (hp + 1) * 128] = yr[core]
        resB = run_bass_kernel_spmd(build_B(), prep_B(inp, l, x, xc, ymix), core_ids=cores)
        x_new = np.empty_like(x)
        xc_new = np.empty_like(xc)
        for core in cores:
            b, hf = core // 2, core % 2
            o = resB.results[core]["xout"]
            xc_new[b][128 * hf:128 * hf + 128] = o[0:128]
            x_new[b][2048 * hf:2048 * hf + 2048] = o[128:]
        x, xc = x_new, xc_new
    return x.astype(np.float32)


GROUPS = [[0, 1], [2, 3], [4, 5], [6, 7]]
DIAG_ENG = "gpsimd"
TM_NAMES = ("NKK", "B0", "B1", "KD0", "KD1", "V", "G", "BV")


def _half_rows(tt):
    if tt < 2:
        return tt, 0
    k = tt - 2
    return k // 16, 128 + (k % 16) * 128


def fused_rwkv_scan(nc, P, ps, ident, scr, sres, ymix, lnrow_d, emit_ctx):
    TS = 32
    m0 = P.mark()
    mask32 = P.sb("mask32", [64, 32], F32)
    for q in range(2):
        P.cp(mask32[q * 32:(q + 1) * 32, :], ident[q * 32:(q + 1) * 32, q * 32:(q + 1) * 32], r=[ident], w=[mask32])
    S = [P.sb("S%d" % d, [128, 64], F32) for d in range(2)]
    Sb = [[P.sb("Sb%d%d" % (d, k), [128, 64], BF16) for k in range(2)] for d in range(2)]
    for d in range(2):
        P.memset(S[d][:], 0.0, w=[S[d]])
        P.memset(Sb[d][1][:], 0.0, w=[Sb[d][1]])
    TB_ = [[dict(nkf=P.sb("nkf", [64, 128], F32), kdf=P.sb("kdf", [64, 128], F32),
                 btf=P.sb("btf", [64, 128], F32), vtf=P.sb("vtf", [64, 64], F32),
                 rf=P.sb("rf", [128, TS], F32), wt=P.sb("wt", [128, TS], F32),
                 nk=P.sb("nk", [64, 128], BF16), kd=P.sb("kd", [64, 128], BF16),
                 rbd=P.sb("rbd", [128, TS, 2], BF16),
                 Bd=P.sb("Bd", [64, 32, 128], BF16), Vd=P.sb("Vd", [64, 32, 64], BF16))
            for k in range(2)] for d in range(2)]
    for d in range(2):
        for k in range(2):
            tb = TB_[d][k]
            for n in ("nkf", "kdf", "btf"):
                P.memset(tb[n][:], 0.0, w=[tb[n]])
            P.memset(tb["rbd"][:], 0.0, w=[tb["rbd"]])
    LS = [[P.sb("LS", [128, 512], BF16) for k in range(3)] for d in range(2)]
    osb = [P.sb("osb", [64, 2 * TS], F32) for d in range(2)]
    LP = [[ps[0], ps[1]], [ps[2], ps[3]]]
    SP = [ps[4], ps[5]]
    OP = [ps[6], ps[7]]
    perm_b = _PERM_B

    def lo_of(d, i):
        return i * TS if d == 0 else int(perm_b[i * TS + TS - 1])

    def loc(d, t):
        return t if d == 0 else TS - 1 - t

    def load(i):
        for d in range(2):
            tb = TB_[d][i % 2]
            lo = lo_of(d, i)
            for (dst, name) in (("nkf", "NKK"), ("kdf", "KD%d" % d), ("btf", "B%d" % d)):
                for h in range(2):
                    P.dma("sync", tb[dst][h * 32:(h + 1) * 32, h * 64:(h + 1) * 64],
                          scr[name][lo:lo + TS, h * 64:(h + 1) * 64], r=[sres[name]], w=[tb[dst]])
            for h in range(2):
                P.dma("sync", tb["vtf"][h * 32:(h + 1) * 32, :], scr["V"][lo:lo + TS, h * 64:(h + 1) * 64],
                      r=[sres["V"]], w=[tb["vtf"]])
            P.dma("sync", tb["wt"][:], scr["WT%d" % d][:, lo:lo + TS], r=[sres["WT%d" % d]], w=[tb["wt"]])
            P.dma("sync", tb["rf"][:], scr["R"][:, lo:lo + TS], r=[sres["R"]], w=[tb["rf"]])
            P.cp(tb["nk"][:], tb["nkf"][:], r=[tb["nkf"]], w=[tb["nk"]], eng="gpsimd")
            P.cp(tb["kd"][:], tb["kdf"][:], r=[tb["kdf"]], w=[tb["kd"]], eng="gpsimd")
            P.cp(tb["rbd"][0:64, :, 0], tb["rf"][0:64, :], r=[tb["rf"]], w=[tb["rbd"]], eng="gpsimd")
            P.cp(tb["rbd"][64:128, :, 1], tb["rf"][64:128, :], r=[tb["rf"]], w=[tb["rbd"]], eng="gpsimd")
            P.tt(tb["Bd"][:], mask32[:].unsqueeze(2).to_broadcast([64, 32, 128]),
                 tb["btf"][:].unsqueeze(1).to_broadcast([64, 32, 128]), ALU.mult,
                 r=[mask32, tb["btf"]], w=[tb["Bd"]], eng=DIAG_ENG)
            P.tt(tb["Vd"][:], mask32[:].unsqueeze(2).to_broadcast([64, 32, 64]),
                 tb["vtf"][:].unsqueeze(1).to_broadcast([64, 32, 64]), ALU.mult,
                 r=[mask32, tb["vtf"]], w=[tb["Vd"]], eng=DIAG_ENG)

    def lgen(q):
        g0 = 4 * q
        i, t0 = g0 // TS, g0 % TS
        for d in range(2):
            b0 = t0 if d == 0 else TS - 4 - t0
            tb = TB_[d][i % 2]
            lp = LP[d][q % 2]
            ls = LS[d][q % 3]
            P.mm(lp[:, 0:512], tb["nk"][:], tb["Bd"][:, b0:b0 + 4, :], r=[tb["nk"], tb["Bd"]], w=[lp])
            P.cp(ls[:], lp[:, 0:512], r=[lp], w=[ls], eng="scalar")

    def mm_step(g):
        i, t = g // TS, g % TS
        for d in range(2):
            tl = loc(d, t)
            tb = TB_[d][i % 2]
            P.mm(SP[d][:, 0:64], tb["kd"][:], tb["Vd"][:, tl, :], start=True, stop=False,
                 r=[tb["kd"], tb["Vd"]], w=[SP[d]])
        for d in range(2):
            ls = LS[d][(g // 4) % 3]
            j = (g % 4) if d == 0 else 3 - (g % 4)
            sbo = Sb[d][(g + 1) % 2]
            P.mm(SP[d][:, 0:64], ls[:, j * 128:(j + 1) * 128], sbo[:], start=False, stop=True, r=[ls, sbo], w=[SP[d]])

    def out_step(g):
        i, t = g // TS, g % TS
        for d in range(2):
            tb = TB_[d][i % 2]
            tl = loc(d, t)
            P.mm(OP[d][0:64, 2 * tl:2 * tl + 2], Sb[d][g % 2][:], tb["rbd"][:, tl, :],
                 r=[Sb[d][g % 2], tb["rbd"]], w=[OP[d]])

    def dve_step(g):
        i, t = g // TS, g % TS
        for d in range(2):
            tb = TB_[d][i % 2]
            tl = loc(d, t)
            P.stt(Sb[d][g % 2][:], S[d][:], tb["wt"][:, tl:tl + 1], SP[d][:, 0:64], ALU.mult, ALU.add,
                  r=[S[d], tb["wt"], SP[d]], w=[Sb[d][g % 2]])
        for d in range(2):
            tb = TB_[d][i % 2]
            tl = loc(d, t)
            P.stt(S[d][:], S[d][:], tb["wt"][:, tl:tl + 1], SP[d][:, 0:64], ALU.mult, ALU.add,
                  r=[S[d], tb["wt"], SP[d]], w=[S[d]])

    def evac(i):
        for d in range(2):
            lo = lo_of(d, i)
            P.cp(osb[d][:], OP[d][0:64, 0:2 * TS], r=[OP[d]], w=[osb[d]])
            P.dma("sync", scr["O%d" % d][:, lo:lo + TS, :], osb[d][:].rearrange("p (t h) -> p t h", h=2),
                  r=[osb[d]], w=[sres["O%d" % d]])

    load(0)
    lgen(0)
    for g in range(T):
        flushed = False
        if g % TS == 0:
            if g > 0:
                out_step(g - 1)
                evac(g // TS - 1)
                flushed = True
            if g // TS + 1 < T // TS:
                load(g // TS + 1)
        if g % 4 == 0 and g + 4 < T:
            lgen(g // 4 + 1)
        mm_step(g)
        if g > 0 and not flushed:
            out_step(g - 1)
        dve_step(g)
    out_step(T - 1)
    evac(T // TS - 1)
    P.release(m0)
    ln = P.sb("ln", [128, 2, 128], F32)
    P.dma("sync", ln[:], lnrow_d[:, :, :], w=[ln])
    o3 = [[P.sb("o3", [64, 128, 2], F32) for i in range(2)] for k in range(2)]
    gb = [[P.sb("gb", [128, 128], F32) for i in range(2)] for k in range(2)]
    ot = [P.sb("ot", [128, 128], F32) for k in range(2)]
    st = P.sb("st", [128, 2, 6], F32)
    mv = P.sb("mv", [128, 2, 2], F32)
    for tt in range(NT):
        if tt < 2 and not emit_ctx:
            continue
        r0 = tt * 128
        of, ob = o3[tt % 2]
        g_, bv = gb[tt % 2]
        o_ = ot[tt % 2]
        P.dma("sync", of[:], scr["O0"][:, r0:r0 + 128, :], r=[sres["O0"]], w=[of])
        P.dma("sync", ob[:], scr["O1"][:, r0:r0 + 128, :], r=[sres["O1"]], w=[ob])
        P.dma("sync", g_[:], scr["G"][r0:r0 + 128, :], r=[sres["G"]], w=[g_])
        P.dma("sync", bv[:], scr["BV"][r0:r0 + 128, :], r=[sres["BV"]], w=[bv])
        P.tt(of[:], of[:], ob[:], ALU.add, r=[of, ob], w=[of])
        pt = ps[tt % 2]
        for h in range(2):
            P.tr(pt[:, h * 64:(h + 1) * 64], of[:, :, h], ident[0:64, 0:64], r=[of, ident], w=[pt])
        P.cp(o_[:], pt[:, 0:128], r=[pt], w=[o_])
        for h in range(2):
            P.call("vector", "bn_stats", st[:, h, :], o_[:, h * 64:(h + 1) * 64], r=[o_], w=[st])
            P.call("vector", "bn_aggr", mv[:, h, :], st[:, h, :], r=[st], w=[mv])
        P.act(mv[:, :, 1:2], mv[:, :, 1:2], AF.Sqrt, r=[mv], w=[mv], bias=64e-5)
        P.call("vector", "reciprocal", mv[:, :, 1:2], mv[:, :, 1:2], r=[mv], w=[mv])
        for h in range(2):
            sl = slice(h * 64, (h + 1) * 64)
            P.ts(o_[:, sl], o_[:, sl], mv[:, h, 0:1], mv[:, h, 1:2], ALU.subtract, ALU.mult, r=[o_, mv], w=[o_])
        P.tt(o_[:], o_[:], ln[:, 0, :], ALU.mult, r=[o_, ln], w=[o_])
        P.tt(o_[:], o_[:], ln[:, 1, :], ALU.add, r=[o_, ln], w=[o_])
        P.tt(o_[:], o_[:], bv[:], ALU.add, r=[o_, bv], w=[o_])
        P.tt(o_[:], o_[:], g_[:], ALU.mult, r=[o_, g_], w=[o_])
        P.dma("sync", ymix[r0:r0 + 128, :], o_[:], r=[o_])
    P.release(m0)


class _Din:
    def __init__(self, nc, pre, shared):
        self.nc, self.pre, self._shared = nc, pre, shared

    def __call__(self, name, shape, dt=F32):
        return self.nc.dram_tensor(self.pre + name, list(shape), dt, kind="ExternalInput").ap()

    def shared(self, name, shape, dt=F32):
        if name not in self._shared:
            self._shared[name] = self.nc.dram_tensor(name, list(shape), dt, kind="ExternalInput").ap()
        return self._shared[name]


def build_fused(nlayers=2):
    nc = bass.Bass("TRN2", target_bir_lowering=False)
    P = Prog(nc)
    P.scoped = True
    shared = {}
    d0 = _Din(nc, "", shared)
    xin0 = d0.shared("xin", [T, D])
    xres0 = d0.shared("xres0", [TB, D])
    sel_d = d0.shared("sel", [128, 2])
    ident_d = d0.shared("ident", [128, 128])
    out = nc.dram_tensor("xout", [TB, D], F32, kind="ExternalOutput").ap()

    ident = P.sb("ident", [128, 128], F32)
    P.dma("sync", ident[:], ident_d[:, :], w=[ident])
    identb = P.sb("identb", [128, 128], BF16)
    P.cp(identb[:], ident[:], r=[ident], w=[identb])
    onesb = P.sb("onesb", [128, 128], BF16)
    P.memset(onesb[:], 1.0, w=[onesb])
    ones32 = P.sb("ones32", [128, 128], F32)
    P.memset(ones32[:], 1.0, w=[ones32])
    sel = P.sb("sel", [128, 2], F32)
    P.dma("sync", sel[:], sel_d[:, :], w=[sel])
    ps = [P.ps("ps%d" % i, [128, 512]) for i in range(8)]

    XG = None
    XHp = None
    xg_res = Res()
    for l in range(nlayers):
        pre = "L%d_" % l
        last = (l == nlayers - 1)
        emit_ctx = not last
        din = _Din(nc, pre, shared)
        YH = nc.dram_tensor(pre + "YH", [T, 512], F32).ap()
        ych = [(0, 1024), (1024, 1024), (2048, 1024), (3072, 1024), (4096, 256)]
        YG = [nc.dram_tensor(pre + "YG%d" % k, [2 * n_, 512], F32).ap() for k, (s_, n_) in enumerate(ych)]
        yg_res = Res()
        mL = P.mark()
        if l == 0:
            def xrow_fn(tt):
                return xin0[tt * 128:(tt + 1) * 128, :], []
        else:
            def xrow_fn(tt, XG=XG):
                j, lo = _half_rows(tt)
                k, off = lo // 512, lo % 512
                n_ = 512 if k < 4 else 128
                return XG[k][j * n_ + off:j * n_ + off + 128, :], [xg_res]
        cT = din.shared("cT", [128, 8, 2])
        wmodA = din("wmodA", [D, 2048])
        bmodA = din("bmodA", [128, 16])
        hT, hres, xt = phase_ln(nc, P, ps, ident, xrow_fn, cT, wmodA, bmodA, None)
        for kind, c0 in (("mlstm", 0), ("rwkv", 128), ("mla", 256)):
            mk = P.mark()
            winA = din("winA_" + kind, [D, NCA[kind]])
            winb = P.sb("winb", [128, 8, NCA[kind]], BF16)
            for kc in range(8):
                P.dma("gpsimd", winb[:, kc, :], winA[kc * 128:(kc + 1) * 128, :], w=[winb])
            proj_fm = make_proj_fm(P, hT, hres, winb)
            ymix = YH[:, c0:c0 + YW[kind]]
            env = dict(nc=nc, P=P, din=din, ps=ps, hT=hT, hres=hres, winb=winb, proj_fm=proj_fm, ident=ident,
                       identb=identb, onesb=onesb, ymix=ymix, emit_ctx=emit_ctx, dbg=(), xt=xt)
            if kind == "mlstm":
                build_mlstm(**env)
            elif kind == "mla":
                build_mla(**env)
            else:
                scr = {}
                sres = {}
                for n in R1_OUT:
                    shp = [T, 128] if n in TM_NAMES else [128, T]
                    scr[n] = nc.dram_tensor(pre + "S_" + n, shp, F32).ap()
                    sres[n] = Res()
                for n in ("O0", "O1"):
                    scr[n] = nc.dram_tensor(pre + "S_" + n, [64, T, 2], F32).ap()
                    sres[n] = Res()
                tmos = [P.sb("tmo", [128, 512], F32) for k in range(2)]
                tmc = [0]

                def r1sink(name, src, t0, w, srcres, scr=scr, sres=sres, tmos=tmos, tmc=tmc):
                    tmo = tmos[tmc[0] % 2]
                    tmc[0] += 1
                    if name not in TM_NAMES:
                        P.dma("sync", scr[name][:, t0:t0 + w], src, r=srcres)
                        return
                    pt = ps[7]
                    for ci in range(w // 128):
                        P.tr(pt[:, ci * 128:(ci + 1) * 128], src[:, ci * 128:(ci + 1) * 128], ident[:],
                             r=list(srcres) + [ident], w=[pt])
                    P.cp(tmo[:, 0:w], pt[:, 0:w], r=[pt], w=[tmo])
                    for ci in range(w // 128):
                        P.dma("sync", scr[name][t0 + ci * 128:t0 + (ci + 1) * 128, :], tmo[:, ci * 128:(ci + 1) * 128],
                              r=[tmo])
                build_rwkv(r1sink=r1sink, **env)
                P.release(mk)
                rln = din("rlnrow", [128, 2, 128])
                fused_rwkv_scan(nc, P, ps, ident, scr, sres, ymix, rln, emit_ctx)
            P.release(mk)
        P.release(mL)
        for k, (s_, n_) in enumerate(ych):
            P.coll("AllGather", YH[s_:s_ + n_, :], YG[k][:, :], GROUPS, w=[yg_res])
        P.barrier()
        XH = out if last else nc.dram_tensor(pre + "XH", [TB, D], F32).ap()

        def xres_fn(tt, buf, l=l, XHp=XHp):
            srcx = xres0 if l == 0 else XHp
            P.dma("sync", buf[:], srcx[tt * 128:(tt + 1) * 128, :], w=[buf])

        def ymx_fn(tt, ym, tmp, YG=YG, yg_res=yg_res):
            ra = 0 if tt == 0 else 256 + (tt - 1) * 128
            rb = 128 if tt == 0 else 256 + 2048 + (tt - 1) * 128
            for r in range(2):
                for (rr_, buf_) in ((ra, ym), (rb, tmp)):
                    k, off = rr_ // 1024, rr_ % 1024
                    n_ = 1024 if k < 4 else 256
                    P.dma("sync", buf_[:, r * 512:(r + 1) * 512], YG[k][r * n_ + off:r * n_ + off + 128, :],
                          r=[yg_res], w=[buf_])
            P.ts(ym[:], ym[:], sel[:, 0:1], None, ALU.mult, r=[ym, sel], w=[ym])
            P.stt(ym[:], tmp[:], sel[:, 1:2], ym[:], ALU.mult, ALU.add, r=[tmp, sel, ym], w=[ym])

        def out_fn(tt, XH=XH):
            return XH[tt * 128:(tt + 1) * 128, :], []
        phase_B(nc, P, ps, ident, ones32, din, xres_fn, ymx_fn, out_fn, pre=pre)
        P.release(mL)
        if not last:
            xch = [(0, 512), (512, 512), (1024, 512), (1536, 512), (2048, 128)]
            XG = [nc.dram_tensor(pre + "XG%d" % k, [2 * n_, D], F32).ap() for k, (s_, n_) in enumerate(xch)]
            P.barrier()
            for k, (s_, n_) in enumerate(xch):
                P.coll("AllGather", XH[s_:s_ + n_, :], XG[k][:, :], GROUPS, w=[xg_res])
            P.barrier()
            XHp = XH
    P.emit()
    return nc


_WOUT_PERM = np.concatenate([np.arange(0, 128), np.arange(256, 384), np.arange(512, 768),
                             np.arange(128, 256), np.arange(384, 512), np.arange(768, 1024)])


def prep_fused(inp, nlayers=2):
    x = inp["x"]
    xc = inp["ctx"]
    maps = [dict() for _ in range(8)]
    ident = np.eye(128, dtype=np.float32)
    for core in range(8):
        b, hp = core // 2, core % 2
        m = maps[core]
        m["xin"] = np.ascontiguousarray(np.concatenate([xc[b], x[b]], 0))
        m["xres0"] = np.ascontiguousarray(np.concatenate([xc[b][128 * hp:128 * hp + 128], x[b][2048 * hp:2048 * hp + 2048]], 0))
        sv = np.zeros((128, 2), np.float32)
        sv[:, hp] = 1.0
        m["sel"] = sv
        m["ident"] = ident
    for l in range(nlayers):
        pre = "L%d_" % l
        for kind in ("mlstm", "rwkv", "mla"):
            pa = prep_A(inp, l, x, xc, kind)
            for core in range(8):
                m = maps[core]
                for k, v in pa[core].items():
                    if k in ("xin", "ident"):
                        continue
                    if k == "cT":
                        m["cT"] = v
                    elif k == "winA":
                        m[pre + "winA_" + kind] = v
                    else:
                        m[pre + k] = v
        bm = inp["b_mod"][l][2048:6144]
        shared_b = {
            "wmodB": np.ascontiguousarray(inp["w_mod"][l][:, 2048:6144]),
            "bmodrow": np.ascontiguousarray(np.broadcast_to(bm[None], (128, 4096))),
            "bmodcol": _col_layout(bm),
            "wout": np.ascontiguousarray(inp["w_out"][l][_WOUT_PERM]),
            "lnrow": np.ascontiguousarray(np.broadcast_to(
                np.stack([inp["ln1_w"][l], inp["ln1_b"][l], inp["ln2_w"][l], inp["ln2_b"][l]])[None], (128, 4, D))),
            "rw": inp["router_w"][l],
            "rbias": np.ascontiguousarray(np.broadcast_to(inp["router_bias"][l][None], (128, 64))),
            "eg": np.concatenate([inp["exp_w_gate"][l], inp["sh_w_gate"][l][None]], 0),
            "eu": np.concatenate([inp["exp_w_up"][l], inp["sh_w_up"][l][None]], 0),
            "ed": np.concatenate([inp["exp_w_down"][l], inp["sh_w_down"][l][None]], 0),
        }
        for core in range(8):
            hp = core % 2
            hc = slice(128 * hp, 128 * hp + 128)
            m = maps[core]
            for k, v in shared_b.items():
                m[pre + k] = v
            m[pre + "rlnrow"] = np.ascontiguousarray(np.broadcast_to(
                np.stack([inp["rwkv_ln_w"][l][hc], inp["rwkv_ln_b"][l][hc]])[None], (128, 2, 128)))
    return maps


def kernel_unfused(**inp):
    return _kernel_unfused(inp)


def kernel(**inp):
    inp = {k: np.asarray(v) for k, v in inp.items()}
    nc = build_fused(2)
    res = run_bass_kernel_spmd(nc, prep_fused(inp, 2), core_ids=list(range(8)))
    x = np.empty_like(inp["x"], dtype=np.float32)
    for core in range(8):
        b, hf = core // 2, core % 2
        o = res.results[core]["xout"]
        x[b][2048 * hf:2048 * hf + 2048] = o[128:]
    return x
```

```python
import numpy as np
import ml_dtypes
import concourse.bass as bass
import concourse.mybir as mybir
from concourse.bass_utils import run_bass_kernel_spmd

F32 = mybir.dt.float32
BF16 = mybir.dt.bfloat16
AF = mybir.ActivationFunctionType
ALU = mybir.AluOpType
AX = mybir.AxisListType

D = 1024
T = 4352
NT = 34
NCTX = 256
ISQ96 = 96 ** -0.5


class Res:
    __slots__ = ("w", "r")

    def __init__(self):
        self.w = None
        self.r = {}


class Buf:
    def __init__(self, t):
        self.t = t
        self.res = Res()
        self._sub = {}

    def __getitem__(self, k):
        return self.t[k]

    def sub(self, k):
        if k not in self._sub:
            self._sub[k] = Res()
        return self._sub[k]


def _res(x):
    return x.res if hasattr(x, "res") else x


class Prog:
    CE = ("tensor", "vector", "scalar", "gpsimd")

    def __init__(self, nc, ndma=12):
        self.nc = nc
        self.lists = {e: [] for e in self.CE + ("sync",)}
        self.sems = {e: nc.alloc_semaphore("es_" + e) for e in self.CE}
        for i in range(ndma):
            self.sems["d%d" % i] = nc.alloc_semaphore("ds%d" % i)
        self.ndma = ndma
        self.duse = [0] * ndma
        self.dnext = 0
        self.tick = {e: 0 for e in self.CE}
        self.seen = {e: {} for e in self.lists}
        self.n = 0
        self.scoped = False
        self.sp = 16384 + 256
        self.cnt = 0
        self.ncoll = 0

    def sb(self, name, shape, dt):
        if not self.scoped:
            self.cnt += 1
            return Buf(self.nc.alloc_sbuf_tensor("s%d_%s" % (self.cnt, name), list(shape), dt))
        esz = 4 if dt == F32 else 2
        nb = esz
        for d_ in shape[1:]:
            nb *= d_
        nb = (nb + 63) // 64 * 64
        off = self.sp
        self.sp += nb
        assert self.sp <= 229000, ("SBUF overflow", name, self.sp)
        self.cnt += 1
        return Buf(self.nc.alloc_sbuf_tensor_at("s%d_%s" % (self.cnt, name), list(shape), dt, offset=off))

    def ps(self, name, shape, dt=F32):
        return Buf(self.nc.alloc_psum_tensor("p_" + name, list(shape), dt))

    def mark(self):
        return self.sp

    def release(self, mark):
        self.barrier()
        self.sp = mark

    def barrier(self):
        tgt = {e: self.tick[e] for e in self.CE if self.tick[e]}
        for i in range(self.ndma):
            if self.duse[i]:
                tgt["d%d" % i] = 16 * self.duse[i]
        for i in range(self.ncoll):
            tgt["c%d" % i] = 1
        for q in self.lists:
            waits = []
            for k, v in tgt.items():
                if k == q:
                    continue
                if self.seen[q].get(k, 0) >= v:
                    continue
                self.seen[q][k] = v
                waits.append((k, v))
            if waits:
                self.lists[q].append((waits, None, None, 0))

    def coll(self, kind, in_ap, out_ap, groups, r=(), w=()):
        key = "c%d" % self.ncoll
        self.ncoll += 1
        self.sems[key] = self.nc.alloc_semaphore("cs_" + key)
        waits = self._deps("gpsimd", r, w)
        tok = (key, 1)
        self.lists["gpsimd"].append((waits, lambda e: e.collective_compute(
            kind, ALU.bypass, replica_groups=groups, ins=[in_ap], outs=[out_ap]), key, 1))
        self._mark(tok, r, w)
        return tok

    def _deps(self, q, r, w, extra=None):
        deps = dict(extra or {})

        def add(k, v):
            if deps.get(k, 0) < v:
                deps[k] = v
        for x in r:
            x = _res(x)
            if x.w is not None:
                add(*x.w)
        for x in w:
            x = _res(x)
            if x.w is not None:
                add(*x.w)
            for k, v in x.r.items():
                add(k, v)
        waits = []
        seen = self.seen[q]
        for k, v in deps.items():
            if k == q and q == "tensor":
                continue
            if seen.get(k, 0) >= v:
                continue
            seen[k] = v
            waits.append((k, v))
        return waits

    def _mark(self, tok, r, w):
        k, v = tok
        for x in r:
            x = _res(x)
            if x.r.get(k, 0) < v:
                x.r[k] = v
        for x in w:
            x = _res(x)
            x.w = tok
            x.r = {}

    def op(self, eng, fn, r=(), w=()):
        waits = self._deps(eng, r, w)
        self.tick[eng] += 1
        tok = (eng, self.tick[eng])
        self.lists[eng].append((waits, fn, eng, 1))
        self._mark(tok, r, w)
        self.n += 1
        return tok

    def dma(self, q, out, in_, r=(), w=()):
        i = self.dnext
        self.dnext = (i + 1) % self.ndma
        key = "d%d" % i
        extra = {key: 16 * self.duse[i]} if self.duse[i] else {}
        waits = self._deps(q, r, w, extra)
        self.duse[i] += 1
        tok = (key, 16 * self.duse[i])
        self.lists[q].append((waits, lambda e: e.dma_start(out=out, in_=in_), key, 16))
        self._mark(tok, r, w)
        self.n += 1
        return tok

    def call(self, eng, name, *args, r=(), w=(), **kw):
        return self.op(eng, lambda e: getattr(e, name)(*args, **kw), r, w)

    def mm(self, out, lhsT, rhs, start=True, stop=True, r=(), w=(), skip=False):
        return self.op("tensor", lambda e: e.matmul(out, lhsT, rhs, start=start, stop=stop,
                                                     skip_group_check=skip), r, w)

    def tr(self, out, in_, ident, r=(), w=()):
        return self.op("tensor", lambda e: e.transpose(out, in_, ident), r, w)

    def act(self, out, in_, func, r=(), w=(), eng="scalar", **kw):
        return self.op(eng, lambda e: e.activation(out, in_, func, **kw), r, w)

    def tt(self, out, in0, in1, op, r=(), w=(), eng="vector"):
        return self.op(eng, lambda e: e.tensor_tensor(out, in0, in1, op), r, w)

    def ts(self, out, in0, s1, s2, op0, op1=None, r=(), w=(), eng="vector", **kw):
        if op1 is None:
            return self.op(eng, lambda e: e.tensor_scalar(out, in0, s1, s2, op0, **kw), r, w)
        return self.op(eng, lambda e: e.tensor_scalar(out, in0, s1, s2, op0, op1, **kw), r, w)

    def stt(self, out, in0, s, in1, op0, op1, r=(), w=()):
        return self.op("vector", lambda e: e.scalar_tensor_tensor(out, in0, s, in1, op0, op1), r, w)

    def cp(self, out, in_, r=(), w=(), eng="vector"):
        if eng == "scalar":
            return self.op(eng, lambda e: e.activation(out, in_, AF.Copy), r, w)
        return self.op(eng, lambda e: e.tensor_copy(out, in_), r, w)

    def memset(self, ap, val, w=(), eng="vector"):
        return self.op(eng, lambda e: e.memset(ap, val), (), w)

    def finish(self):
        waits = []
        for e in self.CE:
            if self.tick[e]:
                waits.append((e, self.tick[e]))
        for i in range(self.ndma):
            if self.duse[i]:
                waits.append(("d%d" % i, 16 * self.duse[i]))
        for i in range(self.ncoll):
            waits.append(("c%d" % i, 1))
        self.lists["sync"].append((waits, None, None, 0))

    def emit(self):
        self.finish()
        P = self
        with self.nc.Block() as block:
            def mk(name):
                def body(eng):
                    for waits, fn, skey, inc in P.lists[name]:
                        for k, v in waits:
                            eng.wait_ge(P.sems[k], v)
                        if fn is not None:
                            if inc == 1 and skey.startswith("c"):
                                fn(eng).then_inc(P.sems[skey])
                            else:
                                fn(eng).then_inc(P.sems[skey], inc)
                return body
            block.tensor(mk("tensor"))
            block.vector(mk("vector"))
            block.scalar(mk("scalar"))
            block.gpsimd(mk("gpsimd"))
            block.sync(mk("sync"))


KIND_COLS = {
    "mla": (("cq", 256), ("ckv", 128), ("kr", 96), ("krs", 96)),
    "mlstm": (("mq", 128), ("mk", 128), ("mv", 128), ("mo", 128), ("mg", 8)),
    "rwkv": (("rr", 128), ("rk", 128), ("rv", 128), ("rw", 128), ("ra", 128), ("rg", 128)),
}
A_COLS = {}
NCA = {}
for _k, _lst in KIND_COLS.items():
    _o = 0
    for _n, _w in _lst:
        A_COLS[_n] = (_o, _w)
        _o += _w
    NCA[_k] = _o
YW = {"mla": 256, "mlstm": 128, "rwkv": 128}

QBLOCKS = [(0, 256)] + [(256 + 512 * i, 512) for i in range(8)]


def phase_ln(nc, P, ps, ident, xrow_fn, cT, wmodA, bmodA, xres_list):
    csil = P.sb("csil", [128, 8, 2], F32)
    P.dma("sync", csil[:], cT[:, :, :], w=[csil])
    P.act(csil[:], csil[:], AF.Silu, r=[csil], w=[csil])
    bmod = P.sb("bmod", [128, 16], F32)
    P.dma("sync", bmod[:], bmodA[:, :], w=[bmod])
    modc = P.sb("modc", [128, 16, 2], F32)
    wm = [P.sb("wm%d" % i, [128, 8, 128], F32) for i in range(2)]
    for oc in range(16):
        wq = wm[oc % 2]
        P.dma("sync", wq[:], wmodA[:, oc * 128:(oc + 1) * 128].rearrange("(k p) c -> p k c", p=128), w=[wq])
        pt = ps[oc % 2]
        for kc in range(8):
            P.mm(pt[:, 0:2], wq[:, kc, :], csil[:, kc, :],
                 start=(kc == 0), stop=(kc == 7), r=[wq, csil], w=[pt])
        P.act(modc[:, oc, :], pt[:, 0:2], AF.Identity, r=[pt, bmod], w=[modc],
              bias=bmod[:, oc:oc + 1])
    P.ts(modc[:, 8:16, :], modc[:, 8:16, :], 1.0, None, ALU.add, r=[modc], w=[modc])
    hT = P.sb("hT", [128, 8, T], BF16)
    xt = [P.sb("xt%d" % i, [128, D], F32) for i in range(2)]
    st = P.sb("st", [128, 12], F32)
    mv = P.sb("mv", [128, 2], F32)
    rs = P.sb("rs", [128, 1], F32)
    for tt in range(NT):
        x_ = xt[tt % 2]
        n_ = x_
        m = 1 if tt < 2 else 0
        src, sres = xrow_fn(tt)
        P.dma("sync", x_[:], src, r=sres, w=[x_])
        P.call("vector", "bn_stats", st[:, 0:6], x_[:, 0:512], r=[x_], w=[st])
        P.call("vector", "bn_stats", st[:, 6:12], x_[:, 512:1024], r=[x_], w=[st])
        P.call("vector", "bn_aggr", mv[:], st[:], r=[st], w=[mv])
        P.act(rs[:], mv[:, 1:2], AF.Sqrt, r=[mv], w=[rs], bias=1e-6)
        P.call("vector", "reciprocal", rs[:], rs[:], r=[rs], w=[rs])
        P.ts(n_[:], x_[:], mv[:, 0:1], rs[:, 0:1], ALU.subtract, ALU.mult, r=[x_, mv, rs], w=[n_])
        for half in range(2):
            pt = ps[half]
            for j4 in range(4):
                j = half * 4 + j4
                P.tr(pt[:, j4 * 128:(j4 + 1) * 128], n_[:, j * 128:(j + 1) * 128], ident[:],
                     r=[n_, ident], w=[pt])
            for j4 in range(4):
                j = half * 4 + j4
                P.act(hT[:, j, tt * 128:(tt + 1) * 128], pt[:, j4 * 128:(j4 + 1) * 128], AF.Identity,
                      r=[pt, modc], w=[hT.sub(tt)],
                      scale=modc[:, 8 + j, m:m + 1], bias=modc[:, j, m:m + 1])
    hres = [hT.sub(tt) for tt in range(NT)]
    return hT, hres, xt


def make_proj_fm(P, hT, hres, winb):
    def proj_fm(pt, col0, ncols, t0, w, first=True, last=True, src=None, wsrc=None):
        src = src or hT
        wsrc = wsrc or winb
        for kc in range(8):
            P.mm(pt[0:ncols, 0:w], wsrc[:, kc, col0:col0 + ncols], src[:, kc, t0:t0 + w],
                 start=(first and kc == 0), stop=(last and kc == 7),
                 r=[wsrc] + (hres if src is hT else [src]), w=[pt])
    return proj_fm


def build_A(kind, emit_ctx, dbg=()):
    nc = bass.Bass("TRN2", target_bir_lowering=False)
    P = Prog(nc)

    def din(name, shape, dt=F32):
        return nc.dram_tensor(name, list(shape), dt, kind="ExternalInput").ap()

    xin = din("xin", [T, D])
    cT = din("cT", [128, 8, 2])
    wmodA = din("wmodA", [D, 2048])
    bmodA = din("bmodA", [128, 16])
    winA = din("winA", [D, NCA[kind]])
    ident_d = din("ident", [128, 128])
    ymix = nc.dram_tensor("ymix", [T, YW[kind]], F32, kind="ExternalOutput").ap()

    ident = P.sb("ident", [128, 128], F32)
    P.dma("sync", ident[:], ident_d[:, :], w=[ident])
    identb = P.sb("identb", [128, 128], BF16)
    P.cp(identb[:], ident[:], r=[ident], w=[identb])
    onesb = P.sb("onesb", [128, 128], BF16)
    P.memset(onesb[:], 1.0, w=[onesb])
    ps = [P.ps("ps%d" % i, [128, 512]) for i in range(8)]
    winb = P.sb("winb", [128, 8, NCA[kind]], BF16)
    for kc in range(8):
        P.dma("gpsimd", winb[:, kc, :], winA[kc * 128:(kc + 1) * 128, :], w=[winb])
    hT, hres, xt = phase_ln(nc, P, ps, ident, lambda tt: (xin[tt * 128:(tt + 1) * 128, :], []), cT, wmodA, bmodA, None)
    proj_fm = make_proj_fm(P, hT, hres, winb)
    env = dict(nc=nc, P=P, din=din, ps=ps, hT=hT, hres=hres, winb=winb, proj_fm=proj_fm, ident=ident,
               identb=identb, onesb=onesb, ymix=ymix, emit_ctx=emit_ctx, dbg=dbg, xt=xt)
    if kind == "mla":
        build_mla(**env)
    elif kind == "mlstm":
        build_mlstm(**env)
    else:
        build_rwkv(**env)
    P.emit()
    return nc


def build_mla(nc, P, din, ps, hT, hres, winb, proj_fm, ident, identb, onesb, ymix, emit_ctx, dbg, xt):
    wuq_d = din("wuq", [256, 4, 192])
    wukv_d = din("wukv", [128, 512])
    qn_d = din("qnorm", [128, 2])
    kvn_d = din("kvnorm", [128, 1])
    cs_d = din("cossin", [32, 2, 4096], BF16)

    wuqb = P.sb("wuqb", [128, 2, 768], BF16)
    qn = P.sb("qn", [128, 2], F32)
    P.dma("sync", qn[:], qn_d[:, :], w=[qn])
    for ch in range(2):
        P.dma("sync", xt[ch][:, 0:768], wuq_d[ch * 128:(ch + 1) * 128, :, :].rearrange("p h c -> p (h c)"), w=[xt[ch]])
    for ch in range(2):
        P.ts(wuqb[:, ch, :], xt[ch][:, 0:768], qn[:, ch:ch + 1], None, ALU.mult, r=[xt[ch], qn], w=[wuqb])
    wukvb = P.sb("wukvb", [128, 512], BF16)
    kvnw = P.sb("kvnw", [128, 1], F32)
    P.dma("sync", kvnw[:], kvn_d[:, :], w=[kvnw])
    cs = P.sb("cs", [128, 2, 4096], BF16)
    P.dma("sync", cs[64:96, :, :], cs_d[:, :, :], w=[cs])

    KT = [P.sb("KT%d" % h, [96, T], BF16) for h in range(4)]
    Vp = P.sb("Vp", [128, NT, 4, 65], BF16)
    P.memset(Vp[:], 1.0, w=[Vp])
    c32 = P.sb("c32", [128, 2, 512], F32)
    sq = P.sb("sqb", [128, 2, 512], BF16)
    rb = P.sb("rb", [128, 512], F32)
    cn = P.sb("cn", [128, 2, 512], BF16)
    t1 = P.sb("ropet1", [128, 512], F32)
    t2 = P.sb("ropet2", [128, 512], F32)
    P.dma("sync", t1[:], wukv_d[:, :], w=[t1])
    P.ts(wukvb[:], t1[:], kvnw[:, 0:1], None, ALU.mult, r=[t1, kvnw], w=[wukvb])

    def rmsnorm(nch, col0, t0, w, pbase):
        for ch in range(nch):
            pt = ps[pbase + ch]
            proj_fm(pt, col0 + ch * 128, 128, t0, w)
            P.act(c32[:, ch, 0:w], pt[:, 0:w], AF.Copy, r=[pt], w=[c32])
            P.act(sq[:, ch, 0:w], pt[:, 0:w], AF.Square, r=[pt], w=[sq], scale=float((128 * nch) ** -0.5))
        pt = ps[pbase + 2]
        for ch in range(nch):
            P.mm(pt[:, 0:w], onesb[:], sq[:, ch, 0:w], start=(ch == 0), stop=(ch == nch - 1), r=[onesb, sq], w=[pt])
        P.act(rb[:, 0:w], pt[:, 0:w], AF.Sqrt, r=[pt], w=[rb], bias=1e-6)
        P.call("vector", "reciprocal", rb[:, 0:w], rb[:, 0:w], r=[rb], w=[rb])
        for ch in range(nch):
            P.tt(cn[:, ch, 0:w], c32[:, ch, 0:w], rb[:, 0:w], ALU.mult, r=[c32, rb], w=[cn])

    def rope(dst, pq, psw, t0, w, lat):
        if not lat:
            P.cp(dst[64:96, t0:t0 + w], pq[64:96, 0:w], r=[pq], w=[dst])
            return
        l0 = t0 - NCTX
        P.tt(t1[64:96, 0:w], pq[64:96, 0:w], cs[64:96, 0, l0:l0 + w], ALU.mult, r=[pq, cs], w=[t1])
        P.tt(t2[64:96, 0:w], psw[64:96, 0:w], cs[64:96, 1, l0:l0 + w], ALU.mult, r=[psw, cs], w=[t2])
        P.tt(dst[64:96, t0:t0 + w], t1[64:96, 0:w], t2[64:96, 0:w], ALU.add, r=[t1, t2], w=[dst])

    ckv0 = A_COLS["ckv"][0]
    for (t0, w) in QBLOCKS:
        lat = t0 >= NCTX
        rmsnorm(1, ckv0, t0, w, 0)
        proj_fm(ps[3], A_COLS["kr"][0], 96, t0, w)
        if lat:
            proj_fm(ps[4], A_COLS["krs"][0], 96, t0, w)
        rope(KT[0], ps[3], ps[4], t0, w, lat)
        for h in range(4):
            pt = ps[5 + (h % 2)]
            P.mm(pt[0:64, 0:w], wukvb[:, h * 64:(h + 1) * 64], cn[:, 0, 0:w], r=[wukvb, cn], w=[pt])
            P.cp(KT[h][0:64, t0:t0 + w], pt[0:64, 0:w], r=[pt], w=[KT[h]], eng="scalar")
            if h > 0:
                P.cp(KT[h][64:96, t0:t0 + w], KT[0][64:96, t0:t0 + w], r=[KT[0]], w=[KT[h]], eng="gpsimd")
        for ti in range(w // 128):
            tt = t0 // 128 + ti
            pt = ps[7]
            P.mm(pt[:, 0:256], cn[:, 0, ti * 128:(ti + 1) * 128], wukvb[:, 256:512], r=[wukvb, cn], w=[pt])
            P.cp(Vp[:, tt, :, 0:64], pt[:, 0:256].rearrange("p (h c) -> p h c", h=4), r=[pt], w=[Vp])

    QT = [P.sb("QT%d" % h, [96, 512], BF16) for h in range(4)]
    PT = [P.sb("PT%d" % i, [128, 512], BF16) for i in range(3)]
    zerob = P.sb("zerob", [128, 512], BF16)
    P.memset(zerob[:], 0.0, w=[zerob])
    rden = P.sb("rden", [128, 4, 1], F32)
    oT = P.sb("oT", [65, 512], F32)
    yo = [P.sb("yo%d" % i, [128, 4, 64], F32) for i in range(2)]
    cq0 = A_COLS["cq"][0]
    it = 0
    for (t0, w) in QBLOCKS:
        lat = t0 >= NCTX
        if not lat and not emit_ctx:
            continue
        rmsnorm(2, cq0, t0, w, 0)
        for h in range(4):
            pq = ps[3]
            psw = ps[4]
            for ch in range(2):
                P.mm(pq[0:96, 0:w], wuqb[:, ch, h * 192:h * 192 + 96], cn[:, ch, 0:w],
                     start=(ch == 0), stop=(ch == 1), r=[wuqb, cn], w=[pq])
            if lat:
                for ch in range(2):
                    P.mm(psw[0:96, 0:w], wuqb[:, ch, h * 192 + 96:h * 192 + 192], cn[:, ch, 0:w],
                         start=(ch == 0), stop=(ch == 1), r=[wuqb, cn], w=[psw])
            P.cp(QT[h][0:64, 0:w], pq[0:64, 0:w], r=[pq], w=[QT[h]], eng="scalar")
            ropeq(P, QT[h], pq, psw, w, lat, t0, cs, t1, t2)
        nq = w // 128
        kts = range(NT) if lat else range(2)
        for h in range(4):
            accT = ps[5 + (h % 2)]
            units = list(kts)

            def qk(kt, j):
                sp_ = ps[j % 3]
                P.mm(sp_[:, 0:w], KT[h][0:96, kt * 128:(kt + 1) * 128], QT[h][0:96, 0:w], r=[KT[h], QT[h]], w=[sp_])
                return sp_
            spq = [qk(units[j], it + j) for j in range(min(2, len(units)))]
            for idx, kt in enumerate(units):
                sp = spq[idx]
                pt_ = PT[it % 3]
                if idx + 2 < len(units):
                    spq.append(qk(units[idx + 2], it + 2))
                it += 1
                P.act(pt_[:, 0:w], sp[:, 0:w], AF.Exp, r=[sp], w=[pt_], scale=ISQ96)
                P.mm(accT[0:65, 0:w], Vp[:, kt, h, :], pt_[:, 0:w], start=(idx == 0), stop=(idx == len(units) - 1),
                     r=[pt_, Vp], w=[accT])
            P.cp(oT[0:65, 0:w], accT[0:65, 0:w], r=[accT], w=[oT])
            acc = ps[7]
            for qi in range(nq):
                P.tr(acc[:, qi * 65:(qi + 1) * 65], oT[0:65, qi * 128:(qi + 1) * 128], ident[0:65, 0:65],
                     r=[oT, ident], w=[acc])
            y_ = yo[h % 2]
            a3 = acc[:, 0:nq * 65].rearrange("p (q c) -> p q c", c=65)
            P.call("vector", "reciprocal", rden[:, 0:nq, :], a3[:, :, 64:65], r=[acc], w=[rden])
            P.tt(y_[:, 0:nq, :], a3[:, :, 0:64], rden[:, 0:nq, :].to_broadcast([128, nq, 64]), ALU.mult,
                 r=[acc, rden], w=[y_])
            for qi in range(nq):
                r0 = t0 + qi * 128
                P.dma("sync", ymix[r0:r0 + 128, h * 64:(h + 1) * 64], y_[:, qi, :], r=[y_])


def ropeq(P, dst, pq, psw, w, lat, t0, cs, t1, t2):
    if not lat:
        P.cp(dst[64:96, 0:w], pq[64:96, 0:w], r=[pq], w=[dst])
        return
    l0 = t0 - NCTX
    P.tt(t1[64:96, 0:w], pq[64:96, 0:w], cs[64:96, 0, l0:l0 + w], ALU.mult, r=[pq, cs], w=[t1])
    P.tt(t2[64:96, 0:w], psw[64:96, 0:w], cs[64:96, 1, l0:l0 + w], ALU.mult, r=[psw, cs], w=[t2])
    P.tt(dst[64:96, 0:w], t1[64:96, 0:w], t2[64:96, 0:w], ALU.add, r=[t1, t2], w=[dst])


def _cols_A(hp, kind):
    N_M = 1040
    RB = N_M
    MB = N_M + 1152
    hs = [2 * hp, 2 * hp + 1]
    idx = {}
    idx["cq"] = list(range(MB, MB + 256))
    idx["ckv"] = list(range(MB + 256, MB + 384))
    kr = list(range(MB + 384, MB + 416))
    idx["kr"] = [-1] * 64 + kr
    idx["krs"] = [-1] * 64 + [kr[i ^ 1] for i in range(32)]
    idx["mq"] = list(range(128 * hp, 128 * hp + 128))
    idx["mk"] = list(range(256 + 128 * hp, 256 + 128 * hp + 128))
    idx["mv"] = list(range(512 + 128 * hp, 512 + 128 * hp + 128))
    idx["mo"] = list(range(768 + 128 * hp, 768 + 128 * hp + 128))
    idx["mg"] = [1024 + g * 4 + h for g in (0, 2, 1, 3) for h in hs]
    idx["rr"] = list(range(RB + 128 * hp, RB + 128 * hp + 128))
    idx["rk"] = list(range(RB + 256 + 128 * hp, RB + 256 + 128 * hp + 128))
    idx["rv"] = list(range(RB + 512 + 128 * hp, RB + 512 + 128 * hp + 128))
    idx["rw"] = list(range(RB + 768, RB + 896))
    idx["ra"] = list(range(RB + 896, RB + 1024))
    idx["rg"] = list(range(RB + 1024, RB + 1152))
    out = []
    for n, w in KIND_COLS[kind]:
        assert len(idx[n]) == w, n
        out += idx[n]
    return np.array(out)


def _gather_cols(w, cols):
    out = np.zeros((w.shape[0], len(cols)), w.dtype)
    m = cols >= 0
    out[:, m] = w[:, cols[m]]
    return out


def _col_layout(v):
    return np.ascontiguousarray(v.reshape(-1, 128).T)


def _rope_tables():
    n = 4096
    row = np.repeat(np.arange(64), 64).astype(np.float32)
    col = np.tile(np.arange(64), 64).astype(np.float32)
    freq = (np.float32(10000.0) ** (-np.arange(8, dtype=np.float32) / np.float32(8))).astype(np.float32)
    ang = np.concatenate([row[:, None] * freq, col[:, None] * freq], -1)
    cos = np.cos(ang).astype(np.float32)
    sin = np.sin(ang).astype(np.float32)
    tab = np.zeros((32, 2, n), np.float32)
    for r in range(32):
        tab[r, 0] = cos[:, r // 2]
        tab[r, 1] = sin[:, r // 2] * (-1.0 if r % 2 == 0 else 1.0)
    return tab.astype(ml_dtypes.bfloat16)


def prep_A(inp, l, x_cur, xc_cur, kind):
    maps = []
    ident = np.eye(128, dtype=np.float32)
    cs = _rope_tables()
    for core in range(8):
        b, hp = core // 2, core % 2
        m = {}
        m["xin"] = np.ascontiguousarray(np.concatenate([xc_cur[b], x_cur[b]], 0))
        cc = np.stack([inp["c"][b], inp["c_ctx"]], -1)
        m["cT"] = np.ascontiguousarray(cc.reshape(8, 128, 2).transpose(1, 0, 2))
        m["wmodA"] = np.ascontiguousarray(inp["w_mod"][l][:, 0:2048])
        m["bmodA"] = _col_layout(inp["b_mod"][l][0:2048])
        m["winA"] = _gather_cols(inp["w_in"][l], _cols_A(hp, kind))
        m["ident"] = ident
        if kind == "mlstm":
            cw = inp["mlstm_conv"][l]
            cc_ = np.concatenate([cw[:, 128 * hp:128 * hp + 128], cw[:, 256 + 128 * hp:256 + 128 * hp + 128]], 1)
            m["convbc"] = np.ascontiguousarray(np.broadcast_to(cc_[None], (128, 3, 256)))
            gb = inp["mlstm_gate_bias"][l]
            gv = np.array([gb[g, h] for g in (0, 2, 1, 3) for h in (2 * hp, 2 * hp + 1)], np.float32)
            m["gbias"] = np.ascontiguousarray(np.broadcast_to(gv[None], (128, 8)))
            nw = inp["mlstm_norm_w"][l][128 * hp:128 * hp + 128]
            m["normw"] = np.ascontiguousarray(np.broadcast_to(nw[None], (128, 128)))
            s_ = np.arange(128)[:, None]
            j_ = np.arange(512)[None, :]
            mf = np.stack([(s_ + 128 * o <= j_) for o in range(4)], 1)
            mb = np.stack([(s_ + 128 * o >= j_) for o in range(4)], 1)
            m["maskf"] = mf.astype(np.float32).astype(ml_dtypes.bfloat16)
            m["maskb"] = mb.astype(np.float32).astype(ml_dtypes.bfloat16)
            m["triu"] = (s_ <= np.arange(128)[None, :]).astype(np.float32)
        if kind == "rwkv":
            RB = 1040
            cols = _cols_A(hp, "rwkv") - RB
            m["mubc"] = np.ascontiguousarray(np.broadcast_to(inp["rwkv_mu"][l][cols][None], (128, 768)))
            hc = slice(128 * hp, 128 * hp + 128)
            pc = np.zeros((128, 8), np.float32)
            pc[:, 0] = inp["rwkv_w0"][l][0][hc]
            pc[:, 1] = inp["rwkv_w0"][l][1][hc]
            pc[:, 2] = inp["rwkv_a0"][l][0][hc]
            pc[:, 3] = inp["rwkv_a0"][l][1][hc]
            pc[:, 4] = inp["rwkv_k_k"][l][hc]
            pc[:, 5] = inp["rwkv_k_a"][l][hc]
            pc[:, 6] = inp["rwkv_r_k"][l][hc]
            m["pcol"] = pc
            m["wup"] = np.ascontiguousarray(inp["rwkv_w_up"][l][:, :, hc].reshape(128, 128))
            m["aup"] = np.ascontiguousarray(inp["rwkv_a_up"][l][:, :, hc].reshape(128, 128))
            m["gup"] = np.ascontiguousarray(inp["rwkv_g_up"][l][:, hc])
            bo = np.zeros((128, 128), np.float32)
            bo[0:64, 0:64] = 1.0
            bo[64:128, 64:128] = 1.0
            m["bones"] = bo
        if kind != "mla":
            maps.append(m)
            continue
        wuq = inp["mla_w_uq"][l].reshape(256, 8, 96)[:, 4 * hp:4 * hp + 4, :]
        w4 = np.zeros((256, 4, 192), np.float32)
        w4[:, :, 0:96] = wuq
        sw = [64 + (i ^ 1) for i in range(32)]
        w4[:, :, 160:192] = wuq[:, :, sw]
        m["wuq"] = w4
        wukv = inp["mla_w_ukv"][l].reshape(128, 8, 128)[:, 4 * hp:4 * hp + 4, :]
        m["wukv"] = np.ascontiguousarray(np.concatenate(
            [wukv[:, :, 0:64].reshape(128, 256), wukv[:, :, 64:128].reshape(128, 256)], 1))
        m["qnorm"] = _col_layout(inp["mla_q_norm"][l])
        m["kvnorm"] = _col_layout(inp["mla_kv_norm"][l])
        m["cossin"] = cs
        maps.append(m)
    return maps


def build_mlstm(nc, P, din, ps, hT, hres, winb, proj_fm, ident, identb, onesb, ymix, emit_ctx, dbg, xt):
    convbc_d = din("convbc", [128, 3, 256])
    gbias_d = din("gbias", [128, 8])
    normw_d = din("normw", [128, 128])
    maskf_d = din("maskf", [128, 4, 512], BF16)
    maskb_d = din("maskb", [128, 4, 512], BF16)
    triu_d = din("triu", [128, 128])

    convbc = P.sb("convbc", [128, 3, 256], F32)
    P.dma("sync", convbc[:], convbc_d[:, :, :], w=[convbc])
    gbias = P.sb("gbias", [128, 8], F32)
    P.dma("sync", gbias[:], gbias_d[:, :], w=[gbias])
    normw = P.sb("normw", [128, 128], F32)
    P.dma("sync", normw[:], normw_d[:, :], w=[normw])
    maskf = P.sb("maskf", [128, 4, 512], BF16)
    P.dma("sync", maskf[:], maskf_d[:, :, :], w=[maskf])
    maskb = P.sb("maskb", [128, 4, 512], BF16)
    P.dma("sync", maskb[:], maskb_d[:, :, :], w=[maskb])
    triu = P.sb("triu", [128, 128], F32)
    P.dma("sync", triu[:], triu_d[:, :], w=[triu])
    ones32 = P.sb("ones32", [128, 128], F32)
    P.memset(ones32[:], 1.0, w=[ones32])

    wc = P.sb("wc", [128, 8, 3, 256], BF16)
    q0 = A_COLS["mq"][0]
    for kc in range(8):
        for j in range(3):
            P.tt(wc[:, kc, j, :], winb[:, kc, q0:q0 + 256], convbc[:, j, :], ALU.mult, r=[winb, convbc], w=[wc],
                 eng="vector")

    QK = [P.sb("QmT", [128, T], BF16), P.sb("KmT", [128, T], BF16)]
    Vp = P.sb("Vpm", [128, NT, 2, 65], BF16)
    P.memset(Vp[:], 1.0, w=[Vp])
    og = P.sb("og", [128, NT, 128], BF16)
    G = P.sb("G", [128, NT, 8], F32)
    sgt = P.sb("sgt", [128, 128], F32)

    STOP = 9
    for (t0, w) in QBLOCKS:
        seg0 = t0 in (0, NCTX)
        seg1 = (t0 + w) in (NCTX, T)
        for qk in range(2):
            pt = ps[5 + qk]
            first = True
            for j in (1, 0, 2):
                a = 1 if (j == 0 and seg0) else 0
                b = 1 if (j == 2 and seg1) else 0
                for kc in range(8):
                    last = (j == 2 and kc == 7)
                    P.mm(pt[:, a:w - b], wc[:, kc, j, qk * 128:(qk + 1) * 128],
                         hT[:, kc, t0 + a + j - 1:t0 + w - b + j - 1],
                         start=first, stop=last, r=[wc] + hres, w=[pt], skip=True)
                    first = False
            P.act(QK[qk][:, t0:t0 + w], pt[:, 0:w], AF.Silu, r=[pt], w=[QK[qk]])
        v0 = A_COLS["mv"][0]
        if STOP == -1:
            continue
        for ti in range(w // 128):
            tt = t0 // 128 + ti
            pt = ps[7]
            for kc in range(8):
                P.mm(pt[:, 0:264], hT[:, kc, tt * 128:(tt + 1) * 128], winb[:, kc, v0:v0 + 264],
                     start=(kc == 0), stop=(kc == 7), r=[winb] + hres, w=[pt])
            P.cp(Vp[:, tt, :, 0:64], pt[:, 0:128].rearrange("p (h c) -> p h c", h=2), r=[pt], w=[Vp])
            P.cp(sgt[:], pt[:, 128:256], r=[pt], w=[sgt])
            P.act(sgt[:], sgt[:], AF.Exp, r=[sgt], w=[sgt], scale=-1.0)
            P.ts(sgt[:], sgt[:], 1.0, None, ALU.add, r=[sgt], w=[sgt])
            P.call("vector", "reciprocal", sgt[:], sgt[:], r=[sgt], w=[sgt])
            P.cp(og[:, tt, :], sgt[:], r=[sgt], w=[og])
            P.tt(G[:, tt, :], pt[:, 256:264], gbias[:], ALU.add, r=[pt, gbias], w=[G])

    if STOP < 1:
        return
    LF = P.sb("LF", [128, NT, 4], F32)
    P.act(LF[:], G[:, :, 4:8], AF.Exp, r=[G], w=[LF], scale=-1.0)
    P.act(LF[:], LF[:], AF.Ln, r=[LF], w=[LF], bias=1.0)
    P.ts(LF[:], LF[:], -1.0, None, ALU.mult, r=[LF], w=[LF])
    PW = P.sb("PW", [128, NT, 4], F32)
    TOT = P.sb("TOT", [128, NT, 4], F32)
    IP = P.sb("IP", [128, NT, 4], F32)
    BQ = P.sb("BQ", [128, NT, 4], F32)
    UK = P.sb("UK", [128, NT, 4], F32)
    lf2 = LF[:].rearrange("p t c -> p (t c)")
    P.mm(ps[5][:, 0:NT * 4], triu[:], lf2, r=[triu, LF], w=[ps[5]])
    P.cp(PW[:].rearrange("p t c -> p (t c)"), ps[5][:, 0:NT * 4], r=[ps[5]], w=[PW])
    P.mm(ps[6][:, 0:NT * 4], ones32[:], lf2, r=[ones32, LF], w=[ps[6]])
    P.cp(TOT[:].rearrange("p t c -> p (t c)"), ps[6][:, 0:NT * 4], r=[ps[6]], w=[TOT])
    for c in range(4):
        for (a, b) in ((0, 2), (2, NT)):
            P.call("vector", "tensor_tensor_scan", IP[:, a:b, c], ones32[:, 0:b - a], TOT[:, a:b, c], 0.0,
                   ALU.mult, ALU.add, r=[TOT, ones32], w=[IP])
    EX = TOT
    P.tt(EX[:], IP[:], TOT[:], ALU.subtract, r=[IP, TOT], w=[TOT])
    P.tt(BQ[:, :, 0:2], PW[:, :, 0:2], EX[:, :, 0:2], ALU.add, r=[PW, TOT], w=[BQ])
    for c in range(2):
        P.ts(BQ[:, 2:NT, c], BQ[:, 2:NT, c], IP[:, 1, c:c + 1], None, ALU.add, r=[BQ, IP], w=[BQ])
    P.tt(BQ[:, :, 2:4], LF[:, :, 2:4], PW[:, :, 2:4], ALU.subtract, r=[LF, PW], w=[BQ])
    P.tt(BQ[:, :, 2:4], BQ[:, :, 2:4], EX[:, :, 2:4], ALU.subtract, r=[BQ, TOT], w=[BQ])
    for c in range(2, 4):
        P.ts(BQ[:, 2:NT, c], BQ[:, 2:NT, c], IP[:, NT - 1, c:c + 1], None, ALU.add, r=[BQ, IP], w=[BQ])
    P.tt(UK[:], G[:, :, 0:4], BQ[:], ALU.subtract, r=[G, BQ], w=[UK])
    P.ts(UK[:], UK[:], float(-np.log(8.0)), None, ALU.add, r=[UK], w=[UK])

    if STOP < 2:
        return
    BL = [P.sb("BL%d" % i, [128, 128], F32) for i in range(2)]
    BCs2 = [P.sb("BCs", [128, 512], F32) for i in range(2)]
    igrp = 0
    DT = [P.sb("DT%d" % i, [128, 512], F32) for i in range(3)]
    WT = [P.sb("WT%d" % i, [128, 512], BF16) for i in range(3)]
    WM = P.sb("WM", [128, 512], F32)
    zerob = P.sb("zerob", [128, 512], BF16)
    P.memset(zerob[:], 0.0, w=[zerob])
    den = P.sb("den", [128, 4, 1], F32)
    oTm = P.sb("oTm", [65, 512], F32)
    nden = P.sb("nden", [128, 4, 1], F32)
    c60 = P.sb("c60", [128, 1], F32)
    P.memset(c60[:], 60.0, w=[c60])
    hd = [P.sb("hd%d" % i, [128, 4, 64], F32) for i in range(2)]
    st = P.sb("st2", [128, 4, 6], F32)
    mv = P.sb("mv2", [128, 4, 2], F32)
    yo = [P.sb("yom%d" % i, [128, 4, 64], F32) for i in range(2)]
    it = 0
    ib = 0
    for (t0, w) in QBLOCKS:
        lat = t0 >= NCTX
        if not lat and not emit_ctx:
            continue
        nq = w // 128
        tq0 = t0 // 128
        for hl in range(2):
            hp_ = slice(hl * 64, hl * 64 + 64)
            for d in range(2):
                c = d * 2 + hl
                BCs = BCs2[igrp % 2]
                igrp += 1
                for qi in range(nq):
                    bl = BL[ib % 2]
                    ib += 1
                    P.ts(bl[:], ones32[:], BQ[:, tq0 + qi, c:c + 1], None, ALU.mult, r=[ones32, BQ], w=[bl])
                    P.mm(ps[2][:, qi * 128:(qi + 1) * 128], bl[:], ident[:], r=[bl, ident], w=[ps[2]])
                P.cp(BCs[:, 0:w], ps[2][:, 0:w], r=[ps[2]], w=[BCs], eng="scalar")
                if d == 0:
                    kts = [kt for kt in range(NT) if kt * 128 < t0 + w and (lat or kt < 2)]
                else:
                    kts = [kt for kt in range(NT) if (kt < 2 and lat) or ((kt * 128 + 128 > t0) and (lat == (kt >= 2)))]
                accT = ps[3 + d]

                def qk(kt, j):
                    sp_ = (ps[0], ps[1], ps[7])[j % 3]
                    P.mm(sp_[:, 0:w], QK[1][hp_, kt * 128:(kt + 1) * 128], QK[0][hp_, t0:t0 + w], r=QK, w=[sp_])
                    return sp_
                spq = [qk(kts[j], it + j) for j in range(min(2, len(kts)))]
                for idx, kt in enumerate(kts):
                    diag = (kt * 128 >= t0) and (kt * 128 < t0 + w)
                    sp = spq[idx]
                    dt_ = DT[it % 3]
                    wt_ = WT[it % 3]
                    if idx + 2 < len(kts):
                        spq.append(qk(kts[idx + 2], it + 2))
                    it += 1
                    if diag:
                        P.ts(dt_[:, 0:w], BCs[:, 0:w], UK[:, kt, c:c + 1], c60[:, 0:1], ALU.add, ALU.min, r=[BCs, UK, c60], w=[dt_])
                        P.act(dt_[:, 0:w], dt_[:, 0:w], AF.Exp, r=[dt_], w=[dt_])
                        P.tt(WM[:, 0:w], sp[:, 0:w], dt_[:, 0:w], ALU.mult, r=[sp, dt_], w=[WM])
                        mk = (maskf if d == 0 else maskb)
                        P.tt(wt_[:, 0:w], WM[:, 0:w], mk[:, (kt * 128 - t0) // 128, 0:w], ALU.mult, r=[WM, mk], w=[wt_])
                    else:
                        P.act(dt_[:, 0:w], BCs[:, 0:w], AF.Exp, r=[BCs, UK], w=[dt_], bias=UK[:, kt, c:c + 1])
                        P.tt(wt_[:, 0:w], sp[:, 0:w], dt_[:, 0:w], ALU.mult, r=[sp, dt_], w=[wt_])
                    P.mm(accT[0:65, 0:w], Vp[:, kt, hl, :], wt_[:, 0:w], start=(idx == 0), stop=(idx == len(kts) - 1),
                         r=[wt_, Vp], w=[accT])
                P.cp(oTm[0:65, 0:w], accT[0:65, 0:w], r=[accT], w=[oTm])
                acc = ps[5 + d]
                for qi in range(nq):
                    P.tr(acc[:, qi * 65:(qi + 1) * 65], oTm[0:65, qi * 128:(qi + 1) * 128], ident[0:65, 0:65],
                         r=[oTm, ident], w=[acc])
                a3 = acc[:, 0:nq * 65].rearrange("p (q c) -> p q c", c=65)
                P.cp(den[:, 0:nq, :], a3[:, :, 64:65], r=[acc], w=[den])
                P.ts(nden[:, 0:nq, :], den[:, 0:nq, :], -1.0, None, ALU.mult, r=[den], w=[nden])
                P.tt(den[:, 0:nq, :], den[:, 0:nq, :], nden[:, 0:nq, :], ALU.max, r=[den, nden], w=[den])
                P.ts(den[:, 0:nq, :], den[:, 0:nq, :], 1.0, None, ALU.max, r=[den], w=[den])
                P.call("vector", "reciprocal", den[:, 0:nq, :], den[:, 0:nq, :], r=[den], w=[den])
                P.tt(hd[d][:, 0:nq, :], a3[:, :, 0:64], den[:, 0:nq, :].to_broadcast([128, nq, 64]), ALU.mult,
                     r=[acc, den], w=[hd[d]])
            h_ = hd[0]
            P.tt(h_[:, 0:nq, :], hd[0][:, 0:nq, :], hd[1][:, 0:nq, :], ALU.add, r=hd, w=[hd[0]])
            y_ = yo[hl]
            for qi in range(nq):
                P.call("vector", "bn_stats", st[:, qi, :], h_[:, qi, :], r=[h_], w=[st])
                P.call("vector", "bn_aggr", mv[:, qi, :], st[:, qi, :], r=[st], w=[mv])
            P.act(mv[:, 0:nq, 1:2], mv[:, 0:nq, 1:2], AF.Sqrt, r=[mv], w=[mv], bias=1e-6)
            P.call("vector", "reciprocal", mv[:, 0:nq, 1:2], mv[:, 0:nq, 1:2], r=[mv], w=[mv])
            for qi in range(nq):
                P.ts(y_[:, qi, :], h_[:, qi, :], mv[:, qi, 0:1], mv[:, qi, 1:2], ALU.subtract, ALU.mult, r=[h_, mv], w=[y_])
                P.tt(y_[:, qi, :], y_[:, qi, :], normw[:, hl * 64:(hl + 1) * 64], ALU.mult, r=[y_, normw], w=[y_])
                P.tt(y_[:, qi, :], y_[:, qi, :], og[:, tq0 + qi, hl * 64:(hl + 1) * 64], ALU.mult, r=[y_, og], w=[y_])
                r0 = t0 + qi * 128
                P.dma("sync", ymix[r0:r0 + 128, hl * 64:(hl + 1) * 64], y_[:, qi, :], r=[y_])


TB = 2176
NTB = 17
ALPHA = 4.0 ** 0.25
BBLOCKS = [(0, 512), (512, 512), (1024, 512), (1536, 512), (2048, 128)]


def build_B():
    nc = bass.Bass("TRN2", target_bir_lowering=False)
    P = Prog(nc)

    def din(name, shape, dt=F32):
        return nc.dram_tensor(name, list(shape), dt, kind="ExternalInput").ap()
    xres = din("xres", [TB, D])
    ymx = din("ymx", [TB, D])
    ident_d = din("ident", [128, 128])
    out = nc.dram_tensor("xout", [TB, D], F32, kind="ExternalOutput").ap()
    ident = P.sb("ident", [128, 128], F32)
    P.dma("sync", ident[:], ident_d[:, :], w=[ident])
    ones32 = P.sb("ones32", [128, 128], F32)
    P.memset(ones32[:], 1.0, w=[ones32])
    ps = [P.ps("ps%d" % i, [128, 512]) for i in range(8)]

    def xres_fn(tt, buf):
        P.dma("sync", buf[:], xres[tt * 128:(tt + 1) * 128, :], w=[buf])

    def ymx_fn(tt, buf, tmp):
        P.dma("sync", buf[:], ymx[tt * 128:(tt + 1) * 128, :], w=[buf])

    def out_fn(tt):
        return out[tt * 128:(tt + 1) * 128, :], []
    phase_B(nc, P, ps, ident, ones32, din, xres_fn, ymx_fn, out_fn)
    P.emit()
    return nc


def phase_B(nc, P, ps, ident, ones32, din, xres_fn, ymx_fn, out_fn, pre=""):
    cT = din("cT", [128, 8, 2]) if not pre else din.shared("cT", [128, 8, 2])
    wmodB = din("wmodB", [D, 4096])
    bmodrow = din("bmodrow", [128, 4096])
    bmodcol = din("bmodcol", [128, 32])
    wout_d = din("wout", [D, D])
    lnrow = din("lnrow", [128, 4, D])
    rw_d = din("rw", [D, 64])
    rb_d = din("rbias", [128, 64])
    eg_d = din("eg", [65, D, 256])
    eu_d = din("eu", [65, D, 256])
    ed_d = din("ed", [65, 256, D])
    x1d = nc.dram_tensor(pre + "x1scr", [TB, D], F32).ap()

    csil = P.sb("csil", [128, 8, 2], F32)
    P.dma("sync", csil[:], cT[:, :, :], w=[csil])
    P.act(csil[:], csil[:], AF.Silu, r=[csil], w=[csil])
    bmc = P.sb("bmc", [128, 32], F32)
    P.dma("sync", bmc[:], bmodcol[:, :], w=[bmc])
    bigall = P.sb("bigall", [128, 4, D], F32)

    class _V:
        def __init__(self, i):
            self.i = i
            self.res = Res()

        def __getitem__(self, k):
            return bigall[:, self.i, :][k] if not isinstance(k, tuple) else bigall[(k[0], self.i) + tuple(k[1:])]
    big = [_V(i) for i in range(4)]
    bigres = [b_.res for b_ in big]
    bigall2 = P.sb("bigall2", [128, 4, D], F32)

    class _V2(_V):
        def __getitem__(self, k):
            return bigall2[:, self.i, :][k] if not isinstance(k, tuple) else bigall2[(k[0], self.i) + tuple(k[1:])]
    big2 = [_V2(i) for i in range(4)]
    modc = P.sb("modc", [128, 16, 2], F32)
    for oc in range(16):
        wq = bigall[:, oc % 2, :].rearrange("p (k c) -> p k c", c=128)
        wqr = bigres[oc % 2]
        c0 = 1024 + oc * 128
        P.dma("sync", wq, wmodB[:, c0:c0 + 128].rearrange("(k p) c -> p k c", p=128), w=[wqr])
        pt = ps[oc % 2]
        for kc in range(8):
            P.mm(pt[:, 0:2], wq[:, kc, :], csil[:, kc, :], start=(kc == 0), stop=(kc == 7), r=[wqr, csil], w=[pt])
        P.act(modc[:, oc, :], pt[:, 0:2], AF.Identity, r=[pt, bmc], w=[modc], bias=bmc[:, 8 + oc:9 + oc])
    P.ts(modc[:, 8:16, :], modc[:, 8:16, :], 1.0, None, ALU.add, r=[modc], w=[modc])
    crep = P.sb("crep", [128, 8, 128], F32)
    growb = [P.sb("grow%d" % m, [128, D], F32) for m in range(2)]
    grow = [[growb[m], growb[m]] for m in range(2)]
    lnr2 = P.sb("lnr", [128, 2, D], F32)

    class _L:
        res = lnr2.res

        def __getitem__(self, k):
            return lnr2[(k[0], k[1] % 2) + tuple(k[2:])]
    lnr = _L()

    def make_gates(k):
        cbase = (0, 3072)[k]
        wrow = bigall[:, 0:2, :].rearrange("p a (k c) -> p (a k) c", c=512)
        for half in range(2):
            c0 = cbase + half * 512
            for m in range(2):
                pt = ps[2 + m]
                for kh in range(2):
                    P.dma("sync", wrow, wmodB[kh * 512:(kh + 1) * 512, c0:c0 + 512].rearrange("(k p) c -> p k c", p=128),
                          w=bigres[0:2])
                    for k4 in range(4):
                        kc = kh * 4 + k4
                        P.ts(crep[:, kc, :], ones32[:], csil[:, kc, m:m + 1], None, ALU.mult, r=[ones32, csil], w=[crep])
                        P.mm(pt[:, :], crep[:, kc, :], wrow[:, k4, :], start=(kc == 0), stop=(kc == 7),
                             r=[crep] + bigres[0:2], w=[pt])
                P.dma("sync", big[2][:, 0:512], bmodrow[:, c0:c0 + 512], w=[big[2]])
                P.tt(growb[m][:, half * 512:(half + 1) * 512], pt[:, :], big[2][:, 0:512], ALU.add,
                     r=[pt, big[2]], w=[growb[m]])
        P.dma("sync", lnr2[:], lnrow[:, 2 * k:2 * k + 2, :], w=[lnr2])
    make_gates(0)
    h2T = P.sb("h2T", [128, 8, TB], BF16)
    GT = P.sb("GT", [128, NTB, 65], F32)
    P.memset(GT[:], 1.0, w=[GT])
    acc = P.sb("acc", [128, NTB, D], F32)
    msub = P.mark()
    woutb = P.sb("woutb", [128, 8, D], BF16)
    for kc in range(8):
        P.dma("gpsimd", woutb[:, kc, :], wout_d[kc * 128:(kc + 1) * 128, :], w=[woutb])
    rw = P.sb("rw", [128, 8, 64], F32)
    P.dma("sync", rw[:], rw_d.rearrange("(k p) e -> p k e", p=128), w=[rw])
    rbias = P.sb("rbias", [128, 64], F32)
    P.dma("sync", rbias[:], rb_d[:, :], w=[rbias])

    yT = P.sb("yT", [128, 8, 128], BF16)
    h32 = P.sb("h32", [128, 8, 128], F32)
    st = P.sb("st", [128, 12], F32)
    mv = P.sb("mv", [128, 2], F32)
    rs = P.sb("rs", [128, 1], F32)
    sc = P.sb("sc", [128, 64], F32)
    bi = P.sb("bi", [128, 64], F32)
    m8 = P.sb("m8", [128, 8, 8], F32)
    gs = P.sb("gs", [128, 8], F32)
    gm = P.sb("gm", [128, 8], F32)
    t64 = P.sb("t64", [128, 64], F32)
    dsum = P.sb("dsum", [128, 1], F32)

    def layernorm(dst, src, srcres):
        P.call("vector", "bn_stats", st[:, 0:6], src[:, 0:512], r=srcres, w=[st])
        P.call("vector", "bn_stats", st[:, 6:12], src[:, 512:1024], r=srcres, w=[st])
        P.call("vector", "bn_aggr", mv[:], st[:], r=[st], w=[mv])
        P.act(rs[:], mv[:, 1:2], AF.Sqrt, r=[mv], w=[rs], bias=1e-6)
        P.call("vector", "reciprocal", rs[:], rs[:], r=[rs], w=[rs])
        P.ts(dst[:], src[:], mv[:, 0:1], rs[:, 0:1], ALU.subtract, ALU.mult, r=srcres + [mv, rs], w=[dst])

    for tt in range(NTB):
        m = 1 if tt == 0 else 0
        xr, ym, u, x1 = (big if tt % 2 == 0 else big2)
        xres_fn(tt, xr)
        ymx_fn(tt, ym, u)
        for half in range(2):
            pt = ps[half]
            for j4 in range(4):
                j = half * 4 + j4
                P.tr(pt[:, j4 * 128:(j4 + 1) * 128], ym[:, j * 128:(j + 1) * 128], ident[:], r=[ym, ident], w=[pt])
            P.cp(yT[:, half * 4:half * 4 + 4, :], pt[:, :].rearrange("p (j c) -> p j c", j=4), r=[pt], w=[yT])
        for half in range(2):
            pt = ps[2 + half]
            for kc in range(8):
                P.mm(pt[:, :], yT[:, kc, :], woutb[:, kc, half * 512:(half + 1) * 512], start=(kc == 0), stop=(kc == 7),
                     r=[yT, woutb], w=[pt])
            sl = slice(half * 512, (half + 1) * 512)
            P.tt(u[:, sl], pt[:, :], grow[m][0][:, sl], ALU.mult, r=[pt, grow[m][0]], w=[u])
            P.stt(u[:, sl], xr[:, sl], ALPHA, u[:, sl], ALU.mult, ALU.add, r=[xr, u], w=[u])
        layernorm(x1, u, [u])
        P.tt(x1[:], x1[:], lnr[:, 0, :], ALU.mult, r=[x1, lnr], w=[x1])
        P.tt(x1[:], x1[:], lnr[:, 1, :], ALU.add, r=[x1, lnr], w=[x1])
        P.dma("sync", x1d[tt * 128:(tt + 1) * 128, :], x1[:], r=[x1])
        layernorm(u, x1, [x1])
        for half in range(2):
            pt = ps[half]
            for j4 in range(4):
                j = half * 4 + j4
                P.tr(pt[:, j4 * 128:(j4 + 1) * 128], u[:, j * 128:(j + 1) * 128], ident[:], r=[u, ident], w=[pt])
            for j4 in range(4):
                j = half * 4 + j4
                src = pt[:, j4 * 128:(j4 + 1) * 128]
                if j4 > 0:
                    P.cp(h32[:, j, :], src, r=[pt], w=[h32])
                    src = h32[:, j, :]
                P.act(h32[:, j, :], src, AF.Identity, r=[pt, modc, h32], w=[h32],
                      scale=modc[:, 8 + j, m:m + 1], bias=modc[:, j, m:m + 1])
        P.cp(h2T[:, :, tt * 128:(tt + 1) * 128], h32[:], r=[h32], w=[h2T])
        pr = ps[4]
        for kc in range(8):
            P.mm(pr[:, 0:64], h32[:, kc, :], rw[:, kc, :], start=(kc == 0), stop=(kc == 7), r=[h32, rw], w=[pr])
        P.cp(sc[:], pr[:, 0:64], r=[pr], w=[sc])
        P.act(sc[:], sc[:], AF.Exp, r=[sc], w=[sc], scale=-1.0)
        P.ts(sc[:], sc[:], 1.0, None, ALU.add, r=[sc], w=[sc])
        P.call("vector", "reciprocal", sc[:], sc[:], r=[sc], w=[sc])
        P.tt(bi[:], sc[:], rbias[:], ALU.add, r=[sc, rbias], w=[bi])
        for g in range(8):
            P.call("vector", "max", m8[:, g, :], bi[:, g * 8:(g + 1) * 8], r=[bi], w=[m8])
        P.tt(gs[:], m8[:, :, 0], m8[:, :, 1], ALU.add, r=[m8], w=[gs])
        P.call("vector", "max", m8[:, 0, :], gs[:], r=[gs], w=[m8])
        P.ts(gm[:], gs[:], m8[:, 0, 3:4], None, ALU.is_ge, r=[gs, m8], w=[gm])
        b3 = bi[:].rearrange("p (g e) -> p g e", g=8)
        t3 = t64[:].rearrange("p (g e) -> p g e", g=8)
        P.tt(t3, b3, gm[:].unsqueeze(2).to_broadcast([128, 8, 8]), ALU.mult, r=[bi, gm], w=[t64])
        P.ts(gm[:], gm[:], 1.0, 1e9, ALU.subtract, ALU.mult, r=[gm], w=[gm])
        P.tt(t3, t3, gm[:].unsqueeze(2).to_broadcast([128, 8, 8]), ALU.add, r=[t64, gm], w=[t64])
        P.call("vector", "max", m8[:, 1, :], t64[:], r=[t64], w=[m8])
        P.ts(t64[:], t64[:], m8[:, 1, 7:8], None, ALU.is_ge, r=[t64, m8], w=[t64])
        P.tt(t64[:], t64[:], sc[:], ALU.mult, r=[t64, sc], w=[t64])
        P.call("vector", "tensor_reduce", dsum[:], t64[:], AX.X, ALU.add, r=[t64], w=[dsum])
        P.call("vector", "reciprocal", dsum[:], dsum[:], r=[dsum], w=[dsum])
        P.ts(GT[:, tt, 0:64], t64[:], dsum[:, 0:1], 2.5, ALU.mult, ALU.mult, r=[t64, dsum], w=[GT])

    P.release(msub)
    wg = [P.sb("wg%d" % i, [128, 8, 256], BF16) for i in range(2)]
    wu = [P.sb("wu%d" % i, [128, 8, 256], BF16) for i in range(2)]
    wd = [P.sb("wd%d" % i, [128, 2, D], BF16) for i in range(2)]
    sg = [P.sb("sg%d" % i, [128, 512], F32) for i in range(2)]
    aT = [P.sb("aT%d" % i, [128, 2, 512], BF16) for i in range(2)]
    ia = 0
    for e in range(65):
        g_, u_, d_ = wg[e % 2], wu[e % 2], wd[e % 2]
        P.dma("gpsimd", g_[:], eg_d[e].rearrange("(k p) f -> p k f", p=128), w=[g_])
        P.dma("gpsimd", u_[:], eu_d[e].rearrange("(k p) f -> p k f", p=128), w=[u_])
        P.dma("gpsimd", d_[:], ed_d[e].rearrange("(k p) f -> p k f", p=128), w=[d_])
        for (t0, w) in BBLOCKS:
            a_ = aT[ia % 2]
            ia += 1
            for fc in range(2):
                pg, pu = ps[fc * 2], ps[fc * 2 + 1]
                for kc in range(8):
                    P.mm(pg[:, 0:w], g_[:, kc, fc * 128:(fc + 1) * 128], h2T[:, kc, t0:t0 + w],
                         start=(kc == 0), stop=(kc == 7), r=[g_, h2T], w=[pg])
                for kc in range(8):
                    P.mm(pu[:, 0:w], u_[:, kc, fc * 128:(fc + 1) * 128], h2T[:, kc, t0:t0 + w],
                         start=(kc == 0), stop=(kc == 7), r=[u_, h2T], w=[pu])
                s_ = sg[fc]
                P.act(s_[:, 0:w], pg[:, 0:w], AF.Silu, r=[pg], w=[s_])
                P.tt(a_[:, fc, 0:w], pu[:, 0:w], s_[:, 0:w], ALU.mult, r=[pu, s_], w=[a_])
            for ti in range(w // 128):
                tt = t0 // 128 + ti
                for half in range(2):
                    pd = ps[4 + (ti * 2 + half) % 4]
                    for fc in range(2):
                        P.mm(pd[:, :], a_[:, fc, ti * 128:(ti + 1) * 128], d_[:, fc, half * 512:(half + 1) * 512],
                             start=(fc == 0), stop=(fc == 1), r=[a_, d_], w=[pd])
                    sl = slice(half * 512, (half + 1) * 512)
                    if e == 0:
                        P.ts(acc[:, tt, sl], pd[:, :], GT[:, tt, e:e + 1], None, ALU.mult, r=[pd, GT], w=[acc.sub(tt)])
                    else:
                        P.stt(acc[:, tt, sl], pd[:, :], GT[:, tt, e:e + 1], acc[:, tt, sl], ALU.mult, ALU.add,
                              r=[pd, GT, acc.sub(tt)], w=[acc.sub(tt)])

    make_gates(1)
    for tt in range(NTB):
        m = 1 if tt == 0 else 0
        x1, u, o_, _ = (big if tt % 2 == 0 else big2)
        P.dma("sync", x1[:], x1d[tt * 128:(tt + 1) * 128, :], w=[x1])
        P.tt(u[:], acc[:, tt, :], grow[m][1][:], ALU.mult, r=[acc.sub(tt), grow[m][1]], w=[u])
        P.stt(u[:], x1[:], ALPHA, u[:], ALU.mult, ALU.add, r=[x1, u], w=[u])
        layernorm(o_, u, [u])
        P.tt(o_[:], o_[:], lnr[:, 2, :], ALU.mult, r=[o_, lnr], w=[o_])
        P.tt(o_[:], o_[:], lnr[:, 3, :], ALU.add, r=[o_, lnr], w=[o_])
        oap, ores = out_fn(tt)
        P.dma("sync", oap, o_[:], r=[o_], w=ores)


def prep_B(inp, l, x_cur, xc_cur, ymix_full):
    maps = []
    ident = np.eye(128, dtype=np.float32)
    bm = inp["b_mod"][l][2048:6144]
    for core in range(8):
        b, hf = core // 2, core % 2
        rows_c = slice(128 * hf, 128 * hf + 128)
        rows_l = slice(2048 * hf, 2048 * hf + 2048)
        m = {}
        m["xres"] = np.ascontiguousarray(np.concatenate([xc_cur[b][rows_c], x_cur[b][rows_l]], 0))
        ym = ymix_full[b]
        m["ymx"] = np.ascontiguousarray(np.concatenate([ym[0:256][rows_c], ym[256:][rows_l]], 0))
        cc = np.stack([inp["c"][b], inp["c_ctx"]], -1)
        m["cT"] = np.ascontiguousarray(cc.reshape(8, 128, 2).transpose(1, 0, 2))
        m["wmodB"] = np.ascontiguousarray(inp["w_mod"][l][:, 2048:6144])
        m["bmodrow"] = np.ascontiguousarray(np.broadcast_to(bm[None], (128, 4096)))
        m["bmodcol"] = _col_layout(bm)
        m["wout"] = inp["w_out"][l]
        m["lnrow"] = np.ascontiguousarray(np.broadcast_to(
            np.stack([inp["ln1_w"][l], inp["ln1_b"][l], inp["ln2_w"][l], inp["ln2_b"][l]])[None], (128, 4, D)))
        m["rw"] = inp["router_w"][l]
        m["rbias"] = np.ascontiguousarray(np.broadcast_to(inp["router_bias"][l][None], (128, 64)))
        m["eg"] = np.concatenate([inp["exp_w_gate"][l], inp["sh_w_gate"][l][None]], 0)
        m["eu"] = np.concatenate([inp["exp_w_up"][l], inp["sh_w_up"][l][None]], 0)
        m["ed"] = np.concatenate([inp["exp_w_down"][l], inp["sh_w_down"][l][None]], 0)
        m["ident"] = ident
        maps.append(m)
    return maps


R1_OUT = ("WT0", "WT1", "NKK", "B0", "B1", "KD0", "KD1", "R", "V", "G", "BV")


def build_rwkv(nc, P, din, ps, hT, hres, winb, proj_fm, ident, identb, onesb, ymix, emit_ctx, dbg, xt, r1sink=None):
    mubc_d = din("mubc", [128, 768])
    pcol_d = din("pcol", [128, 8])
    wup_d = din("wup", [128, 128])
    aup_d = din("aup", [128, 128])
    gup_d = din("gup", [128, 128])
    bones_d = din("bones", [128, 128])
    outs = {} if r1sink is not None else {n: nc.dram_tensor("o_" + n, [128, T], F32, kind="ExternalOutput").ap() for n in R1_OUT}

    mubc = P.sb("mubc", [128, 768], F32)
    P.dma("sync", mubc[:], mubc_d[:, :], w=[mubc])
    cb = P.sb("cb", [128, 2, 768], F32)
    P.ts(cb[:, 0, :], mubc[:], 0.5, None, ALU.mult, r=[mubc], w=[cb])
    P.ts(cb[:, 1, :], mubc[:], -1.0, None, ALU.mult, r=[mubc], w=[cb])
    P.ts(cb[:, 1, :], cb[:, 1, :], 1.0, None, ALU.add, r=[cb], w=[cb])
    wc = P.sb("wcr", [128, 8, 3, 768], BF16)
    for kc in range(8):
        for j in range(3):
            P.tt(wc[:, kc, j, :], winb[:, kc, :], cb[:, j % 2, :], ALU.mult, r=[winb, cb], w=[wc])
    pcol = P.sb("pcol", [128, 8], F32)
    P.dma("sync", pcol[:], pcol_d[:, :], w=[pcol])
    npc = P.sb("npc", [128, 8], F32)
    P.ts(npc[:], pcol[:], -1.0, None, ALU.mult, r=[pcol], w=[npc])
    wup = P.sb("wup", [128, 128], F32)
    P.dma("sync", wup[:], wup_d[:, :], w=[wup])
    aup = P.sb("aup", [128, 128], F32)
    P.dma("sync", aup[:], aup_d[:, :], w=[aup])
    gup = P.sb("gup", [128, 128], F32)
    P.dma("sync", gup[:], gup_d[:, :], w=[gup])
    bones = P.sb("bones", [128, 128], F32)
    P.dma("sync", bones[:], bones_d[:, :], w=[bones])
    cm1 = P.sb("cm1", [128, 1], F32)
    P.memset(cm1[:], -1.0, w=[cm1])

    Zs = [[P.sb("Z%d" % g, [128, 512], F32) for g in range(6)] for k in range(2)]
    tAs = [[P.sb("tA%d" % i, [128, 512], F32) for i in range(2)] for k in range(2)]
    tKs = [[P.sb("tK%d" % i, [128, 512], F32) for i in range(2)] for k in range(2)]
    t0s = [P.sb("t0", [128, 512], F32) for k in range(2)]
    t1s = [P.sb("t1", [128, 512], F32) for k in range(2)]
    t2s = [P.sb("t2", [128, 512], F32) for k in range(2)]
    ob = [P.sb("ob%d" % i, [128, 512], F32) for i in range(3)]
    io = [0]

    def emit(name, src, t0, w, srcres):
        if r1sink is not None:
            r1sink(name, src, t0, w, srcres)
            return
        P.dma("sync", outs[name][:, t0:t0 + w], src, r=srcres)

    def sigmoid_from_psum(dst, pt, w, negbias):
        if negbias is None:
            P.act(dst[:, 0:w], pt[:, 0:w], AF.Exp, r=[pt], w=[dst], scale=-1.0)
        else:
            P.act(dst[:, 0:w], pt[:, 0:w], AF.Exp, r=[pt, npc], w=[dst], scale=-1.0, bias=negbias)
        P.ts(dst[:, 0:w], dst[:, 0:w], 1.0, None, ALU.add, r=[dst], w=[dst])
        P.call("vector", "reciprocal", dst[:, 0:w], dst[:, 0:w], r=[dst], w=[dst])

    for bi, (t0, w) in enumerate(QBLOCKS):
        Z, tA, tK = Zs[bi % 2], tAs[bi % 2], tKs[bi % 2]
        t0_, t1_, t2_ = t0s[bi % 2], t1s[bi % 2], t2s[bi % 2]
        seg0 = t0 in (0, NCTX)
        seg1 = (t0 + w) in (NCTX, T)
        for g in range(6):
            pt = ps[g % 4]
            first = True
            for j in (1, 0, 2):
                a = 1 if (j == 0 and seg0) else 0
                b = 1 if (j == 2 and seg1) else 0
                for kc in range(8):
                    last = (j == 2 and kc == 7)
                    P.mm(pt[:, a:w - b], wc[:, kc, j, g * 128:(g + 1) * 128],
                         hT[:, kc, t0 + a + j - 1:t0 + w - b + j - 1],
                         start=first, stop=last, r=[wc] + hres, w=[pt], skip=True)
                    first = False
            P.cp(Z[g][:, 0:w], pt[:, 0:w], r=[pt], w=[Z[g]])
        zr, zk, zv, zw, za, zg = Z
        emit("R", zr[:, 0:w], t0, w, [zr])
        emit("V", zv[:, 0:w], t0, w, [zv])
        P.act(zw[:, 0:w], zw[:, 0:w], AF.Tanh, r=[zw], w=[zw])
        for d in range(2):
            pt = ps[4 + d]
            rows = slice(d * 64, d * 64 + 64)
            P.mm(pt[:, 0:w], wup[rows, :], zw[rows, 0:w], r=[wup, zw], w=[pt])
            o_ = ob[io[0] % 3]; io[0] += 1
            sigmoid_from_psum(o_, pt, w, npc[:, d:d + 1])
            P.act(o_[:, 0:w], o_[:, 0:w], AF.Exp, r=[o_], w=[o_], scale=-float(np.exp(-0.5)))
            emit("WT%d" % d, o_[:, 0:w], t0, w, [o_])
        for d in range(2):
            pt = ps[6 + d]
            rows = slice(d * 64, d * 64 + 64)
            P.mm(pt[:, 0:w], aup[rows, :], za[rows, 0:w], r=[aup, za], w=[pt])
            sigmoid_from_psum(tA[d], pt, w, npc[:, 2 + d:3 + d])
        sigmoid_from_psum(t0_, zg, w, None) if False else None
        P.act(t0_[:, 0:w], zg[:, 0:w], AF.Exp, r=[zg], w=[t0_], scale=-1.0)
        P.ts(t0_[:, 0:w], t0_[:, 0:w], 1.0, None, ALU.add, r=[t0_], w=[t0_])
        P.call("vector", "reciprocal", t0_[:, 0:w], t0_[:, 0:w], r=[t0_], w=[t0_])
        pt = ps[4]
        P.mm(pt[:, 0:w], gup[:], t0_[:, 0:w], r=[gup, t0_], w=[pt])
        o_ = ob[io[0] % 3]; io[0] += 1
        P.cp(o_[:, 0:w], pt[:, 0:w], r=[pt], w=[o_])
        emit("G", o_[:, 0:w], t0, w, [o_])
        P.ts(t1_[:, 0:w], zk[:, 0:w], pcol[:, 4:5], None, ALU.mult, r=[zk, pcol], w=[t1_])
        P.tt(t2_[:, 0:w], t1_[:, 0:w], t1_[:, 0:w], ALU.mult, r=[t1_], w=[t2_])
        pt = ps[5]
        P.mm(pt[:, 0:w], bones[:], t2_[:, 0:w], r=[bones, t2_], w=[pt])
        P.ts(t2_[:, 0:w], pt[:, 0:w], 1e-12, None, ALU.max, r=[pt], w=[t2_])
        P.act(t2_[:, 0:w], t2_[:, 0:w], AF.Sqrt, r=[t2_], w=[t2_])
        P.call("vector", "reciprocal", t2_[:, 0:w], t2_[:, 0:w], r=[t2_], w=[t2_])
        P.tt(t1_[:, 0:w], t1_[:, 0:w], t2_[:, 0:w], ALU.mult, r=[t1_, t2_], w=[t1_])
        o_ = ob[io[0] % 3]; io[0] += 1
        P.ts(o_[:, 0:w], t1_[:, 0:w], -1.0, None, ALU.mult, r=[t1_], w=[o_])
        emit("NKK", o_[:, 0:w], t0, w, [o_])
        for d in range(2):
            o_ = ob[io[0] % 3]; io[0] += 1
            P.tt(o_[:, 0:w], t1_[:, 0:w], tA[d][:, 0:w], ALU.mult, r=[t1_, tA[d]], w=[o_])
            emit("B%d" % d, o_[:, 0:w], t0, w, [o_])
            P.ts(tK[d][:, 0:w], tA[d][:, 0:w], cm1[:, 0:1], pcol[:, 5:6], ALU.add, ALU.mult, r=[tA[d], cm1, pcol], w=[tK[d]])
            P.stt(tK[d][:, 0:w], tK[d][:, 0:w], 1.0, zk[:, 0:w], ALU.add, ALU.mult, r=[tK[d], zk], w=[tK[d]])
            emit("KD%d" % d, tK[d][:, 0:w], t0, w, [tK[d]])
        P.tt(t2_[:, 0:w], tK[0][:, 0:w], tK[1][:, 0:w], ALU.add, r=tK, w=[t2_])
        P.stt(t2_[:, 0:w], zr[:, 0:w], pcol[:, 6:7], t2_[:, 0:w], ALU.mult, ALU.mult, r=[zr, pcol, t2_], w=[t2_])
        pt = ps[6]
        P.mm(pt[:, 0:w], bones[:], t2_[:, 0:w], r=[bones, t2_], w=[pt])
        o_ = ob[io[0] % 3]; io[0] += 1
        P.tt(o_[:, 0:w], pt[:, 0:w], zv[:, 0:w], ALU.mult, r=[pt, zv], w=[o_])
        emit("BV", o_[:, 0:w], t0, w, [o_])


def build_R2():
    nc = bass.Bass("TRN2", target_bir_lowering=False)
    P = Prog(nc)

    def din(name, shape, dt=F32):
        return nc.dram_tensor(name, list(shape), dt, kind="ExternalInput").ap()
    I = []
    for d in range(2):
        I.append(dict(nk2=din("nk2_%d" % d, [T, 2, 128]), bt=din("bt_%d" % d, [T, 128]),
                      kd2=din("kd2_%d" % d, [T, 2, 128]), vt=din("vt_%d" % d, [T, 128]),
                      wt=din("wt_%d" % d, [128, T]), rbd=din("rbd_%d" % d, [128, T, 2])))
    TS = 64
    mask_d = din("mask32", [128, 32])
    O = [nc.dram_tensor("O_%d" % d, [64, T, 2], F32, kind="ExternalOutput").ap() for d in range(2)]

    mask32 = P.sb("mask32", [128, 32], F32)
    P.dma("sync", mask32[:], mask_d[:, :], w=[mask32])
    S = [P.sb("S%d" % d, [128, 64], F32) for d in range(2)]
    Sb = [[P.sb("Sb%d%d" % (d, k), [128, 64], BF16) for k in range(2)] for d in range(2)]
    for d in range(2):
        P.memset(S[d][:], 0.0, w=[S[d]])
        P.memset(Sb[d][1][:], 0.0, w=[Sb[d][1]])
    TB_ = [[dict(nk2f=P.sb("nk2f_%d%d" % (d, k), [TS, 2, 128], F32), bt=P.sb("bt_%d%d" % (d, k), [TS, 128], F32),
                 kd2f=P.sb("kd2f_%d%d" % (d, k), [TS, 2, 128], F32), vt=P.sb("vt_%d%d" % (d, k), [TS, 128], F32),
                 rbdf=P.sb("rbdf_%d%d" % (d, k), [128, TS, 2], F32),
                 nk2=P.sb("nk2_%d%d" % (d, k), [TS, 2, 128], BF16), kd2=P.sb("kd2_%d%d" % (d, k), [TS, 2, 128], BF16),
                 wt=P.sb("wt_%d%d" % (d, k), [128, TS], F32), rbd=P.sb("rbd_%d%d" % (d, k), [128, TS, 2], BF16),
                 Bd=P.sb("Bd_%d%d" % (d, k), [TS, 32, 128], BF16), Vd=P.sb("Vd_%d%d" % (d, k), [TS, 32, 128], BF16))
            for k in range(2)] for d in range(2)]
    LS = [[P.sb("LS%d%d" % (d, k), [128, 128], BF16) for k in range(4)] for d in range(2)]
    osb = [P.sb("osb%d" % d, [64, 2 * TS], F32) for d in range(2)]
    LP = [[P.ps("LP%d%d" % (d, k), [128, 512]) for k in range(2)] for d in range(2)]
    SP = [P.ps("SP%d" % d, [128, 512]) for d in range(2)]
    OP = [P.ps("OP%d" % d, [128, 512]) for d in range(2)]

    def load(i):
        for d in range(2):
            tb = TB_[d][i % 2]
            r0 = i * TS
            P.dma("sync", tb["nk2f"][:], I[d]["nk2"][r0:r0 + TS, :, :], w=[tb["nk2f"]])
            P.dma("sync", tb["bt"][:], I[d]["bt"][r0:r0 + TS, :], w=[tb["bt"]])
            P.dma("sync", tb["kd2f"][:], I[d]["kd2"][r0:r0 + TS, :, :], w=[tb["kd2f"]])
            P.dma("sync", tb["vt"][:], I[d]["vt"][r0:r0 + TS, :], w=[tb["vt"]])
            P.dma("sync", tb["wt"][:], I[d]["wt"][:, r0:r0 + TS], w=[tb["wt"]])
            P.dma("sync", tb["rbdf"][:], I[d]["rbd"][:, r0:r0 + TS, :], w=[tb["rbdf"]])
            P.cp(tb["nk2"][:], tb["nk2f"][:], r=[tb["nk2f"]], w=[tb["nk2"]], eng="gpsimd")
            P.cp(tb["kd2"][:], tb["kd2f"][:], r=[tb["kd2f"]], w=[tb["kd2"]], eng="gpsimd")
            P.cp(tb["rbd"][:], tb["rbdf"][:], r=[tb["rbdf"]], w=[tb["rbd"]], eng="gpsimd")
            mb = mask32[0:TS, :].unsqueeze(2).to_broadcast([TS, 32, 128])
            P.tt(tb["Bd"][:], mb, tb["bt"][:].unsqueeze(1).to_broadcast([TS, 32, 128]), ALU.mult,
                 r=[mask32, tb["bt"]], w=[tb["Bd"]])
            P.tt(tb["Vd"][:], mb, tb["vt"][:].unsqueeze(1).to_broadcast([TS, 32, 128]), ALU.mult,
                 r=[mask32, tb["vt"]], w=[tb["Vd"]])

    def lgen(g):
        i, t = g // TS, g % TS
        rows = slice((t // 32) * 32, (t // 32) * 32 + 32)
        tl = t % 32
        for d in range(2):
            tb = TB_[d][i % 2]
            lp = LP[d][g % 2]
            ls = LS[d][g % 4]
            P.mm(lp[:, 0:64], tb["nk2"][rows, 0, :], tb["Bd"][rows, tl, 0:64], r=[tb["nk2"], tb["Bd"]], w=[lp])
            P.mm(lp[:, 64:128], tb["nk2"][rows, 1, :], tb["Bd"][rows, tl, 64:128], r=[tb["nk2"], tb["Bd"]], w=[lp])
            P.cp(ls[:], lp[:, 0:128], r=[lp], w=[ls], eng="scalar")

    def step(g):
        i, t = g // TS, g % TS
        rows = slice((t // 32) * 32, (t // 32) * 32 + 32)
        tl = t % 32
        for d in range(2):
            tb = TB_[d][i % 2]
            ls = LS[d][g % 4]
            sp = SP[d]
            sbo = Sb[d][(g + 1) % 2]
            P.mm(sp[:, 0:64], ls[:], sbo[:], start=True, stop=False, r=[ls, sbo], w=[sp])
            P.mm(sp[:, 0:64], tb["kd2"][rows, 0, :], tb["Vd"][rows, tl, 0:64], start=False, stop=False,
                 r=[tb["kd2"], tb["Vd"]], w=[sp])
            P.mm(sp[:, 0:64], tb["kd2"][rows, 1, :], tb["Vd"][rows, tl, 64:128], start=False, stop=True,
                 r=[tb["kd2"], tb["Vd"]], w=[sp])
        for d in range(2):
            tb = TB_[d][i % 2]
            sbn = Sb[d][g % 2]
            P.stt(sbn[:], S[d][:], tb["wt"][:, t:t + 1], SP[d][:, 0:64], ALU.mult, ALU.add,
                  r=[S[d], tb["wt"], SP[d]], w=[sbn])
        for d in range(2):
            tb = TB_[d][i % 2]
            P.mm(OP[d][0:64, 2 * t:2 * t + 2], Sb[d][g % 2][:], tb["rbd"][:, t, :], r=[Sb[d][g % 2], tb["rbd"]], w=[OP[d]])
        for d in range(2):
            tb = TB_[d][i % 2]
            P.stt(S[d][:], S[d][:], tb["wt"][:, t:t + 1], SP[d][:, 0:64], ALU.mult, ALU.add,
                  r=[S[d], tb["wt"], SP[d]], w=[S[d]])

    LA = 2
    load(0)
    for g in range(LA):
        lgen(g)
    for g in range(T):
        if g % TS == 0 and g // TS + 1 < T // TS:
            load(g // TS + 1)
        if g + LA < T:
            lgen(g + LA)
        step(g)
        if g % TS == TS - 1:
            i = g // TS
            for d in range(2):
                P.cp(osb[d][:], OP[d][0:64, 0:2 * TS], r=[OP[d]], w=[osb[d]])
                P.dma("sync", O[d][:, i * TS:(i + 1) * TS, :], osb[d][:].rearrange("p (t h) -> p t h", h=2), r=[osb[d]])
    P.emit()
    return nc


def build_R3():
    nc = bass.Bass("TRN2", target_bir_lowering=False)
    P = Prog(nc)

    def din(name, shape, dt=F32):
        return nc.dram_tensor(name, list(shape), dt, kind="ExternalInput").ap()
    of_d = din("of", [T, 128])
    ob_d = din("ob", [T, 128])
    g_d = din("g", [T, 128])
    bv_d = din("bv", [T, 128])
    ln_d = din("lnrow", [128, 2, 128])
    y_d = nc.dram_tensor("ymix", [T, 128], F32, kind="ExternalOutput").ap()
    ln = P.sb("ln", [128, 2, 128], F32)
    P.dma("sync", ln[:], ln_d[:, :, :], w=[ln])
    bufs = [[P.sb("r3_%d%d" % (k, i), [128, 128], F32) for i in range(4)] for k in range(2)]
    st = P.sb("st", [128, 2, 6], F32)
    mv = P.sb("mv", [128, 2, 2], F32)
    for tt in range(NT):
        of, ob, g, bv = bufs[tt % 2]
        r0 = tt * 128
        P.dma("sync", of[:], of_d[r0:r0 + 128, :], w=[of])
        P.dma("sync", ob[:], ob_d[r0:r0 + 128, :], w=[ob])
        P.dma("sync", g[:], g_d[r0:r0 + 128, :], w=[g])
        P.dma("sync", bv[:], bv_d[r0:r0 + 128, :], w=[bv])
        P.tt(of[:], of[:], ob[:], ALU.add, r=[of, ob], w=[of])
        for h in range(2):
            P.call("vector", "bn_stats", st[:, h, :], of[:, h * 64:(h + 1) * 64], r=[of], w=[st])
            P.call("vector", "bn_aggr", mv[:, h, :], st[:, h, :], r=[st], w=[mv])
        P.act(mv[:, :, 1:2], mv[:, :, 1:2], AF.Sqrt, r=[mv], w=[mv], bias=64e-5)
        P.call("vector", "reciprocal", mv[:, :, 1:2], mv[:, :, 1:2], r=[mv], w=[mv])
        for h in range(2):
            sl = slice(h * 64, (h + 1) * 64)
            P.ts(of[:, sl], of[:, sl], mv[:, h, 0:1], mv[:, h, 1:2], ALU.subtract, ALU.mult, r=[of, mv], w=[of])
        P.tt(of[:], of[:], ln[:, 0, :], ALU.mult, r=[of, ln], w=[of])
        P.tt(of[:], of[:], ln[:, 1, :], ALU.add, r=[of, ln], w=[of])
        P.tt(of[:], of[:], bv[:], ALU.add, r=[of, bv], w=[of])
        P.tt(of[:], of[:], g[:], ALU.mult, r=[of, g], w=[of])
        P.dma("sync", y_d[r0:r0 + 128, :], of[:], r=[of])
    P.emit()
    return nc


_PERM_B = np.concatenate([np.arange(255, -1, -1), np.arange(T - 1, 255, -1)])


def prep_R2(r1res):
    maps = []
    mask32 = (np.arange(128)[:, None] % 32 == np.arange(32)[None, :]).astype(np.float32)
    for core in range(8):
        o = r1res[core]
        m = {"mask32": mask32}
        for d in range(2):
            perm = np.arange(T) if d == 0 else _PERM_B

            def tm(a):
                return np.ascontiguousarray(a.T[perm])

            def mask2(a_tm):
                out = np.zeros((T, 2, 128), np.float32)
                out[:, 0, 0:64] = a_tm[:, 0:64]
                out[:, 1, 64:128] = a_tm[:, 64:128]
                return out
            m["nk2_%d" % d] = mask2(tm(o["o_NKK"]))
            m["bt_%d" % d] = tm(o["o_B%d" % d])
            m["kd2_%d" % d] = mask2(tm(o["o_KD%d" % d]))
            m["vt_%d" % d] = tm(o["o_V"])
            m["wt_%d" % d] = np.ascontiguousarray(o["o_WT%d" % d][:, perm])
            r = o["o_R"][:, perm]
            rbd = np.zeros((128, T, 2), np.float32)
            rbd[0:64, :, 0] = r[0:64]
            rbd[64:128, :, 1] = r[64:128]
            m["rbd_%d" % d] = rbd
        maps.append(m)
    return maps


def prep_R3(inp, l, r1res, r2res):
    maps = []
    inv = np.argsort(_PERM_B)
    for core in range(8):
        hp = core % 2
        hc = slice(128 * hp, 128 * hp + 128)
        o0 = r2res[core]["O_0"]
        o1 = r2res[core]["O_1"][:, inv, :]
        m = {}
        m["of"] = np.ascontiguousarray(o0.transpose(1, 2, 0).reshape(T, 128))
        m["ob"] = np.ascontiguousarray(o1.transpose(1, 2, 0).reshape(T, 128))
        m["g"] = np.ascontiguousarray(r1res[core]["o_G"].T)
        m["bv"] = np.ascontiguousarray(r1res[core]["o_BV"].T)
        m["lnrow"] = np.ascontiguousarray(np.broadcast_to(
            np.stack([inp["rwkv_ln_w"][l][hc], inp["rwkv_ln_b"][l][hc]])[None], (128, 2, 128)))
        maps.append(m)
    return maps


def run_rwkv(inp, l, x, xc, emit_ctx=True):
    cores = list(range(8))
    r1 = run_bass_kernel_spmd(build_A("rwkv", emit_ctx), prep_A(inp, l, x, xc, "rwkv"), core_ids=cores).results
    r2 = run_bass_kernel_spmd(build_R2(), prep_R2(r1), core_ids=cores).results
    r3 = run_bass_kernel_spmd(build_R3(), prep_R3(inp, l, r1, r2), core_ids=cores).results
    return [r3[c]["ymix"] for c in cores]


def _kernel_unfused(inp, nlayers=2):
    inp = {k: np.asarray(v) for k, v in inp.items()}
    x = inp["x"].astype(np.float32)
    xc = inp["ctx"].astype(np.float32)
    cores = list(range(8))
    for l in range(nlayers):
        emit_ctx = (l == 0)
        ymix = np.zeros((4, T, 1024), np.float32)
        for kind, c0, W in (("mlstm", 0, 128), ("mla", 512, 256)):
            nc = build_A(kind, emit_ctx)
            res = run_bass_kernel_spmd(nc, prep_A(inp, l, x, xc, kind), core_ids=cores)
            for core in cores:
                b, hp = core // 2, core % 2
                ymix[b][:, c0 + hp * W:c0 + (hp + 1) * W] = res.results[core]["ymix"]
        yr = run_rwkv(inp, l, x, xc, emit_ctx)
        for core in cores:
            b, hp = core // 2, core % 2
            ymix[b][:, 256 + hp * 128:256 + (hp + 1) * 128] = yr[core]
        resB = run_bass_kernel_spmd(build_B(), prep_B(inp, l, x, xc, ymix), core_ids=cores)
        x_new = np.empty_like(x)
        xc_new = np.empty_like(xc)
        for core in cores:
            b, hf = core // 2, core % 2
            o = resB.results[core]["xout"]
            xc_new[b][128 * hf:128 * hf + 128] = o[0:128]
            x_new[b][2048 * hf:2048 * hf + 2048] = o[128:]
        x, xc = x_new, xc_new
    return x.astype(np.float32)


GROUPS = [[0, 1], [2, 3], [4, 5], [6, 7]]
DIAG_ENG = "gpsimd"
TM_NAMES = ("NKK", "B0", "B1", "KD0", "KD1", "V", "G", "BV")


def _half_rows(tt):
    if tt < 2:
        return tt, 0
    k = tt - 2
    return k // 16, 128 + (k % 16) * 128


def fused_rwkv_scan(nc, P, ps, ident, scr, sres, ymix, lnrow_d, emit_ctx):
    TS = 32
    m0 = P.mark()
    mask32 = P.sb("mask32", [64, 32], F32)
    for q in range(2):
        P.cp(mask32[q * 32:(q + 1) * 32, :], ident[q * 32:(q + 1) * 32, q * 32:(q + 1) * 32], r=[ident], w=[mask32])
    S = [P.sb("S%d" % d, [128, 64], F32) for d in range(2)]
    Sb = [[P.sb("Sb%d%d" % (d, k), [128, 64], BF16) for k in range(2)] for d in range(2)]
    for d in range(2):
        P.memset(S[d][:], 0.0, w=[S[d]])
        P.memset(Sb[d][1][:], 0.0, w=[Sb[d][1]])
    TB_ = [[dict(nkf=P.sb("nkf", [64, 128], F32), kdf=P.sb("kdf", [64, 128], F32),
                 btf=P.sb("btf", [64, 128], F32), vtf=P.sb("vtf", [64, 64], F32),
                 rf=P.sb("rf", [128, TS], F32), wt=P.sb("wt", [128, TS], F32),
                 nk=P.sb("nk", [64, 128], BF16), kd=P.sb("kd", [64, 128], BF16),
                 rbd=P.sb("rbd", [128, TS, 2], BF16),
                 Bd=P.sb("Bd", [64, 32, 128], BF16), Vd=P.sb("Vd", [64, 32, 64], BF16))
            for k in range(2)] for d in range(2)]
    for d in range(2):
        for k in range(2):
            tb = TB_[d][k]
            for n in ("nkf", "kdf", "btf"):
                P.memset(tb[n][:], 0.0, w=[tb[n]])
            P.memset(tb["rbd"][:], 0.0, w=[tb["rbd"]])
    LS = [[P.sb("LS", [128, 512], BF16) for k in range(3)] for d in range(2)]
    osb = [P.sb("osb", [64, 2 * TS], F32) for d in range(2)]
    LP = [[ps[0], ps[1]], [ps[2], ps[3]]]
    SP = [ps[4], ps[5]]
    OP = [ps[6], ps[7]]
    perm_b = _PERM_B

    def lo_of(d, i):
        return i * TS if d == 0 else int(perm_b[i * TS + TS - 1])

    def loc(d, t):
        return t if d == 0 else TS - 1 - t

    def load(i):
        for d in range(2):
            tb = TB_[d][i % 2]
            lo = lo_of(d, i)
            for (dst, name) in (("nkf", "NKK"), ("kdf", "KD%d" % d), ("btf", "B%d" % d)):
                for h in range(2):
                    P.dma("sync", tb[dst][h * 32:(h + 1) * 32, h * 64:(h + 1) * 64],
                          scr[name][lo:lo + TS, h * 64:(h + 1) * 64], r=[sres[name]], w=[tb[dst]])
            for h in range(2):
                P.dma("sync", tb["vtf"][h * 32:(h + 1) * 32, :], scr["V"][lo:lo + TS, h * 64:(h + 1) * 64],
                      r=[sres["V"]], w=[tb["vtf"]])
            P.dma("sync", tb["wt"][:], scr["WT%d" % d][:, lo:lo + TS], r=[sres["WT%d" % d]], w=[tb["wt"]])
            P.dma("sync", tb["rf"][:], scr["R"][:, lo:lo + TS], r=[sres["R"]], w=[tb["rf"]])
            P.cp(tb["nk"][:], tb["nkf"][:], r=[tb["nkf"]], w=[tb["nk"]], eng="gpsimd")
            P.cp(tb["kd"][:], tb["kdf"][:], r=[tb["kdf"]], w=[tb["kd"]], eng="gpsimd")
            P.cp(tb["rbd"][0:64, :, 0], tb["rf"][0:64, :], r=[tb["rf"]], w=[tb["rbd"]], eng="gpsimd")
            P.cp(tb["rbd"][64:128, :, 1], tb["rf"][64:128, :], r=[tb["rf"]], w=[tb["rbd"]], eng="gpsimd")
            P.tt(tb["Bd"][:], mask32[:].unsqueeze(2).to_broadcast([64, 32, 128]),
                 tb["btf"][:].unsqueeze(1).to_broadcast([64, 32, 128]), ALU.mult,
                 r=[mask32, tb["btf"]], w=[tb["Bd"]], eng=DIAG_ENG)
            P.tt(tb["Vd"][:], mask32[:].unsqueeze(2).to_broadcast([64, 32, 64]),
                 tb["vtf"][:].unsqueeze(1).to_broadcast([64, 32, 64]), ALU.mult,
                 r=[mask32, tb["vtf"]], w=[tb["Vd"]], eng=DIAG_ENG)

    def lgen(q):
        g0 = 4 * q
        i, t0 = g0 // TS, g0 % TS
        for d in range(2):
            b0 = t0 if d == 0 else TS - 4 - t0
            tb = TB_[d][i % 2]
            lp = LP[d][q % 2]
            ls = LS[d][q % 3]
            P.mm(lp[:, 0:512], tb["nk"][:], tb["Bd"][:, b0:b0 + 4, :], r=[tb["nk"], tb["Bd"]], w=[lp])
            P.cp(ls[:], lp[:, 0:512], r=[lp], w=[ls], eng="scalar")

    def mm_step(g):
        i, t = g // TS, g % TS
        for d in range(2):
            tl = loc(d, t)
            tb = TB_[d][i % 2]
            P.mm(SP[d][:, 0:64], tb["kd"][:], tb["Vd"][:, tl, :], start=True, stop=False,
                 r=[tb["kd"], tb["Vd"]], w=[SP[d]])
        for d in range(2):
            ls = LS[d][(g // 4) % 3]
            j = (g % 4) if d == 0 else 3 - (g % 4)
            sbo = Sb[d][(g + 1) % 2]
            P.mm(SP[d][:, 0:64], ls[:, j * 128:(j + 1) * 128], sbo[:], start=False, stop=True, r=[ls, sbo], w=[SP[d]])

    def out_step(g):
        i, t = g // TS, g % TS
        for d in range(2):
            tb = TB_[d][i % 2]
            tl = loc(d, t)
            P.mm(OP[d][0:64, 2 * tl:2 * tl + 2], Sb[d][g % 2][:], tb["rbd"][:, tl, :],
                 r=[Sb[d][g % 2], tb["rbd"]], w=[OP[d]])

    def dve_step(g):
        i, t = g // TS, g % TS
        for d in range(2):
            tb = TB_[d][i % 2]
            tl = loc(d, t)
            P.stt(Sb[d][g % 2][:], S[d][:], tb["wt"][:, tl:tl + 1], SP[d][:, 0:64], ALU.mult, ALU.add,
                  r=[S[d], tb["wt"], SP[d]], w=[Sb[d][g % 2]])
        for d in range(2):
            tb = TB_[d][i % 2]
            tl = loc(d, t)
            P.stt(S[d][:], S[d][:], tb["wt"][:, tl:tl + 1], SP[d][:, 0:64], ALU.mult, ALU.add,
                  r=[S[d], tb["wt"], SP[d]], w=[S[d]])

    def evac(i):
        for d in range(2):
            lo = lo_of(d, i)
            P.cp(osb[d][:], OP[d][0:64, 0:2 * TS], r=[OP[d]], w=[osb[d]])
            P.dma("sync", scr["O%d" % d][:, lo:lo + TS, :], osb[d][:].rearrange("p (t h) -> p t h", h=2),
                  r=[osb[d]], w=[sres["O%d" % d]])

    load(0)
    lgen(0)
    for g in range(T):
        flushed = False
        if g % TS == 0:
            if g > 0:
                out_step(g - 1)
                evac(g // TS - 1)
                flushed = True
            if g // TS + 1 < T // TS:
                load(g // TS + 1)
        if g % 4 == 0 and g + 4 < T:
            lgen(g // 4 + 1)
        mm_step(g)
        if g > 0 and not flushed:
            out_step(g - 1)
        dve_step(g)
    out_step(T - 1)
    evac(T // TS - 1)
    P.release(m0)
    ln = P.sb("ln", [128, 2, 128], F32)
    P.dma("sync", ln[:], lnrow_d[:, :, :], w=[ln])
    o3 = [[P.sb("o3", [64, 128, 2], F32) for i in range(2)] for k in range(2)]
    gb = [[P.sb("gb", [128, 128], F32) for i in range(2)] for k in range(2)]
    ot = [P.sb("ot", [128, 128], F32) for k in range(2)]
    st = P.sb("st", [128, 2, 6], F32)
    mv = P.sb("mv", [128, 2, 2], F32)
    for tt in range(NT):
        if tt < 2 and not emit_ctx:
            continue
        r0 = tt * 128
        of, ob = o3[tt % 2]
        g_, bv = gb[tt % 2]
        o_ = ot[tt % 2]
        P.dma("sync", of[:], scr["O0"][:, r0:r0 + 128, :], r=[sres["O0"]], w=[of])
        P.dma("sync", ob[:], scr["O1"][:, r0:r0 + 128, :], r=[sres["O1"]], w=[ob])
        P.dma("sync", g_[:], scr["G"][r0:r0 + 128, :], r=[sres["G"]], w=[g_])
        P.dma("sync", bv[:], scr["BV"][r0:r0 + 128, :], r=[sres["BV"]], w=[bv])
        P.tt(of[:], of[:], ob[:], ALU.add, r=[of, ob], w=[of])
        pt = ps[tt % 2]
        for h in range(2):
            P.tr(pt[:, h * 64:(h + 1) * 64], of[:, :, h], ident[0:64, 0:64], r=[of, ident], w=[pt])
        P.cp(o_[:], pt[:, 0:128], r=[pt], w=[o_])
        for h in range(2):
            P.call("vector", "bn_stats", st[:, h, :], o_[:, h * 64:(h + 1) * 64], r=[o_], w=[st])
            P.call("vector", "bn_aggr", mv[:, h, :], st[:, h, :], r=[st], w=[mv])
        P.act(mv[:, :, 1:2], mv[:, :, 1:2], AF.Sqrt, r=[mv], w=[mv], bias=64e-5)
        P.call("vector", "reciprocal", mv[:, :, 1:2], mv[:, :, 1:2], r=[mv], w=[mv])
        for h in range(2):
            sl = slice(h * 64, (h + 1) * 64)
            P.ts(o_[:, sl], o_[:, sl], mv[:, h, 0:1], mv[:, h, 1:2], ALU.subtract, ALU.mult, r=[o_, mv], w=[o_])
        P.tt(o_[:], o_[:], ln[:, 0, :], ALU.mult, r=[o_, ln], w=[o_])
        P.tt(o_[:], o_[:], ln[:, 1, :], ALU.add, r=[o_, ln], w=[o_])
        P.tt(o_[:], o_[:], bv[:], ALU.add, r=[o_, bv], w=[o_])
        P.tt(o_[:], o_[:], g_[:], ALU.mult, r=[o_, g_], w=[o_])
        P.dma("sync", ymix[r0:r0 + 128, :], o_[:], r=[o_])
    P.release(m0)


class _Din:
    def __init__(self, nc, pre, shared):
        self.nc, self.pre, self._shared = nc, pre, shared

    def __call__(self, name, shape, dt=F32):
        return self.nc.dram_tensor(self.pre + name, list(shape), dt, kind="ExternalInput").ap()

    def shared(self, name, shape, dt=F32):
        if name not in self._shared:
            self._shared[name] = self.nc.dram_tensor(name, list(shape), dt, kind="ExternalInput").ap()
        return self._shared[name]


def build_fused(nlayers=2):
    nc = bass.Bass("TRN2", target_bir_lowering=False)
    P = Prog(nc)
    P.scoped = True
    shared = {}
    d0 = _Din(nc, "", shared)
    xin0 = d0.shared("xin", [T, D])
    xres0 = d0.shared("xres0", [TB, D])
    sel_d = d0.shared("sel", [128, 2])
    ident_d = d0.shared("ident", [128, 128])
    out = nc.dram_tensor("xout", [TB, D], F32, kind="ExternalOutput").ap()

    ident = P.sb("ident", [128, 128], F32)
    P.dma("sync", ident[:], ident_d[:, :], w=[ident])
    identb = P.sb("identb", [128, 128], BF16)
    P.cp(identb[:], ident[:], r=[ident], w=[identb])
    onesb = P.sb("onesb", [128, 128], BF16)
    P.memset(onesb[:], 1.0, w=[onesb])
    ones32 = P.sb("ones32", [128, 128], F32)
    P.memset(ones32[:], 1.0, w=[ones32])
    sel = P.sb("sel", [128, 2], F32)
    P.dma("sync", sel[:], sel_d[:, :], w=[sel])
    ps = [P.ps("ps%d" % i, [128, 512]) for i in range(8)]

    XG = None
    XHp = None
    xg_res = Res()
    for l in range(nlayers):
        pre = "L%d_" % l
        last = (l == nlayers - 1)
        emit_ctx = not last
        din = _Din(nc, pre, shared)
        YH = nc.dram_tensor(pre + "YH", [T, 512], F32).ap()
        ych = [(0, 1024), (1024, 1024), (2048, 1024), (3072, 1024), (4096, 256)]
        YG = [nc.dram_tensor(pre + "YG%d" % k, [2 * n_, 512], F32).ap() for k, (s_, n_) in enumerate(ych)]
        yg_res = Res()
        mL = P.mark()
        if l == 0:
            def xrow_fn(tt):
                return xin0[tt * 128:(tt + 1) * 128, :], []
        else:
            def xrow_fn(tt, XG=XG):
                j, lo = _half_rows(tt)
                k, off = lo // 512, lo % 512
                n_ = 512 if k < 4 else 128
                return XG[k][j * n_ + off:j * n_ + off + 128, :], [xg_res]
        cT = din.shared("cT", [128, 8, 2])
        wmodA = din("wmodA", [D, 2048])
        bmodA = din("bmodA", [128, 16])
        hT, hres, xt = phase_ln(nc, P, ps, ident, xrow_fn, cT, wmodA, bmodA, None)
        for kind, c0 in (("mlstm", 0), ("rwkv", 128), ("mla", 256)):
            mk = P.mark()
            winA = din("winA_" + kind, [D, NCA[kind]])
            winb = P.sb("winb", [128, 8, NCA[kind]], BF16)
            for kc in range(8):
                P.dma("gpsimd", winb[:, kc, :], winA[kc * 128:(kc + 1) * 128, :], w=[winb])
            proj_fm = make_proj_fm(P, hT, hres, winb)
            ymix = YH[:, c0:c0 + YW[kind]]
            env = dict(nc=nc, P=P, din=din, ps=ps, hT=hT, hres=hres, winb=winb, proj_fm=proj_fm, ident=ident,
                       identb=identb, onesb=onesb, ymix=ymix, emit_ctx=emit_ctx, dbg=(), xt=xt)
            if kind == "mlstm":
                build_mlstm(**env)
            elif kind == "mla":
                build_mla(**env)
            else:
                scr = {}
                sres = {}
                for n in R1_OUT:
                    shp = [T, 128] if n in TM_NAMES else [128, T]
                    scr[n] = nc.dram_tensor(pre + "S_" + n, shp, F32).ap()
                    sres[n] = Res()
                for n in ("O0", "O1"):
                    scr[n] = nc.dram_tensor(pre + "S_" + n, [64, T, 2], F32).ap()
                    sres[n] = Res()
                tmos = [P.sb("tmo", [128, 512], F32) for k in range(2)]
                tmc = [0]

                def r1sink(name, src, t0, w, srcres, scr=scr, sres=sres, tmos=tmos, tmc=tmc):
                    tmo = tmos[tmc[0] % 2]
                    tmc[0] += 1
                    if name not in TM_NAMES:
                        P.dma("sync", scr[name][:, t0:t0 + w], src, r=srcres, w=[sres[name]])
                        return
                    pt = ps[7]
                    for ci in range(w // 128):
                        P.tr(pt[:, ci * 128:(ci + 1) * 128], src[:, ci * 128:(ci + 1) * 128], ident[:],
                             r=list(srcres) + [ident], w=[pt])
                    P.cp(tmo[:, 0:w], pt[:, 0:w], r=[pt], w=[tmo])
                    for ci in range(w // 128):
                        P.dma("sync", scr[name][t0 + ci * 128:t0 + (ci + 1) * 128, :], tmo[:, ci * 128:(ci + 1) * 128],
                              r=[tmo], w=[sres[name]])
                build_rwkv(r1sink=r1sink, **env)
                P.release(mk)
                rln = din("rlnrow", [128, 2, 128])
                fused_rwkv_scan(nc, P, ps, ident, scr, sres, ymix, rln, emit_ctx)
            P.release(mk)
        P.release(mL)
        for k, (s_, n_) in enumerate(ych):
            P.coll("AllGather", YH[s_:s_ + n_, :], YG[k][:, :], GROUPS, w=[yg_res])
        P.barrier()
        XH = out if last else nc.dram_tensor(pre + "XH", [TB, D], F32).ap()

        def xres_fn(tt, buf, l=l, XHp=XHp):
            srcx = xres0 if l == 0 else XHp
            P.dma("sync", buf[:], srcx[tt * 128:(tt + 1) * 128, :], w=[buf])

        def ymx_fn(tt, ym, tmp, YG=YG, yg_res=yg_res):
            ra = 0 if tt == 0 else 256 + (tt - 1) * 128
            rb = 128 if tt == 0 else 256 + 2048 + (tt - 1) * 128
            for r in range(2):
                for (rr_, buf_) in ((ra, ym), (rb, tmp)):
                    k, off = rr_ // 1024, rr_ % 1024
                    n_ = 1024 if k < 4 else 256
                    P.dma("sync", buf_[:, r * 512:(r + 1) * 512], YG[k][r * n_ + off:r * n_ + off + 128, :],
                          r=[yg_res], w=[buf_])
            P.ts(ym[:], ym[:], sel[:, 0:1], None, ALU.mult, r=[ym, sel], w=[ym])
            P.stt(ym[:], tmp[:], sel[:, 1:2], ym[:], ALU.mult, ALU.add, r=[tmp, sel, ym], w=[ym])

        def out_fn(tt, XH=XH):
            return XH[tt * 128:(tt + 1) * 128, :], []
        phase_B(nc, P, ps, ident, ones32, din, xres_fn, ymx_fn, out_fn, pre=pre)
        P.release(mL)
        if not last:
            xch = [(0, 512), (512, 512), (1024, 512), (1536, 512), (2048, 128)]
            XG = [nc.dram_tensor(pre + "XG%d" % k, [2 * n_, D], F32).ap() for k, (s_, n_) in enumerate(xch)]
            P.barrier()
            for k, (s_, n_) in enumerate(xch):
                P.coll("AllGather", XH[s_:s_ + n_, :], XG[k][:, :], GROUPS, w=[xg_res])
            P.barrier()
            XHp = XH
    P.emit()
    return nc


_WOUT_PERM = np.concatenate([np.arange(0, 128), np.arange(256, 384), np.arange(512, 768),
                             np.arange(128, 256), np.arange(384, 512), np.arange(768, 1024)])


def prep_fused(inp, nlayers=2):
    x = inp["x"]
    xc = inp["ctx"]
    maps = [dict() for _ in range(8)]
    ident = np.eye(128, dtype=np.float32)
    for core in range(8):
        b, hp = core // 2, core % 2
        m = maps[core]
        m["xin"] = np.ascontiguousarray(np.concatenate([xc[b], x[b]], 0))
        m["xres0"] = np.ascontiguousarray(np.concatenate([xc[b][128 * hp:128 * hp + 128], x[b][2048 * hp:2048 * hp + 2048]], 0))
        sv = np.zeros((128, 2), np.float32)
        sv[:, hp] = 1.0
        m["sel"] = sv
        m["ident"] = ident
    for l in range(nlayers):
        pre = "L%d_" % l
        for kind in ("mlstm", "rwkv", "mla"):
            pa = prep_A(inp, l, x, xc, kind)
            for core in range(8):
                m = maps[core]
                for k, v in pa[core].items():
                    if k in ("xin", "ident"):
                        continue
                    if k == "cT":
                        m["cT"] = v
                    elif k == "winA":
                        m[pre + "winA_" + kind] = v
                    else:
                        m[pre + k] = v
        bm = inp["b_mod"][l][2048:6144]
        shared_b = {
            "wmodB": np.ascontiguousarray(inp["w_mod"][l][:, 2048:6144]),
            "bmodrow": np.ascontiguousarray(np.broadcast_to(bm[None], (128, 4096))),
            "bmodcol": _col_layout(bm),
            "wout": np.ascontiguousarray(inp["w_out"][l][_WOUT_PERM]),
            "lnrow": np.ascontiguousarray(np.broadcast_to(
                np.stack([inp["ln1_w"][l], inp["ln1_b"][l], inp["ln2_w"][l], inp["ln2_b"][l]])[None], (128, 4, D))),
            "rw": inp["router_w"][l],
            "rbias": np.ascontiguousarray(np.broadcast_to(inp["router_bias"][l][None], (128, 64))),
            "eg": np.concatenate([inp["exp_w_gate"][l], inp["sh_w_gate"][l][None]], 0),
            "eu": np.concatenate([inp["exp_w_up"][l], inp["sh_w_up"][l][None]], 0),
            "ed": np.concatenate([inp["exp_w_down"][l], inp["sh_w_down"][l][None]], 0),
        }
        for core in range(8):
            hp = core % 2
            hc = slice(128 * hp, 128 * hp + 128)
            m = maps[core]
            for k, v in shared_b.items():
                m[pre + k] = v
            m[pre + "rlnrow"] = np.ascontiguousarray(np.broadcast_to(
                np.stack([inp["rwkv_ln_w"][l][hc], inp["rwkv_ln_b"][l][hc]])[None], (128, 2, 128)))
    return maps


def kernel_unfused(**inp):
    return _kernel_unfused(inp)


def kernel(**inp):
    inp = {k: np.asarray(v) for k, v in inp.items()}
    nc = build_fused(2)
    res = run_bass_kernel_spmd(nc, prep_fused(inp, 2), core_ids=list(range(8)))
    x = np.empty_like(inp["x"], dtype=np.float32)
    for core in range(8):
        b, hf = core // 2, core % 2
        o = res.results[core]["xout"]
        x[b][2048 * hf:2048 * hf + 2048] = o[128:]
    return x
```

```python
import numpy as np
import ml_dtypes
import concourse.bass as bass
import concourse.mybir as mybir
from concourse.bass_utils import run_bass_kernel_spmd

F32 = mybir.dt.float32
BF16 = mybir.dt.bfloat16
AF = mybir.ActivationFunctionType
ALU = mybir.AluOpType
AX = mybir.AxisListType

D = 1024
T = 4352
NT = 34
NCTX = 256
ISQ96 = 96 ** -0.5


class Res:
    __slots__ = ("w", "r")

    def __init__(self):
        self.w = None
        self.r = {}


class Buf:
    def __init__(self, t):
        self.t = t
        self.res = Res()
        self._sub = {}

    def __getitem__(self, k):
        return self.t[k]

    def sub(self, k):
        if k not in self._sub:
            self._sub[k] = Res()
        return self._sub[k]


def _res(x):
    return x.res if hasattr(x, "res") else x


class Prog:
    CE = ("tensor", "vector", "scalar", "gpsimd")

    def __init__(self, nc, ndma=12):
        self.nc = nc
        self.lists = {e: [] for e in self.CE + ("sync",)}
        self.sems = {e: nc.alloc_semaphore("es_" + e) for e in self.CE}
        for i in range(ndma):
            self.sems["d%d" % i] = nc.alloc_semaphore("ds%d" % i)
        self.ndma = ndma
        self.duse = [0] * ndma
        self.dnext = 0
        self.tick = {e: 0 for e in self.CE}
        self.seen = {e: {} for e in self.lists}
        self.n = 0
        self.scoped = False
        self.sp = 16384 + 256
        self.cnt = 0
        self.ncoll = 0

    def sb(self, name, shape, dt):
        if not self.scoped:
            self.cnt += 1
            return Buf(self.nc.alloc_sbuf_tensor("s%d_%s" % (self.cnt, name), list(shape), dt))
        esz = 4 if dt == F32 else 2
        nb = esz
        for d_ in shape[1:]:
            nb *= d_
        nb = (nb + 63) // 64 * 64
        off = self.sp
        self.sp += nb
        assert self.sp <= 229000, ("SBUF overflow", name, self.sp)
        self.cnt += 1
        return Buf(self.nc.alloc_sbuf_tensor_at("s%d_%s" % (self.cnt, name), list(shape), dt, offset=off))

    def ps(self, name, shape, dt=F32):
        return Buf(self.nc.alloc_psum_tensor("p_" + name, list(shape), dt))

    def mark(self):
        return self.sp

    def release(self, mark):
        self.barrier()
        self.sp = mark

    def barrier(self):
        tgt = {e: self.tick[e] for e in self.CE if self.tick[e]}
        for i in range(self.ndma):
            if self.duse[i]:
                tgt["d%d" % i] = 16 * self.duse[i]
        for i in range(self.ncoll):
            tgt["c%d" % i] = 1
        for q in self.lists:
            waits = []
            for k, v in tgt.items():
                if k == q:
                    continue
                if self.seen[q].get(k, 0) >= v:
                    continue
                self.seen[q][k] = v
                waits.append((k, v))
            if waits:
                self.lists[q].append((waits, None, None, 0))

    def coll(self, kind, in_ap, out_ap, groups, r=(), w=()):
        key = "c%d" % self.ncoll
        self.ncoll += 1
        self.sems[key] = self.nc.alloc_semaphore("cs_" + key)
        waits = self._deps("gpsimd", r, w)
        tok = (key, 1)
        self.lists["gpsimd"].append((waits, lambda e: e.collective_compute(
            kind, ALU.bypass, replica_groups=groups, ins=[in_ap], outs=[out_ap]), key, 1))
        self._mark(tok, r, w)
        return tok

    def _deps(self, q, r, w, extra=None):
        deps = dict(extra or {})

        def add(k, v):
            if deps.get(k, 0) < v:
                deps[k] = v
        for x in r:
            x = _res(x)
            if x.w is not None:
                add(*x.w)
        for x in w:
            x = _res(x)
            if x.w is not None:
                add(*x.w)
            for k, v in x.r.items():
                add(k, v)
        waits = []
        seen = self.seen[q]
        for k, v in deps.items():
            if k == q and q == "tensor":
                continue
            if seen.get(k, 0) >= v:
                continue
            seen[k] = v
            waits.append((k, v))
        return waits

    def _mark(self, tok, r, w):
        k, v = tok
        for x in r:
            x = _res(x)
            if x.r.get(k, 0) < v:
                x.r[k] = v
        for x in w:
            x = _res(x)
            x.w = tok
            x.r = {}

    def op(self, eng, fn, r=(), w=()):
        waits = self._deps(eng, r, w)
        self.tick[eng] += 1
        tok = (eng, self.tick[eng])
        self.lists[eng].append((waits, fn, eng, 1))
        self._mark(tok, r, w)
        self.n += 1
        return tok

    def dma(self, q, out, in_, r=(), w=()):
        i = self.dnext
        self.dnext = (i + 1) % self.ndma
        key = "d%d" % i
        extra = {key: 16 * self.duse[i]} if self.duse[i] else {}
        waits = self._deps(q, r, w, extra)
        self.duse[i] += 1
        tok = (key, 16 * self.duse[i])
        self.lists[q].append((waits, lambda e: e.dma_start(out=out, in_=in_), key, 16))
        self._mark(tok, r, w)
        self.n += 1
        return tok

    def call(self, eng, name, *args, r=(), w=(), **kw):
        return self.op(eng, lambda e: getattr(e, name)(*args, **kw), r, w)

    def mm(self, out, lhsT, rhs, start=True, stop=True, r=(), w=(), skip=False):
        return self.op("tensor", lambda e: e.matmul(out, lhsT, rhs, start=start, stop=stop,
                                                     skip_group_check=skip), r, w)

    def tr(self, out, in_, ident, r=(), w=()):
        return self.op("tensor", lambda e: e.transpose(out, in_, ident), r, w)

    def act(self, out, in_, func, r=(), w=(), eng="scalar", **kw):
        return self.op(eng, lambda e: e.activation(out, in_, func, **kw), r, w)

    def tt(self, out, in0, in1, op, r=(), w=(), eng="vector"):
        return self.op(eng, lambda e: e.tensor_tensor(out, in0, in1, op), r, w)

    def ts(self, out, in0, s1, s2, op0, op1=None, r=(), w=(), eng="vector", **kw):
        if op1 is None:
            return self.op(eng, lambda e: e.tensor_scalar(out, in0, s1, s2, op0, **kw), r, w)
        return self.op(eng, lambda e: e.tensor_scalar(out, in0, s1, s2, op0, op1, **kw), r, w)

    def stt(self, out, in0, s, in1, op0, op1, r=(), w=()):
        return self.op("vector", lambda e: e.scalar_tensor_tensor(out, in0, s, in1, op0, op1), r, w)

    def cp(self, out, in_, r=(), w=(), eng="vector"):
        if eng == "scalar":
            return self.op(eng, lambda e: e.activation(out, in_, AF.Copy), r, w)
        return self.op(eng, lambda e: e.tensor_copy(out, in_), r, w)

    def memset(self, ap, val, w=(), eng="vector"):
        return self.op(eng, lambda e: e.memset(ap, val), (), w)

    def finish(self):
        waits = []
        for e in self.CE:
            if self.tick[e]:
                waits.append((e, self.tick[e]))
        for i in range(self.ndma):
            if self.duse[i]:
                waits.append(("d%d" % i, 16 * self.duse[i]))
        for i in range(self.ncoll):
            waits.append(("c%d" % i, 1))
        self.lists["sync"].append((waits, None, None, 0))

    def emit(self):
        self.finish()
        P = self
        with self.nc.Block() as block:
            def mk(name):
                def body(eng):
                    for waits, fn, skey, inc in P.lists[name]:
                        for k, v in waits:
                            eng.wait_ge(P.sems[k], v)
                        if fn is not None:
                            if inc == 1 and skey.startswith("c"):
                                fn(eng).then_inc(P.sems[skey])
                            else:
                                fn(eng).then_inc(P.sems[skey], inc)
                return body
            block.tensor(mk("tensor"))
            block.vector(mk("vector"))
            block.scalar(mk("scalar"))
            block.gpsimd(mk("gpsimd"))
            block.sync(mk("sync"))


KIND_COLS = {
    "mla": (("cq", 256), ("ckv", 128), ("kr", 96), ("krs", 96)),
    "mlstm": (("mq", 128), ("mk", 128), ("mv", 128), ("mo", 128), ("mg", 8)),
    "rwkv": (("rr", 128), ("rk", 128), ("rv", 128), ("rw", 128), ("ra", 128), ("rg", 128)),
}
A_COLS = {}
NCA = {}
for _k, _lst in KIND_COLS.items():
    _o = 0
    for _n, _w in _lst:
        A_COLS[_n] = (_o, _w)
        _o += _w
    NCA[_k] = _o
YW = {"mla": 256, "mlstm": 128, "rwkv": 128}

QBLOCKS = [(0, 256)] + [(256 + 512 * i, 512) for i in range(8)]


def phase_ln(nc, P, ps, ident, xrow_fn, cT, wmodA, bmodA, xres_list):
    csil = P.sb("csil", [128, 8, 2], F32)
    P.dma("sync", csil[:], cT[:, :, :], w=[csil])
    P.act(csil[:], csil[:], AF.Silu, r=[csil], w=[csil])
    bmod = P.sb("bmod", [128, 16], F32)
    P.dma("sync", bmod[:], bmodA[:, :], w=[bmod])
    modc = P.sb("modc", [128, 16, 2], F32)
    wm = [P.sb("wm%d" % i, [128, 8, 128], F32) for i in range(2)]
    for oc in range(16):
        wq = wm[oc % 2]
        P.dma("sync", wq[:], wmodA[:, oc * 128:(oc + 1) * 128].rearrange("(k p) c -> p k c", p=128), w=[wq])
        pt = ps[oc % 2]
        for kc in range(8):
            P.mm(pt[:, 0:2], wq[:, kc, :], csil[:, kc, :],
                 start=(kc == 0), stop=(kc == 7), r=[wq, csil], w=[pt])
        P.act(modc[:, oc, :], pt[:, 0:2], AF.Identity, r=[pt, bmod], w=[modc],
              bias=bmod[:, oc:oc + 1])
    P.ts(modc[:, 8:16, :], modc[:, 8:16, :], 1.0, None, ALU.add, r=[modc], w=[modc])
    hT = P.sb("hT", [128, 8, T], BF16)
    xt = [P.sb("xt%d" % i, [128, D], F32) for i in range(2)]
    st = P.sb("st", [128, 12], F32)
    mv = P.sb("mv", [128, 2], F32)
    rs = P.sb("rs", [128, 1], F32)
    for tt in range(NT):
        x_ = xt[tt % 2]
        n_ = x_
        m = 1 if tt < 2 else 0
        src, sres = xrow_fn(tt)
        P.dma("sync", x_[:], src, r=sres, w=[x_])
        P.call("vector", "bn_stats", st[:, 0:6], x_[:, 0:512], r=[x_], w=[st])
        P.call("vector", "bn_stats", st[:, 6:12], x_[:, 512:1024], r=[x_], w=[st])
        P.call("vector", "bn_aggr", mv[:], st[:], r=[st], w=[mv])
        P.act(rs[:], mv[:, 1:2], AF.Sqrt, r=[mv], w=[rs], bias=1e-6)
        P.call("vector", "reciprocal", rs[:], rs[:], r=[rs], w=[rs])
        P.ts(n_[:], x_[:], mv[:, 0:1], rs[:, 0:1], ALU.subtract, ALU.mult, r=[x_, mv, rs], w=[n_])
        for half in range(2):
            pt = ps[half]
            for j4 in range(4):
                j = half * 4 + j4
                P.tr(pt[:, j4 * 128:(j4 + 1) * 128], n_[:, j * 128:(j + 1) * 128], ident[:],
                     r=[n_, ident], w=[pt])
            for j4 in range(4):
                j = half * 4 + j4
                P.act(hT[:, j, tt * 128:(tt + 1) * 128], pt[:, j4 * 128:(j4 + 1) * 128], AF.Identity,
                      r=[pt, modc], w=[hT.sub(tt)],
                      scale=modc[:, 8 + j, m:m + 1], bias=modc[:, j, m:m + 1])
    hres = [hT.sub(tt) for tt in range(NT)]
    return hT, hres, xt


def make_proj_fm(P, hT, hres, winb):
    def proj_fm(pt, col0, ncols, t0, w, first=True, last=True, src=None, wsrc=None):
        src = src or hT
        wsrc = wsrc or winb
        for kc in range(8):
            P.mm(pt[0:ncols, 0:w], wsrc[:, kc, col0:col0 + ncols], src[:, kc, t0:t0 + w],
                 start=(first and kc == 0), stop=(last and kc == 7),
                 r=[wsrc] + (hres if src is hT else [src]), w=[pt])
    return proj_fm


def build_A(kind, emit_ctx, dbg=()):
    nc = bass.Bass("TRN2", target_bir_lowering=False)
    P = Prog(nc)

    def din(name, shape, dt=F32):
        return nc.dram_tensor(name, list(shape), dt, kind="ExternalInput").ap()

    xin = din("xin", [T, D])
    cT = din("cT", [128, 8, 2])
    wmodA = din("wmodA", [D, 2048])
    bmodA = din("bmodA", [128, 16])
    winA = din("winA", [D, NCA[kind]])
    ident_d = din("ident", [128, 128])
    ymix = nc.dram_tensor("ymix", [T, YW[kind]], F32, kind="ExternalOutput").ap()

    ident = P.sb("ident", [128, 128], F32)
    P.dma("sync", ident[:], ident_d[:, :], w=[ident])
    identb = P.sb("identb", [128, 128], BF16)
    P.cp(identb[:], ident[:], r=[ident], w=[identb])
    onesb = P.sb("onesb", [128, 128], BF16)
    P.memset(onesb[:], 1.0, w=[onesb])
    ps = [P.ps("ps%d" % i, [128, 512]) for i in range(8)]
    winb = P.sb("winb", [128, 8, NCA[kind]], BF16)
    for kc in range(8):
        P.dma("gpsimd", winb[:, kc, :], winA[kc * 128:(kc + 1) * 128, :], w=[winb])
    hT, hres, xt = phase_ln(nc, P, ps, ident, lambda tt: (xin[tt * 128:(tt + 1) * 128, :], []), cT, wmodA, bmodA, None)
    proj_fm = make_proj_fm(P, hT, hres, winb)
    env = dict(nc=nc, P=P, din=din, ps=ps, hT=hT, hres=hres, winb=winb, proj_fm=proj_fm, ident=ident,
               identb=identb, onesb=onesb, ymix=ymix, emit_ctx=emit_ctx, dbg=dbg, xt=xt)
    if kind == "mla":
        build_mla(**env)
    elif kind == "mlstm":
        build_mlstm(**env)
    else:
        build_rwkv(**env)
    P.emit()
    return nc


def build_mla(nc, P, din, ps, hT, hres, winb, proj_fm, ident, identb, onesb, ymix, emit_ctx, dbg, xt):
    wuq_d = din("wuq", [256, 4, 192])
    wukv_d = din("wukv", [128, 512])
    qn_d = din("qnorm", [128, 2])
    kvn_d = din("kvnorm", [128, 1])
    cs_d = din("cossin", [32, 2, 4096], BF16)

    wuqb = P.sb("wuqb", [128, 2, 768], BF16)
    qn = P.sb("qn", [128, 2], F32)
    P.dma("sync", qn[:], qn_d[:, :], w=[qn])
    for ch in range(2):
        P.dma("sync", xt[ch][:, 0:768], wuq_d[ch * 128:(ch + 1) * 128, :, :].rearrange("p h c -> p (h c)"), w=[xt[ch]])
    for ch in range(2):
        P.ts(wuqb[:, ch, :], xt[ch][:, 0:768], qn[:, ch:ch + 1], None, ALU.mult, r=[xt[ch], qn], w=[wuqb])
    wukvb = P.sb("wukvb", [128, 512], BF16)
    kvnw = P.sb("kvnw", [128, 1], F32)
    P.dma("sync", kvnw[:], kvn_d[:, :], w=[kvnw])
    cs = P.sb("cs", [128, 2, 4096], BF16)
    P.dma("sync", cs[64:96, :, :], cs_d[:, :, :], w=[cs])

    KT = [P.sb("KT%d" % h, [96, T], BF16) for h in range(4)]
    Vp = P.sb("Vp", [128, NT, 4, 65], BF16)
    P.memset(Vp[:], 1.0, w=[Vp])
    c32 = P.sb("c32", [128, 2, 512], F32)
    sq = P.sb("sqb", [128, 2, 512], BF16)
    rb = P.sb("rb", [128, 512], F32)
    cn = P.sb("cn", [128, 2, 512], BF16)
    t1 = P.sb("ropet1", [128, 512], F32)
    t2 = P.sb("ropet2", [128, 512], F32)
    P.dma("sync", t1[:], wukv_d[:, :], w=[t1])
    P.ts(wukvb[:], t1[:], kvnw[:, 0:1], None, ALU.mult, r=[t1, kvnw], w=[wukvb])

    def rmsnorm(nch, col0, t0, w, pbase):
        for ch in range(nch):
            pt = ps[pbase + ch]
            proj_fm(pt, col0 + ch * 128, 128, t0, w)
            P.act(c32[:, ch, 0:w], pt[:, 0:w], AF.Copy, r=[pt], w=[c32])
            P.act(sq[:, ch, 0:w], pt[:, 0:w], AF.Square, r=[pt], w=[sq], scale=float((128 * nch) ** -0.5))
        pt = ps[pbase + 2]
        for ch in range(nch):
            P.mm(pt[:, 0:w], onesb[:], sq[:, ch, 0:w], start=(ch == 0), stop=(ch == nch - 1), r=[onesb, sq], w=[pt])
        P.act(rb[:, 0:w], pt[:, 0:w], AF.Sqrt, r=[pt], w=[rb], bias=1e-6)
        P.call("vector", "reciprocal", rb[:, 0:w], rb[:, 0:w], r=[rb], w=[rb])
        for ch in range(nch):
            P.tt(cn[:, ch, 0:w], c32[:, ch, 0:w], rb[:, 0:w], ALU.mult, r=[c32, rb], w=[cn])

    def rope(dst, pq, psw, t0, w, lat):
        if not lat:
            P.cp(dst[64:96, t0:t0 + w], pq[64:96, 0:w], r=[pq], w=[dst])
            return
        l0 = t0 - NCTX
        P.tt(t1[64:96, 0:w], pq[64:96, 0:w], cs[64:96, 0, l0:l0 + w], ALU.mult, r=[pq, cs], w=[t1])
        P.tt(t2[64:96, 0:w], psw[64:96, 0:w], cs[64:96, 1, l0:l0 + w], ALU.mult, r=[psw, cs], w=[t2])
        P.tt(dst[64:96, t0:t0 + w], t1[64:96, 0:w], t2[64:96, 0:w], ALU.add, r=[t1, t2], w=[dst])

    ckv0 = A_COLS["ckv"][0]
    for (t0, w) in QBLOCKS:
        lat = t0 >= NCTX
        rmsnorm(1, ckv0, t0, w, 0)
        proj_fm(ps[3], A_COLS["kr"][0], 96, t0, w)
        if lat:
            proj_fm(ps[4], A_COLS["krs"][0], 96, t0, w)
        rope(KT[0], ps[3], ps[4], t0, w, lat)
        for h in range(4):
            pt = ps[5 + (h % 2)]
            P.mm(pt[0:64, 0:w], wukvb[:, h * 64:(h + 1) * 64], cn[:, 0, 0:w], r=[wukvb, cn], w=[pt])
            P.cp(KT[h][0:64, t0:t0 + w], pt[0:64, 0:w], r=[pt], w=[KT[h]], eng="scalar")
            if h > 0:
                P.cp(KT[h][64:96, t0:t0 + w], KT[0][64:96, t0:t0 + w], r=[KT[0]], w=[KT[h]], eng="gpsimd")
        for ti in range(w // 128):
            tt = t0 // 128 + ti
            pt = ps[7]
            P.mm(pt[:, 0:256], cn[:, 0, ti * 128:(ti + 1) * 128], wukvb[:, 256:512], r=[wukvb, cn], w=[pt])
            P.cp(Vp[:, tt, :, 0:64], pt[:, 0:256].rearrange("p (h c) -> p h c", h=4), r=[pt], w=[Vp])

    QT = [P.sb("QT%d" % h, [96, 512], BF16) for h in range(4)]
    PT = [P.sb("PT%d" % i, [128, 512], BF16) for i in range(3)]
    zerob = P.sb("zerob", [128, 512], BF16)
    P.memset(zerob[:], 0.0, w=[zerob])
    rden = P.sb("rden", [128, 4, 1], F32)
    oT = P.sb("oT", [65, 512], F32)
    yo = [P.sb("yo%d" % i, [128, 4, 64], F32) for i in range(2)]
    cq0 = A_COLS["cq"][0]
    it = 0
    for (t0, w) in QBLOCKS:
        lat = t0 >= NCTX
        if not lat and not emit_ctx:
            continue
        rmsnorm(2, cq0, t0, w, 0)
        for h in range(4):
            pq = ps[3]
            psw = ps[4]
            for ch in range(2):
                P.mm(pq[0:96, 0:w], wuqb[:, ch, h * 192:h * 192 + 96], cn[:, ch, 0:w],
                     start=(ch == 0), stop=(ch == 1), r=[wuqb, cn], w=[pq])
            if lat:
                for ch in range(2):
                    P.mm(psw[0:96, 0:w], wuqb[:, ch, h * 192 + 96:h * 192 + 192], cn[:, ch, 0:w],
                         start=(ch == 0), stop=(ch == 1), r=[wuqb, cn], w=[psw])
            P.cp(QT[h][0:64, 0:w], pq[0:64, 0:w], r=[pq], w=[QT[h]], eng="scalar")
            ropeq(P, QT[h], pq, psw, w, lat, t0, cs, t1, t2)
        nq = w // 128
        kts = range(NT) if lat else range(2)
        for h in range(4):
            accT = ps[5 + (h % 2)]
            units = list(kts)

            def qk(kt, j):
                sp_ = ps[j % 3]
                P.mm(sp_[:, 0:w], KT[h][0:96, kt * 128:(kt + 1) * 128], QT[h][0:96, 0:w], r=[KT[h], QT[h]], w=[sp_])
                return sp_
            spq = [qk(units[j], it + j) for j in range(min(2, len(units)))]
            for idx, kt in enumerate(units):
                sp = spq[idx]
                pt_ = PT[it % 3]
                if idx + 2 < len(units):
                    spq.append(qk(units[idx + 2], it + 2))
                it += 1
                P.act(pt_[:, 0:w], sp[:, 0:w], AF.Exp, r=[sp], w=[pt_], scale=ISQ96)
                P.mm(accT[0:65, 0:w], Vp[:, kt, h, :], pt_[:, 0:w], start=(idx == 0), stop=(idx == len(units) - 1),
                     r=[pt_, Vp], w=[accT])
            P.cp(oT[0:65, 0:w], accT[0:65, 0:w], r=[accT], w=[oT])
            acc = ps[7]
            for qi in range(nq):
                P.tr(acc[:, qi * 65:(qi + 1) * 65], oT[0:65, qi * 128:(qi + 1) * 128], ident[0:65, 0:65],
                     r=[oT, ident], w=[acc])
            y_ = yo[h % 2]
            a3 = acc[:, 0:nq * 65].rearrange("p (q c) -> p q c", c=65)
            P.call("vector", "reciprocal", rden[:, 0:nq, :], a3[:, :, 64:65], r=[acc], w=[rden])
            P.tt(y_[:, 0:nq, :], a3[:, :, 0:64], rden[:, 0:nq, :].to_broadcast([128, nq, 64]), ALU.mult,
                 r=[acc, rden], w=[y_])
            for qi in range(nq):
                r0 = t0 + qi * 128
                P.dma("sync", ymix[r0:r0 + 128, h * 64:(h + 1) * 64], y_[:, qi, :], r=[y_])


def ropeq(P, dst, pq, psw, w, lat, t0, cs, t1, t2):
    if not lat:
        P.cp(dst[64:96, 0:w], pq[64:96, 0:w], r=[pq], w=[dst])
        return
    l0 = t0 - NCTX
    P.tt(t1[64:96, 0:w], pq[64:96, 0:w], cs[64:96, 0, l0:l0 + w], ALU.mult, r=[pq, cs], w=[t1])
    P.tt(t2[64:96, 0:w], psw[64:96, 0:w], cs[64:96, 1, l0:l0 + w], ALU.mult, r=[psw, cs], w=[t2])
    P.tt(dst[64:96, 0:w], t1[64:96, 0:w], t2[64:96, 0:w], ALU.add, r=[t1, t2], w=[dst])


def _cols_A(hp, kind):
    N_M = 1040
    RB = N_M
    MB = N_M + 1152
    hs = [2 * hp, 2 * hp + 1]
    idx = {}
    idx["cq"] = list(range(MB, MB + 256))
    idx["ckv"] = list(range(MB + 256, MB + 384))
    kr = list(range(MB + 384, MB + 416))
    idx["kr"] = [-1] * 64 + kr
    idx["krs"] = [-1] * 64 + [kr[i ^ 1] for i in range(32)]
    idx["mq"] = list(range(128 * hp, 128 * hp + 128))
    idx["mk"] = list(range(256 + 128 * hp, 256 + 128 * hp + 128))
    idx["mv"] = list(range(512 + 128 * hp, 512 + 128 * hp + 128))
    idx["mo"] = list(range(768 + 128 * hp, 768 + 128 * hp + 128))
    idx["mg"] = [1024 + g * 4 + h for g in (0, 2, 1, 3) for h in hs]
    idx["rr"] = list(range(RB + 128 * hp, RB + 128 * hp + 128))
    idx["rk"] = list(range(RB + 256 + 128 * hp, RB + 256 + 128 * hp + 128))
    idx["rv"] = list(range(RB + 512 + 128 * hp, RB + 512 + 128 * hp + 128))
    idx["rw"] = list(range(RB + 768, RB + 896))
    idx["ra"] = list(range(RB + 896, RB + 1024))
    idx["rg"] = list(range(RB + 1024, RB + 1152))
    out = []
    for n, w in KIND_COLS[kind]:
        assert len(idx[n]) == w, n
        out += idx[n]
    return np.array(out)


def _gather_cols(w, cols):
    out = np.zeros((w.shape[0], len(cols)), w.dtype)
    m = cols >= 0
    out[:, m] = w[:, cols[m]]
    return out


def _col_layout(v):
    return np.ascontiguousarray(v.reshape(-1, 128).T)


def _rope_tables():
    n = 4096
    row = np.repeat(np.arange(64), 64).astype(np.float32)
    col = np.tile(np.arange(64), 64).astype(np.float32)
    freq = (np.float32(10000.0) ** (-np.arange(8, dtype=np.float32) / np.float32(8))).astype(np.float32)
    ang = np.concatenate([row[:, None] * freq, col[:, None] * freq], -1)
    cos = np.cos(ang).astype(np.float32)
    sin = np.sin(ang).astype(np.float32)
    tab = np.zeros((32, 2, n), np.float32)
    for r in range(32):
        tab[r, 0] = cos[:, r // 2]
        tab[r, 1] = sin[:, r // 2] * (-1.0 if r % 2 == 0 else 1.0)
    return tab.astype(ml_dtypes.bfloat16)


def prep_A(inp, l, x_cur, xc_cur, kind):
    maps = []
    ident = np.eye(128, dtype=np.float32)
    cs = _rope_tables()
    for core in range(8):
        b, hp = core // 2, core % 2
        m = {}
        m["xin"] = np.ascontiguousarray(np.concatenate([xc_cur[b], x_cur[b]], 0))
        cc = np.stack([inp["c"][b], inp["c_ctx"]], -1)
        m["cT"] = np.ascontiguousarray(cc.reshape(8, 128, 2).transpose(1, 0, 2))
        m["wmodA"] = np.ascontiguousarray(inp["w_mod"][l][:, 0:2048])
        m["bmodA"] = _col_layout(inp["b_mod"][l][0:2048])
        m["winA"] = _gather_cols(inp["w_in"][l], _cols_A(hp, kind))
        m["ident"] = ident
        if kind == "mlstm":
            cw = inp["mlstm_conv"][l]
            cc_ = np.concatenate([cw[:, 128 * hp:128 * hp + 128], cw[:, 256 + 128 * hp:256 + 128 * hp + 128]], 1)
            m["convbc"] = np.ascontiguousarray(np.broadcast_to(cc_[None], (128, 3, 256)))
            gb = inp["mlstm_gate_bias"][l]
            gv = np.array([gb[g, h] for g in (0, 2, 1, 3) for h in (2 * hp, 2 * hp + 1)], np.float32)
            m["gbias"] = np.ascontiguousarray(np.broadcast_to(gv[None], (128, 8)))
            nw = inp["mlstm_norm_w"][l][128 * hp:128 * hp + 128]
            m["normw"] = np.ascontiguousarray(np.broadcast_to(nw[None], (128, 128)))
            s_ = np.arange(128)[:, None]
            j_ = np.arange(512)[None, :]
            mf = np.stack([(s_ + 128 * o <= j_) for o in range(4)], 1)
            mb = np.stack([(s_ + 128 * o >= j_) for o in range(4)], 1)
            m["maskf"] = mf.astype(np.float32).astype(ml_dtypes.bfloat16)
            m["maskb"] = mb.astype(np.float32).astype(ml_dtypes.bfloat16)
            m["triu"] = (s_ <= np.arange(128)[None, :]).astype(np.float32)
        if kind == "rwkv":
            RB = 1040
            cols = _cols_A(hp, "rwkv") - RB
            m["mubc"] = np.ascontiguousarray(np.broadcast_to(inp["rwkv_mu"][l][cols][None], (128, 768)))
            hc = slice(128 * hp, 128 * hp + 128)
            pc = np.zeros((128, 8), np.float32)
            pc[:, 0] = inp["rwkv_w0"][l][0][hc]
            pc[:, 1] = inp["rwkv_w0"][l][1][hc]
            pc[:, 2] = inp["rwkv_a0"][l][0][hc]
            pc[:, 3] = inp["rwkv_a0"][l][1][hc]
            pc[:, 4] = inp["rwkv_k_k"][l][hc]
            pc[:, 5] = inp["rwkv_k_a"][l][hc]
            pc[:, 6] = inp["rwkv_r_k"][l][hc]
            m["pcol"] = pc
            m["wup"] = np.ascontiguousarray(inp["rwkv_w_up"][l][:, :, hc].reshape(128, 128))
            m["aup"] = np.ascontiguousarray(inp["rwkv_a_up"][l][:, :, hc].reshape(128, 128))
            m["gup"] = np.ascontiguousarray(inp["rwkv_g_up"][l][:, hc])
            bo = np.zeros((128, 128), np.float32)
            bo[0:64, 0:64] = 1.0
            bo[64:128, 64:128] = 1.0
            m["bones"] = bo
        if kind != "mla":
            maps.append(m)
            continue
        wuq = inp["mla_w_uq"][l].reshape(256, 8, 96)[:, 4 * hp:4 * hp + 4, :]
        w4 = np.zeros((256, 4, 192), np.float32)
        w4[:, :, 0:96] = wuq
        sw = [64 + (i ^ 1) for i in range(32)]
        w4[:, :, 160:192] = wuq[:, :, sw]
        m["wuq"] = w4
        wukv = inp["mla_w_ukv"][l].reshape(128, 8, 128)[:, 4 * hp:4 * hp + 4, :]
        m["wukv"] = np.ascontiguousarray(np.concatenate(
            [wukv[:, :, 0:64].reshape(128, 256), wukv[:, :, 64:128].reshape(128, 256)], 1))
        m["qnorm"] = _col_layout(inp["mla_q_norm"][l])
        m["kvnorm"] = _col_layout(inp["mla_kv_norm"][l])
        m["cossin"] = cs
        maps.append(m)
    return maps


def build_mlstm(nc, P, din, ps, hT, hres, winb, proj_fm, ident, identb, onesb, ymix, emit_ctx, dbg, xt):
    convbc_d = din("convbc", [128, 3, 256])
    gbias_d = din("gbias", [128, 8])
    normw_d = din("normw", [128, 128])
    maskf_d = din("maskf", [128, 4, 512], BF16)
    maskb_d = din("maskb", [128, 4, 512], BF16)
    triu_d = din("triu", [128, 128])

    convbc = P.sb("convbc", [128, 3, 256], F32)
    P.dma("sync", convbc[:], convbc_d[:, :, :], w=[convbc])
    gbias = P.sb("gbias", [128, 8], F32)
    P.dma("sync", gbias[:], gbias_d[:, :], w=[gbias])
    normw = P.sb("normw", [128, 128], F32)
    P.dma("sync", normw[:], normw_d[:, :], w=[normw])
    maskf = P.sb("maskf", [128, 4, 512], BF16)
    P.dma("sync", maskf[:], maskf_d[:, :, :], w=[maskf])
    maskb = P.sb("maskb", [128, 4, 512], BF16)
    P.dma("sync", maskb[:], maskb_d[:, :, :], w=[maskb])
    triu = P.sb("triu", [128, 128], F32)
    P.dma("sync", triu[:], triu_d[:, :], w=[triu])
    ones32 = P.sb("ones32", [128, 128], F32)
    P.memset(ones32[:], 1.0, w=[ones32])

    wc = P.sb("wc", [128, 8, 3, 256], BF16)
    q0 = A_COLS["mq"][0]
    for kc in range(8):
        for j in range(3):
            P.tt(wc[:, kc, j, :], winb[:, kc, q0:q0 + 256], convbc[:, j, :], ALU.mult, r=[winb, convbc], w=[wc],
                 eng="vector")

    QK = [P.sb("QmT", [128, T], BF16), P.sb("KmT", [128, T], BF16)]
    Vp = P.sb("Vpm", [128, NT, 2, 65], BF16)
    P.memset(Vp[:], 1.0, w=[Vp])
    og = P.sb("og", [128, NT, 128], BF16)
    G = P.sb("G", [128, NT, 8], F32)
    sgt = P.sb("sgt", [128, 128], F32)

    STOP = 9
    for (t0, w) in QBLOCKS:
        seg0 = t0 in (0, NCTX)
        seg1 = (t0 + w) in (NCTX, T)
        for qk in range(2):
            pt = ps[5 + qk]
            first = True
            for j in (1, 0, 2):
                a = 1 if (j == 0 and seg0) else 0
                b = 1 if (j == 2 and seg1) else 0
                for kc in range(8):
                    last = (j == 2 and kc == 7)
                    P.mm(pt[:, a:w - b], wc[:, kc, j, qk * 128:(qk + 1) * 128],
                         hT[:, kc, t0 + a + j - 1:t0 + w - b + j - 1],
                         start=first, stop=last, r=[wc] + hres, w=[pt], skip=True)
                    first = False
            P.act(QK[qk][:, t0:t0 + w], pt[:, 0:w], AF.Silu, r=[pt], w=[QK[qk]])
        v0 = A_COLS["mv"][0]
        if STOP == -1:
            continue
        for ti in range(w // 128):
            tt = t0 // 128 + ti
            pt = ps[7]
            for kc in range(8):
                P.mm(pt[:, 0:264], hT[:, kc, tt * 128:(tt + 1) * 128], winb[:, kc, v0:v0 + 264],
                     start=(kc == 0), stop=(kc == 7), r=[winb] + hres, w=[pt])
            P.cp(Vp[:, tt, :, 0:64], pt[:, 0:128].rearrange("p (h c) -> p h c", h=2), r=[pt], w=[Vp])
            P.cp(sgt[:], pt[:, 128:256], r=[pt], w=[sgt])
            P.act(sgt[:], sgt[:], AF.Exp, r=[sgt], w=[sgt], scale=-1.0)
            P.ts(sgt[:], sgt[:], 1.0, None, ALU.add, r=[sgt], w=[sgt])
            P.call("vector", "reciprocal", sgt[:], sgt[:], r=[sgt], w=[sgt])
            P.cp(og[:, tt, :], sgt[:], r=[sgt], w=[og])
            P.tt(G[:, tt, :], pt[:, 256:264], gbias[:], ALU.add, r=[pt, gbias], w=[G])

    if STOP < 1:
        return
    LF = P.sb("LF", [128, NT, 4], F32)
    P.act(LF[:], G[:, :, 4:8], AF.Exp, r=[G], w=[LF], scale=-1.0)
    P.act(LF[:], LF[:], AF.Ln, r=[LF], w=[LF], bias=1.0)
    P.ts(LF[:], LF[:], -1.0, None, ALU.mult, r=[LF], w=[LF])
    PW = P.sb("PW", [128, NT, 4], F32)
    TOT = P.sb("TOT", [128, NT, 4], F32)
    IP = P.sb("IP", [128, NT, 4], F32)
    BQ = P.sb("BQ", [128, NT, 4], F32)
    UK = P.sb("UK", [128, NT, 4], F32)
    lf2 = LF[:].rearrange("p t c -> p (t c)")
    P.mm(ps[5][:, 0:NT * 4], triu[:], lf2, r=[triu, LF], w=[ps[5]])
    P.cp(PW[:].rearrange("p t c -> p (t c)"), ps[5][:, 0:NT * 4], r=[ps[5]], w=[PW])
    P.mm(ps[6][:, 0:NT * 4], ones32[:], lf2, r=[ones32, LF], w=[ps[6]])
    P.cp(TOT[:].rearrange("p t c -> p (t c)"), ps[6][:, 0:NT * 4], r=[ps[6]], w=[TOT])
    for c in range(4):
        for (a, b) in ((0, 2), (2, NT)):
            P.call("vector", "tensor_tensor_scan", IP[:, a:b, c], ones32[:, 0:b - a], TOT[:, a:b, c], 0.0,
                   ALU.mult, ALU.add, r=[TOT, ones32], w=[IP])
    EX = TOT
    P.tt(EX[:], IP[:], TOT[:], ALU.subtract, r=[IP, TOT], w=[TOT])
    P.tt(BQ[:, :, 0:2], PW[:, :, 0:2], EX[:, :, 0:2], ALU.add, r=[PW, TOT], w=[BQ])
    for c in range(2):
        P.ts(BQ[:, 2:NT, c], BQ[:, 2:NT, c], IP[:, 1, c:c + 1], None, ALU.add, r=[BQ, IP], w=[BQ])
    P.tt(BQ[:, :, 2:4], LF[:, :, 2:4], PW[:, :, 2:4], ALU.subtract, r=[LF, PW], w=[BQ])
    P.tt(BQ[:, :, 2:4], BQ[:, :, 2:4], EX[:, :, 2:4], ALU.subtract, r=[BQ, TOT], w=[BQ])
    for c in range(2, 4):
        P.ts(BQ[:, 2:NT, c], BQ[:, 2:NT, c], IP[:, NT - 1, c:c + 1], None, ALU.add, r=[BQ, IP], w=[BQ])
    P.tt(UK[:], G[:, :, 0:4], BQ[:], ALU.subtract, r=[G, BQ], w=[UK])
    P.ts(UK[:], UK[:], float(-np.log(8.0)), None, ALU.add, r=[UK], w=[UK])

    if STOP < 2:
        return
    BL = [P.sb("BL%d" % i, [128, 128], F32) for i in range(2)]
    BCs2 = [P.sb("BCs", [128, 512], F32) for i in range(2)]
    igrp = 0
    DT = [P.sb("DT%d" % i, [128, 512], F32) for i in range(3)]
    WT = [P.sb("WT%d" % i, [128, 512], BF16) for i in range(3)]
    WM = P.sb("WM", [128, 512], F32)
    zerob = P.sb("zerob", [128, 512], BF16)
    P.memset(zerob[:], 0.0, w=[zerob])
    den = P.sb("den", [128, 4, 1], F32)
    oTm = P.sb("oTm", [65, 512], F32)
    nden = P.sb("nden", [128, 4, 1], F32)
    c60 = P.sb("c60", [128, 1], F32)
    P.memset(c60[:], 60.0, w=[c60])
    hd = [P.sb("hd%d" % i, [128, 4, 64], F32) for i in range(2)]
    st = P.sb("st2", [128, 4, 6], F32)
    mv = P.sb("mv2", [128, 4, 2], F32)
    yo = [P.sb("yom%d" % i, [128, 4, 64], F32) for i in range(2)]
    it = 0
    ib = 0
    for (t0, w) in QBLOCKS:
        lat = t0 >= NCTX
        if not lat and not emit_ctx:
            continue
        nq = w // 128
        tq0 = t0 // 128
        for hl in range(2):
            hp_ = slice(hl * 64, hl * 64 + 64)
            for d in range(2):
                c = d * 2 + hl
                BCs = BCs2[igrp % 2]
                igrp += 1
                for qi in range(nq):
                    bl = BL[ib % 2]
                    ib += 1
                    P.ts(bl[:], ones32[:], BQ[:, tq0 + qi, c:c + 1], None, ALU.mult, r=[ones32, BQ], w=[bl])
                    P.mm(ps[2][:, qi * 128:(qi + 1) * 128], bl[:], ident[:], r=[bl, ident], w=[ps[2]])
                P.cp(BCs[:, 0:w], ps[2][:, 0:w], r=[ps[2]], w=[BCs], eng="scalar")
                if d == 0:
                    kts = [kt for kt in range(NT) if kt * 128 < t0 + w and (lat or kt < 2)]
                else:
                    kts = [kt for kt in range(NT) if (kt < 2 and lat) or ((kt * 128 + 128 > t0) and (lat == (kt >= 2)))]
                accT = ps[3 + d]

                def qk(kt, j):
                    sp_ = (ps[0], ps[1], ps[7])[j % 3]
                    P.mm(sp_[:, 0:w], QK[1][hp_, kt * 128:(kt + 1) * 128], QK[0][hp_, t0:t0 + w], r=QK, w=[sp_])
                    return sp_
                spq = [qk(kts[j], it + j) for j in range(min(2, len(kts)))]
                for idx, kt in enumerate(kts):
                    diag = (kt * 128 >= t0) and (kt * 128 < t0 + w)
                    sp = spq[idx]
                    dt_ = DT[it % 3]
                    wt_ = WT[it % 3]
                    if idx + 2 < len(kts):
                        spq.append(qk(kts[idx + 2], it + 2))
                    it += 1
                    if diag:
                        P.ts(dt_[:, 0:w], BCs[:, 0:w], UK[:, kt, c:c + 1], c60[:, 0:1], ALU.add, ALU.min, r=[BCs, UK, c60], w=[dt_])
                        P.act(dt_[:, 0:w], dt_[:, 0:w], AF.Exp, r=[dt_], w=[dt_])
                        P.tt(WM[:, 0:w], sp[:, 0:w], dt_[:, 0:w], ALU.mult, r=[sp, dt_], w=[WM])
                        mk = (maskf if d == 0 else maskb)
                        P.tt(wt_[:, 0:w], WM[:, 0:w], mk[:, (kt * 128 - t0) // 128, 0:w], ALU.mult, r=[WM, mk], w=[wt_])
                    else:
                        P.act(dt_[:, 0:w], BCs[:, 0:w], AF.Exp, r=[BCs, UK], w=[dt_], bias=UK[:, kt, c:c + 1])
                        P.tt(wt_[:, 0:w], sp[:, 0:w], dt_[:, 0:w], ALU.mult, r=[sp, dt_], w=[wt_])
                    P.mm(accT[0:65, 0:w], Vp[:, kt, hl, :], wt_[:, 0:w], start=(idx == 0), stop=(idx == len(kts) - 1),
                         r=[wt_, Vp], w=[accT])
                P.cp(oTm[0:65, 0:w], accT[0:65, 0:w], r=[accT], w=[oTm])
                acc = ps[5 + d]
                for qi in range(nq):
                    P.tr(acc[:, qi * 65:(qi + 1) * 65], oTm[0:65, qi * 128:(qi + 1) * 128], ident[0:65, 0:65],
                         r=[oTm, ident], w=[acc])
                a3 = acc[:, 0:nq * 65].rearrange("p (q c) -> p q c", c=65)
                P.cp(den[:, 0:nq, :], a3[:, :, 64:65], r=[acc], w=[den])
                P.ts(nden[:, 0:nq, :], den[:, 0:nq, :], -1.0, None, ALU.mult, r=[den], w=[nden])
                P.tt(den[:, 0:nq, :], den[:, 0:nq, :], nden[:, 0:nq, :], ALU.max, r=[den, nden], w=[den])
                P.ts(den[:, 0:nq, :], den[:, 0:nq, :], 1.0, None, ALU.max, r=[den], w=[den])
                P.call("vector", "reciprocal", den[:, 0:nq, :], den[:, 0:nq, :], r=[den], w=[den])
                P.tt(hd[d][:, 0:nq, :], a3[:, :, 0:64], den[:, 0:nq, :].to_broadcast([128, nq, 64]), ALU.mult,
                     r=[acc, den], w=[hd[d]])
            h_ = hd[0]
            P.tt(h_[:, 0:nq, :], hd[0][:, 0:nq, :], hd[1][:, 0:nq, :], ALU.add, r=hd, w=[hd[0]])
            y_ = yo[hl]
            for qi in range(nq):
                P.call("vector", "bn_stats", st[:, qi, :], h_[:, qi, :], r=[h_], w=[st])
                P.call("vector", "bn_aggr", mv[:, qi, :], st[:, qi, :], r=[st], w=[mv])
            P.act(mv[:, 0:nq, 1:2], mv[:, 0:nq, 1:2], AF.Sqrt, r=[mv], w=[mv], bias=1e-6)
            P.call("vector", "reciprocal", mv[:, 0:nq, 1:2], mv[:, 0:nq, 1:2], r=[mv], w=[mv])
            for qi in range(nq):
                P.ts(y_[:, qi, :], h_[:, qi, :], mv[:, qi, 0:1], mv[:, qi, 1:2], ALU.subtract, ALU.mult, r=[h_, mv], w=[y_])
                P.tt(y_[:, qi, :], y_[:, qi, :], normw[:, hl * 64:(hl + 1) * 64], ALU.mult, r=[y_, normw], w=[y_])
                P.tt(y_[:, qi, :], y_[:, qi, :], og[:, tq0 + qi, hl * 64:(hl + 1) * 64], ALU.mult, r=[y_, og], w=[y_])
                r0 = t0 + qi * 128
                P.dma("sync", ymix[r0:r0 + 128, hl * 64:(hl + 1) * 64], y_[:, qi, :], r=[y_])


TB = 2176
NTB = 17
ALPHA = 4.0 ** 0.25
BBLOCKS = [(0, 512), (512, 512), (1024, 512), (1536, 512), (2048, 128)]


def build_B():
    nc = bass.Bass("TRN2", target_bir_lowering=False)
    P = Prog(nc)

    def din(name, shape, dt=F32):
        return nc.dram_tensor(name, list(shape), dt, kind="ExternalInput").ap()
    xres = din("xres", [TB, D])
    ymx = din("ymx", [TB, D])
    ident_d = din("ident", [128, 128])
    out = nc.dram_tensor("xout", [TB, D], F32, kind="ExternalOutput").ap()
    ident = P.sb("ident", [128, 128], F32)
    P.dma("sync", ident[:], ident_d[:, :], w=[ident])
    ones32 = P.sb("ones32", [128, 128], F32)
    P.memset(ones32[:], 1.0, w=[ones32])
    ps = [P.ps("ps%d" % i, [128, 512]) for i in range(8)]

    def xres_fn(tt, buf):
        P.dma("sync", buf[:], xres[tt * 128:(tt + 1) * 128, :], w=[buf])

    def ymx_fn(tt, buf, tmp):
        P.dma("sync", buf[:], ymx[tt * 128:(tt + 1) * 128, :], w=[buf])

    def out_fn(tt):
        return out[tt * 128:(tt + 1) * 128, :], []
    phase_B(nc, P, ps, ident, ones32, din, xres_fn, ymx_fn, out_fn)
    P.emit()
    return nc


def phase_B(nc, P, ps, ident, ones32, din, xres_fn, ymx_fn, out_fn, pre=""):
    cT = din("cT", [128, 8, 2]) if not pre else din.shared("cT", [128, 8, 2])
    wmodB = din("wmodB", [D, 4096])
    bmodrow = din("bmodrow", [128, 4096])
    bmodcol = din("bmodcol", [128, 32])
    wout_d = din("wout", [D, D])
    lnrow = din("lnrow", [128, 4, D])
    rw_d = din("rw", [D, 64])
    rb_d = din("rbias", [128, 64])
    eg_d = din("eg", [65, D, 256])
    eu_d = din("eu", [65, D, 256])
    ed_d = din("ed", [65, 256, D])
    x1d = nc.dram_tensor(pre + "x1scr", [TB, D], F32).ap()

    csil = P.sb("csil", [128, 8, 2], F32)
    P.dma("sync", csil[:], cT[:, :, :], w=[csil])
    P.act(csil[:], csil[:], AF.Silu, r=[csil], w=[csil])
    bmc = P.sb("bmc", [128, 32], F32)
    P.dma("sync", bmc[:], bmodcol[:, :], w=[bmc])
    bigall = P.sb("bigall", [128, 4, D], F32)

    class _V:
        def __init__(self, i):
            self.i = i
            self.res = Res()

        def __getitem__(self, k):
            return bigall[:, self.i, :][k] if not isinstance(k, tuple) else bigall[(k[0], self.i) + tuple(k[1:])]
    big = [_V(i) for i in range(4)]
    bigres = [b_.res for b_ in big]
    bigall2 = P.sb("bigall2", [128, 4, D], F32)

    class _V2(_V):
        def __getitem__(self, k):
            return bigall2[:, self.i, :][k] if not isinstance(k, tuple) else bigall2[(k[0], self.i) + tuple(k[1:])]
    big2 = [_V2(i) for i in range(4)]
    modc = P.sb("modc", [128, 16, 2], F32)
    for oc in range(16):
        wq = bigall[:, oc % 2, :].rearrange("p (k c) -> p k c", c=128)
        wqr = bigres[oc % 2]
        c0 = 1024 + oc * 128
        P.dma("sync", wq, wmodB[:, c0:c0 + 128].rearrange("(k p) c -> p k c", p=128), w=[wqr])
        pt = ps[oc % 2]
        for kc in range(8):
            P.mm(pt[:, 0:2], wq[:, kc, :], csil[:, kc, :], start=(kc == 0), stop=(kc == 7), r=[wqr, csil], w=[pt])
        P.act(modc[:, oc, :], pt[:, 0:2], AF.Identity, r=[pt, bmc], w=[modc], bias=bmc[:, 8 + oc:9 + oc])
    P.ts(modc[:, 8:16, :], modc[:, 8:16, :], 1.0, None, ALU.add, r=[modc], w=[modc])
    crep = P.sb("crep", [128, 8, 128], F32)
    growb = [P.sb("grow%d" % m, [128, D], F32) for m in range(2)]
    grow = [[growb[m], growb[m]] for m in range(2)]
    lnr2 = P.sb("lnr", [128, 2, D], F32)

    class _L:
        res = lnr2.res

        def __getitem__(self, k):
            return lnr2[(k[0], k[1] % 2) + tuple(k[2:])]
    lnr = _L()

    def make_gates(k):
        cbase = (0, 3072)[k]
        wrow = bigall[:, 0:2, :].rearrange("p a (k c) -> p (a k) c", c=512)
        for half in range(2):
            c0 = cbase + half * 512
            for m in range(2):
                pt = ps[2 + m]
                for kh in range(2):
                    P.dma("sync", wrow, wmodB[kh * 512:(kh + 1) * 512, c0:c0 + 512].rearrange("(k p) c -> p k c", p=128),
                          w=bigres[0:2])
                    for k4 in range(4):
                        kc = kh * 4 + k4
                        P.ts(crep[:, kc, :], ones32[:], csil[:, kc, m:m + 1], None, ALU.mult, r=[ones32, csil], w=[crep])
                        P.mm(pt[:, :], crep[:, kc, :], wrow[:, k4, :], start=(kc == 0), stop=(kc == 7),
                             r=[crep] + bigres[0:2], w=[pt])
                P.dma("sync", big[2][:, 0:512], bmodrow[:, c0:c0 + 512], w=[big[2]])
                P.tt(growb[m][:, half * 512:(half + 1) * 512], pt[:, :], big[2][:, 0:512], ALU.add,
                     r=[pt, big[2]], w=[growb[m]])
        P.dma("sync", lnr2[:], lnrow[:, 2 * k:2 * k + 2, :], w=[lnr2])
    make_gates(0)
    h2T = P.sb("h2T", [128, 8, TB], BF16)
    GT = P.sb("GT", [128, NTB, 65], F32)
    P.memset(GT[:], 1.0, w=[GT])
    acc = P.sb("acc", [128, NTB, D], F32)
    msub = P.mark()
    woutb = P.sb("woutb", [128, 8, D], BF16)
    for kc in range(8):
        P.dma("gpsimd", woutb[:, kc, :], wout_d[kc * 128:(kc + 1) * 128, :], w=[woutb])
    rw = P.sb("rw", [128, 8, 64], F32)
    P.dma("sync", rw[:], rw_d.rearrange("(k p) e -> p k e", p=128), w=[rw])
    rbias = P.sb("rbias", [128, 64], F32)
    P.dma("sync", rbias[:], rb_d[:, :], w=[rbias])

    yT = P.sb("yT", [128, 8, 128], BF16)
    h32 = P.sb("h32", [128, 8, 128], F32)
    st = P.sb("st", [128, 12], F32)
    mv = P.sb("mv", [128, 2], F32)
    rs = P.sb("rs", [128, 1], F32)
    sc = P.sb("sc", [128, 64], F32)
    bi = P.sb("bi", [128, 64], F32)
    m8 = P.sb("m8", [128, 8, 8], F32)
    gs = P.sb("gs", [128, 8], F32)
    gm = P.sb("gm", [128, 8], F32)
    t64 = P.sb("t64", [128, 64], F32)
    dsum = P.sb("dsum", [128, 1], F32)

    def layernorm(dst, src, srcres):
        P.call("vector", "bn_stats", st[:, 0:6], src[:, 0:512], r=srcres, w=[st])
        P.call("vector", "bn_stats", st[:, 6:12], src[:, 512:1024], r=srcres, w=[st])
        P.call("vector", "bn_aggr", mv[:], st[:], r=[st], w=[mv])
        P.act(rs[:], mv[:, 1:2], AF.Sqrt, r=[mv], w=[rs], bias=1e-6)
        P.call("vector", "reciprocal", rs[:], rs[:], r=[rs], w=[rs])
        P.ts(dst[:], src[:], mv[:, 0:1], rs[:, 0:1], ALU.subtract, ALU.mult, r=srcres + [mv, rs], w=[dst])

    for tt in range(NTB):
        m = 1 if tt == 0 else 0
        xr, ym, u, x1 = (big if tt % 2 == 0 else big2)
        xres_fn(tt, xr)
        ymx_fn(tt, ym, u)
        for half in range(2):
            pt = ps[half]
            for j4 in range(4):
                j = half * 4 + j4
                P.tr(pt[:, j4 * 128:(j4 + 1) * 128], ym[:, j * 128:(j + 1) * 128], ident[:], r=[ym, ident], w=[pt])
            P.cp(yT[:, half * 4:half * 4 + 4, :], pt[:, :].rearrange("p (j c) -> p j c", j=4), r=[pt], w=[yT])
        for half in range(2):
            pt = ps[2 + half]
            for kc in range(8):
                P.mm(pt[:, :], yT[:, kc, :], woutb[:, kc, half * 512:(half + 1) * 512], start=(kc == 0), stop=(kc == 7),
                     r=[yT, woutb], w=[pt])
            sl = slice(half * 512, (half + 1) * 512)
            P.tt(u[:, sl], pt[:, :], grow[m][0][:, sl], ALU.mult, r=[pt, grow[m][0]], w=[u])
            P.stt(u[:, sl], xr[:, sl], ALPHA, u[:, sl], ALU.mult, ALU.add, r=[xr, u], w=[u])
        layernorm(x1, u, [u])
        P.tt(x1[:], x1[:], lnr[:, 0, :], ALU.mult, r=[x1, lnr], w=[x1])
        P.tt(x1[:], x1[:], lnr[:, 1, :], ALU.add, r=[x1, lnr], w=[x1])
        P.dma("sync", x1d[tt * 128:(tt + 1) * 128, :], x1[:], r=[x1])
        layernorm(u, x1, [x1])
        for half in range(2):
            pt = ps[half]
            for j4 in range(4):
                j = half * 4 + j4
                P.tr(pt[:, j4 * 128:(j4 + 1) * 128], u[:, j * 128:(j + 1) * 128], ident[:], r=[u, ident], w=[pt])
            for j4 in range(4):
                j = half * 4 + j4
                src = pt[:, j4 * 128:(j4 + 1) * 128]
                if j4 > 0:
                    P.cp(h32[:, j, :], src, r=[pt], w=[h32])
                    src = h32[:, j, :]
                P.act(h32[:, j, :], src, AF.Identity, r=[pt, modc, h32], w=[h32],
                      scale=modc[:, 8 + j, m:m + 1], bias=modc[:, j, m:m + 1])
        P.cp(h2T[:, :, tt * 128:(tt + 1) * 128], h32[:], r=[h32], w=[h2T])
        pr = ps[4]
        for kc in range(8):
            P.mm(pr[:, 0:64], h32[:, kc, :], rw[:, kc, :], start=(kc == 0), stop=(kc == 7), r=[h32, rw], w=[pr])
        P.cp(sc[:], pr[:, 0:64], r=[pr], w=[sc])
        P.act(sc[:], sc[:], AF.Exp, r=[sc], w=[sc], scale=-1.0)
        P.ts(sc[:], sc[:], 1.0, None, ALU.add, r=[sc], w=[sc])
        P.call("vector", "reciprocal", sc[:], sc[:], r=[sc], w=[sc])
        P.tt(bi[:], sc[:], rbias[:], ALU.add, r=[sc, rbias], w=[bi])
        for g in range(8):
            P.call("vector", "max", m8[:, g, :], bi[:, g * 8:(g + 1) * 8], r=[bi], w=[m8])
        P.tt(gs[:], m8[:, :, 0], m8[:, :, 1], ALU.add, r=[m8], w=[gs])
        P.call("vector", "max", m8[:, 0, :], gs[:], r=[gs], w=[m8])
        P.ts(gm[:], gs[:], m8[:, 0, 3:4], None, ALU.is_ge, r=[gs, m8], w=[gm])
        b3 = bi[:].rearrange("p (g e) -> p g e", g=8)
        t3 = t64[:].rearrange("p (g e) -> p g e", g=8)
        P.tt(t3, b3, gm[:].unsqueeze(2).to_broadcast([128, 8, 8]), ALU.mult, r=[bi, gm], w=[t64])
        P.ts(gm[:], gm[:], 1.0, 1e9, ALU.subtract, ALU.mult, r=[gm], w=[gm])
        P.tt(t3, t3, gm[:].unsqueeze(2).to_broadcast([128, 8, 8]), ALU.add, r=[t64, gm], w=[t64])
        P.call("vector", "max", m8[:, 1, :], t64[:], r=[t64], w=[m8])
        P.ts(t64[:], t64[:], m8[:, 1, 7:8], None, ALU.is_ge, r=[t64, m8], w=[t64])
        P.tt(t64[:], t64[:], sc[:], ALU.mult, r=[t64, sc], w=[t64])
        P.call("vector", "tensor_reduce", dsum[:], t64[:], AX.X, ALU.add, r=[t64], w=[dsum])
        P.call("vector", "reciprocal", dsum[:], dsum[:], r=[dsum], w=[dsum])
        P.ts(GT[:, tt, 0:64], t64[:], dsum[:, 0:1], 2.5, ALU.mult, ALU.mult, r=[t64, dsum], w=[GT])

    P.release(msub)
    wg = [P.sb("wg%d" % i, [128, 8, 256], BF16) for i in range(2)]
    wu = [P.sb("wu%d" % i, [128, 8, 256], BF16) for i in range(2)]
    wd = [P.sb("wd%d" % i, [128, 2, D], BF16) for i in range(2)]
    sg = [P.sb("sg%d" % i, [128, 512], F32) for i in range(2)]
    aT = [P.sb("aT%d" % i, [128, 2, 512], BF16) for i in range(2)]
    items = []
    for e in range(65):
        for (t0, w) in BBLOCKS:
            items.append((e, t0, w))
    loaded = set()

    def gu(n, fc):
        e, t0, w = items[n]
        g_, u_, d_ = wg[e % 2], wu[e % 2], wd[e % 2]
        if e not in loaded:
            loaded.add(e)
            P.dma("gpsimd", g_[:], eg_d[e].rearrange("(k p) f -> p k f", p=128), w=[g_])
            P.dma("gpsimd", u_[:], eu_d[e].rearrange("(k p) f -> p k f", p=128), w=[u_])
            P.dma("gpsimd", d_[:], ed_d[e].rearrange("(k p) f -> p k f", p=128), w=[d_])
        a_ = aT[n % 2]
        pg, pu = ps[fc * 2], ps[fc * 2 + 1]
        for kc in range(8):
            P.mm(pg[:, 0:w], g_[:, kc, fc * 128:(fc + 1) * 128], h2T[:, kc, t0:t0 + w],
                 start=(kc == 0), stop=(kc == 7), r=[g_, h2T], w=[pg])
        for kc in range(8):
            P.mm(pu[:, 0:w], u_[:, kc, fc * 128:(fc + 1) * 128], h2T[:, kc, t0:t0 + w],
                 start=(kc == 0), stop=(kc == 7), r=[u_, h2T], w=[pu])
        s_ = sg[fc]
        P.act(s_[:, 0:w], pg[:, 0:w], AF.Silu, r=[pg], w=[s_])
        P.tt(a_[:, fc, 0:w], pu[:, 0:w], s_[:, 0:w], ALU.mult, r=[pu, s_], w=[a_])

    def down(n):
        e, t0, w = items[n]
        d_ = wd[e % 2]
        a_ = aT[n % 2]
        for ti in range(w // 128):
            tt = t0 // 128 + ti
            for half in range(2):
                pd = ps[4 + (ti * 2 + half) % 4]
                for fc in range(2):
                    P.mm(pd[:, :], a_[:, fc, ti * 128:(ti + 1) * 128], d_[:, fc, half * 512:(half + 1) * 512],
                         start=(fc == 0), stop=(fc == 1), r=[a_, d_], w=[pd])
                sl = slice(half * 512, (half + 1) * 512)
                if e == 0:
                    P.ts(acc[:, tt, sl], pd[:, :], GT[:, tt, e:e + 1], None, ALU.mult, r=[pd, GT], w=[acc.sub(tt)])
                else:
                    P.stt(acc[:, tt, sl], pd[:, :], GT[:, tt, e:e + 1], acc[:, tt, sl], ALU.mult, ALU.add,
                          r=[pd, GT, acc.sub(tt)], w=[acc.sub(tt)])

    gu(0, 0)
    gu(0, 1)
    for n in range(len(items)):
        if n + 1 < len(items):
            gu(n + 1, 0)
        down(n)
        if n + 1 < len(items):
            gu(n + 1, 1)

    make_gates(1)
    for tt in range(NTB):
        m = 1 if tt == 0 else 0
        x1, u, o_, _ = (big if tt % 2 == 0 else big2)
        P.dma("sync", x1[:], x1d[tt * 128:(tt + 1) * 128, :], w=[x1])
        P.tt(u[:], acc[:, tt, :], grow[m][1][:], ALU.mult, r=[acc.sub(tt), grow[m][1]], w=[u])
        P.stt(u[:], x1[:], ALPHA, u[:], ALU.mult, ALU.add, r=[x1, u], w=[u])
        layernorm(o_, u, [u])
        P.tt(o_[:], o_[:], lnr[:, 2, :], ALU.mult, r=[o_, lnr], w=[o_])
        P.tt(o_[:], o_[:], lnr[:, 3, :], ALU.add, r=[o_, lnr], w=[o_])
        oap, ores = out_fn(tt)
        P.dma("sync", oap, o_[:], r=[o_], w=ores)


def prep_B(inp, l, x_cur, xc_cur, ymix_full):
    maps = []
    ident = np.eye(128, dtype=np.float32)
    bm = inp["b_mod"][l][2048:6144]
    for core in range(8):
        b, hf = core // 2, core % 2
        rows_c = slice(128 * hf, 128 * hf + 128)
        rows_l = slice(2048 * hf, 2048 * hf + 2048)
        m = {}
        m["xres"] = np.ascontiguousarray(np.concatenate([xc_cur[b][rows_c], x_cur[b][rows_l]], 0))
        ym = ymix_full[b]
        m["ymx"] = np.ascontiguousarray(np.concatenate([ym[0:256][rows_c], ym[256:][rows_l]], 0))
        cc = np.stack([inp["c"][b], inp["c_ctx"]], -1)
        m["cT"] = np.ascontiguousarray(cc.reshape(8, 128, 2).transpose(1, 0, 2))
        m["wmodB"] = np.ascontiguousarray(inp["w_mod"][l][:, 2048:6144])
        m["bmodrow"] = np.ascontiguousarray(np.broadcast_to(bm[None], (128, 4096)))
        m["bmodcol"] = _col_layout(bm)
        m["wout"] = inp["w_out"][l]
        m["lnrow"] = np.ascontiguousarray(np.broadcast_to(
            np.stack([inp["ln1_w"][l], inp["ln1_b"][l], inp["ln2_w"][l], inp["ln2_b"][l]])[None], (128, 4, D)))
        m["rw"] = inp["router_w"][l]
        m["rbias"] = np.ascontiguousarray(np.broadcast_to(inp["router_bias"][l][None], (128, 64)))
        m["eg"] = np.concatenate([inp["exp_w_gate"][l], inp["sh_w_gate"][l][None]], 0)
        m["eu"] = np.concatenate([inp["exp_w_up"][l], inp["sh_w_up"][l][None]], 0)
        m["ed"] = np.concatenate([inp["exp_w_down"][l], inp["sh_w_down"][l][None]], 0)
        m["ident"] = ident
        maps.append(m)
    return maps


R1_OUT = ("WT0", "WT1", "NKK", "B0", "B1", "KD0", "KD1", "R", "V", "G", "BV")


def build_rwkv(nc, P, din, ps, hT, hres, winb, proj_fm, ident, identb, onesb, ymix, emit_ctx, dbg, xt, r1sink=None):
    mubc_d = din("mubc", [128, 768])
    pcol_d = din("pcol", [128, 8])
    wup_d = din("wup", [128, 128])
    aup_d = din("aup", [128, 128])
    gup_d = din("gup", [128, 128])
    bones_d = din("bones", [128, 128])
    outs = {} if r1sink is not None else {n: nc.dram_tensor("o_" + n, [128, T], F32, kind="ExternalOutput").ap() for n in R1_OUT}

    mubc = P.sb("mubc", [128, 768], F32)
    P.dma("sync", mubc[:], mubc_d[:, :], w=[mubc])
    cb = P.sb("cb", [128, 2, 768], F32)
    P.ts(cb[:, 0, :], mubc[:], 0.5, None, ALU.mult, r=[mubc], w=[cb])
    P.ts(cb[:, 1, :], mubc[:], -1.0, None, ALU.mult, r=[mubc], w=[cb])
    P.ts(cb[:, 1, :], cb[:, 1, :], 1.0, None, ALU.add, r=[cb], w=[cb])
    wc = P.sb("wcr", [128, 8, 3, 768], BF16)
    for kc in range(8):
        for j in range(3):
            P.tt(wc[:, kc, j, :], winb[:, kc, :], cb[:, j % 2, :], ALU.mult, r=[winb, cb], w=[wc])
    pcol = P.sb("pcol", [128, 8], F32)
    P.dma("sync", pcol[:], pcol_d[:, :], w=[pcol])
    npc = P.sb("npc", [128, 8], F32)
    P.ts(npc[:], pcol[:], -1.0, None, ALU.mult, r=[pcol], w=[npc])
    wup = P.sb("wup", [128, 128], F32)
    P.dma("sync", wup[:], wup_d[:, :], w=[wup])
    aup = P.sb("aup", [128, 128], F32)
    P.dma("sync", aup[:], aup_d[:, :], w=[aup])
    gup = P.sb("gup", [128, 128], F32)
    P.dma("sync", gup[:], gup_d[:, :], w=[gup])
    bones = P.sb("bones", [128, 128], F32)
    P.dma("sync", bones[:], bones_d[:, :], w=[bones])
    cm1 = P.sb("cm1", [128, 1], F32)
    P.memset(cm1[:], -1.0, w=[cm1])

    Zs = [[P.sb("Z%d" % g, [128, 512], F32) for g in range(6)] for k in range(2)]
    tAs = [[P.sb("tA%d" % i, [128, 512], F32) for i in range(2)] for k in range(2)]
    tKs = [[P.sb("tK%d" % i, [128, 512], F32) for i in range(2)] for k in range(2)]
    t0s = [P.sb("t0", [128, 512], F32) for k in range(2)]
    t1s = [P.sb("t1", [128, 512], F32) for k in range(2)]
    t2s = [P.sb("t2", [128, 512], F32) for k in range(2)]
    ob = [P.sb("ob%d" % i, [128, 512], F32) for i in range(3)]
    io = [0]

    def emit(name, src, t0, w, srcres):
        if r1sink is not None:
            r1sink(name, src, t0, w, srcres)
            return
        P.dma("sync", outs[name][:, t0:t0 + w], src, r=srcres)

    def sigmoid_from_psum(dst, pt, w, negbias):
        if negbias is None:
            P.act(dst[:, 0:w], pt[:, 0:w], AF.Exp, r=[pt], w=[dst], scale=-1.0)
        else:
            P.act(dst[:, 0:w], pt[:, 0:w], AF.Exp, r=[pt, npc], w=[dst], scale=-1.0, bias=negbias)
        P.ts(dst[:, 0:w], dst[:, 0:w], 1.0, None, ALU.add, r=[dst], w=[dst])
        P.call("vector", "reciprocal", dst[:, 0:w], dst[:, 0:w], r=[dst], w=[dst])

    for bi, (t0, w) in enumerate(QBLOCKS):
        Z, tA, tK = Zs[bi % 2], tAs[bi % 2], tKs[bi % 2]
        t0_, t1_, t2_ = t0s[bi % 2], t1s[bi % 2], t2s[bi % 2]
        seg0 = t0 in (0, NCTX)
        seg1 = (t0 + w) in (NCTX, T)
        for g in range(6):
            pt = ps[g % 4]
            first = True
            for j in (1, 0, 2):
                a = 1 if (j == 0 and seg0) else 0
                b = 1 if (j == 2 and seg1) else 0
                for kc in range(8):
                    last = (j == 2 and kc == 7)
                    P.mm(pt[:, a:w - b], wc[:, kc, j, g * 128:(g + 1) * 128],
                         hT[:, kc, t0 + a + j - 1:t0 + w - b + j - 1],
                         start=first, stop=last, r=[wc] + hres, w=[pt], skip=True)
                    first = False
            P.cp(Z[g][:, 0:w], pt[:, 0:w], r=[pt], w=[Z[g]])
        zr, zk, zv, zw, za, zg = Z
        emit("R", zr[:, 0:w], t0, w, [zr])
        emit("V", zv[:, 0:w], t0, w, [zv])
        P.act(zw[:, 0:w], zw[:, 0:w], AF.Tanh, r=[zw], w=[zw])
        for d in range(2):
            pt = ps[4 + d]
            rows = slice(d * 64, d * 64 + 64)
            P.mm(pt[:, 0:w], wup[rows, :], zw[rows, 0:w], r=[wup, zw], w=[pt])
            o_ = ob[io[0] % 3]; io[0] += 1
            sigmoid_from_psum(o_, pt, w, npc[:, d:d + 1])
            P.act(o_[:, 0:w], o_[:, 0:w], AF.Exp, r=[o_], w=[o_], scale=-float(np.exp(-0.5)))
            emit("WT%d" % d, o_[:, 0:w], t0, w, [o_])
        for d in range(2):
            pt = ps[6 + d]
            rows = slice(d * 64, d * 64 + 64)
            P.mm(pt[:, 0:w], aup[rows, :], za[rows, 0:w], r=[aup, za], w=[pt])
            sigmoid_from_psum(tA[d], pt, w, npc[:, 2 + d:3 + d])
        sigmoid_from_psum(t0_, zg, w, None) if False else None
        P.act(t0_[:, 0:w], zg[:, 0:w], AF.Exp, r=[zg], w=[t0_], scale=-1.0)
        P.ts(t0_[:, 0:w], t0_[:, 0:w], 1.0, None, ALU.add, r=[t0_], w=[t0_])
        P.call("vector", "reciprocal", t0_[:, 0:w], t0_[:, 0:w], r=[t0_], w=[t0_])
        pt = ps[4]
        P.mm(pt[:, 0:w], gup[:], t0_[:, 0:w], r=[gup, t0_], w=[pt])
        o_ = ob[io[0] % 3]; io[0] += 1
        P.cp(o_[:, 0:w], pt[:, 0:w], r=[pt], w=[o_])
        emit("G", o_[:, 0:w], t0, w, [o_])
        P.ts(t1_[:, 0:w], zk[:, 0:w], pcol[:, 4:5], None, ALU.mult, r=[zk, pcol], w=[t1_])
        P.tt(t2_[:, 0:w], t1_[:, 0:w], t1_[:, 0:w], ALU.mult, r=[t1_], w=[t2_])
        pt = ps[5]
        P.mm(pt[:, 0:w], bones[:], t2_[:, 0:w], r=[bones, t2_], w=[pt])
        P.ts(t2_[:, 0:w], pt[:, 0:w], 1e-12, None, ALU.max, r=[pt], w=[t2_])
        P.act(t2_[:, 0:w], t2_[:, 0:w], AF.Sqrt, r=[t2_], w=[t2_])
        P.call("vector", "reciprocal", t2_[:, 0:w], t2_[:, 0:w], r=[t2_], w=[t2_])
        P.tt(t1_[:, 0:w], t1_[:, 0:w], t2_[:, 0:w], ALU.mult, r=[t1_, t2_], w=[t1_])
        o_ = ob[io[0] % 3]; io[0] += 1
        P.ts(o_[:, 0:w], t1_[:, 0:w], -1.0, None, ALU.mult, r=[t1_], w=[o_])
        emit("NKK", o_[:, 0:w], t0, w, [o_])
        for d in range(2):
            o_ = ob[io[0] % 3]; io[0] += 1
            P.tt(o_[:, 0:w], t1_[:, 0:w], tA[d][:, 0:w], ALU.mult, r=[t1_, tA[d]], w=[o_])
            emit("B%d" % d, o_[:, 0:w], t0, w, [o_])
            P.ts(tK[d][:, 0:w], tA[d][:, 0:w], cm1[:, 0:1], pcol[:, 5:6], ALU.add, ALU.mult, r=[tA[d], cm1, pcol], w=[tK[d]])
            P.stt(tK[d][:, 0:w], tK[d][:, 0:w], 1.0, zk[:, 0:w], ALU.add, ALU.mult, r=[tK[d], zk], w=[tK[d]])
            emit("KD%d" % d, tK[d][:, 0:w], t0, w, [tK[d]])
        P.tt(t2_[:, 0:w], tK[0][:, 0:w], tK[1][:, 0:w], ALU.add, r=tK, w=[t2_])
        P.stt(t2_[:, 0:w], zr[:, 0:w], pcol[:, 6:7], t2_[:, 0:w], ALU.mult, ALU.mult, r=[zr, pcol, t2_], w=[t2_])
        pt = ps[6]
        P.mm(pt[:, 0:w], bones[:], t2_[:, 0:w], r=[bones, t2_], w=[pt])
        o_ = ob[io[0] % 3]; io[0] += 1
        P.tt(o_[:, 0:w], pt[:, 0:w], zv[:, 0:w], ALU.mult, r=[pt, zv], w=[o_])
        emit("BV", o_[:, 0:w], t0, w, [o_])


def build_R2():
    nc = bass.Bass("TRN2", target_bir_lowering=False)
    P = Prog(nc)

    def din(name, shape, dt=F32):
        return nc.dram_tensor(name, list(shape), dt, kind="ExternalInput").ap()
    I = []
    for d in range(2):
        I.append(dict(nk2=din("nk2_%d" % d, [T, 2, 128]), bt=din("bt_%d" % d, [T, 128]),
                      kd2=din("kd2_%d" % d, [T, 2, 128]), vt=din("vt_%d" % d, [T, 128]),
                      wt=din("wt_%d" % d, [128, T]), rbd=din("rbd_%d" % d, [128, T, 2])))
    TS = 64
    mask_d = din("mask32", [128, 32])
    O = [nc.dram_tensor("O_%d" % d, [64, T, 2], F32, kind="ExternalOutput").ap() for d in range(2)]

    mask32 = P.sb("mask32", [128, 32], F32)
    P.dma("sync", mask32[:], mask_d[:, :], w=[mask32])
    S = [P.sb("S%d" % d, [128, 64], F32) for d in range(2)]
    Sb = [[P.sb("Sb%d%d" % (d, k), [128, 64], BF16) for k in range(2)] for d in range(2)]
    for d in range(2):
        P.memset(S[d][:], 0.0, w=[S[d]])
        P.memset(Sb[d][1][:], 0.0, w=[Sb[d][1]])
    TB_ = [[dict(nk2f=P.sb("nk2f_%d%d" % (d, k), [TS, 2, 128], F32), bt=P.sb("bt_%d%d" % (d, k), [TS, 128], F32),
                 kd2f=P.sb("kd2f_%d%d" % (d, k), [TS, 2, 128], F32), vt=P.sb("vt_%d%d" % (d, k), [TS, 128], F32),
                 rbdf=P.sb("rbdf_%d%d" % (d, k), [128, TS, 2], F32),
                 nk2=P.sb("nk2_%d%d" % (d, k), [TS, 2, 128], BF16), kd2=P.sb("kd2_%d%d" % (d, k), [TS, 2, 128], BF16),
                 wt=P.sb("wt_%d%d" % (d, k), [128, TS], F32), rbd=P.sb("rbd_%d%d" % (d, k), [128, TS, 2], BF16),
                 Bd=P.sb("Bd_%d%d" % (d, k), [TS, 32, 128], BF16), Vd=P.sb("Vd_%d%d" % (d, k), [TS, 32, 128], BF16))
            for k in range(2)] for d in range(2)]
    LS = [[P.sb("LS%d%d" % (d, k), [128, 128], BF16) for k in range(4)] for d in range(2)]
    osb = [P.sb("osb%d" % d, [64, 2 * TS], F32) for d in range(2)]
    LP = [[P.ps("LP%d%d" % (d, k), [128, 512]) for k in range(2)] for d in range(2)]
    SP = [P.ps("SP%d" % d, [128, 512]) for d in range(2)]
    OP = [P.ps("OP%d" % d, [128, 512]) for d in range(2)]

    def load(i):
        for d in range(2):
            tb = TB_[d][i % 2]
            r0 = i * TS
            P.dma("sync", tb["nk2f"][:], I[d]["nk2"][r0:r0 + TS, :, :], w=[tb["nk2f"]])
            P.dma("sync", tb["bt"][:], I[d]["bt"][r0:r0 + TS, :], w=[tb["bt"]])
            P.dma("sync", tb["kd2f"][:], I[d]["kd2"][r0:r0 + TS, :, :], w=[tb["kd2f"]])
            P.dma("sync", tb["vt"][:], I[d]["vt"][r0:r0 + TS, :], w=[tb["vt"]])
            P.dma("sync", tb["wt"][:], I[d]["wt"][:, r0:r0 + TS], w=[tb["wt"]])
            P.dma("sync", tb["rbdf"][:], I[d]["rbd"][:, r0:r0 + TS, :], w=[tb["rbdf"]])
            P.cp(tb["nk2"][:], tb["nk2f"][:], r=[tb["nk2f"]], w=[tb["nk2"]], eng="gpsimd")
            P.cp(tb["kd2"][:], tb["kd2f"][:], r=[tb["kd2f"]], w=[tb["kd2"]], eng="gpsimd")
            P.cp(tb["rbd"][:], tb["rbdf"][:], r=[tb["rbdf"]], w=[tb["rbd"]], eng="gpsimd")
            mb = mask32[0:TS, :].unsqueeze(2).to_broadcast([TS, 32, 128])
            P.tt(tb["Bd"][:], mb, tb["bt"][:].unsqueeze(1).to_broadcast([TS, 32, 128]), ALU.mult,
                 r=[mask32, tb["bt"]], w=[tb["Bd"]])
            P.tt(tb["Vd"][:], mb, tb["vt"][:].unsqueeze(1).to_broadcast([TS, 32, 128]), ALU.mult,
                 r=[mask32, tb["vt"]], w=[tb["Vd"]])

    def lgen(g):
        i, t = g // TS, g % TS
        rows = slice((t // 32) * 32, (t // 32) * 32 + 32)
        tl = t % 32
        for d in range(2):
            tb = TB_[d][i % 2]
            lp = LP[d][g % 2]
            ls = LS[d][g % 4]
            P.mm(lp[:, 0:64], tb["nk2"][rows, 0, :], tb["Bd"][rows, tl, 0:64], r=[tb["nk2"], tb["Bd"]], w=[lp])
            P.mm(lp[:, 64:128], tb["nk2"][rows, 1, :], tb["Bd"][rows, tl, 64:128], r=[tb["nk2"], tb["Bd"]], w=[lp])
            P.cp(ls[:], lp[:, 0:128], r=[lp], w=[ls], eng="scalar")

    def step(g):
        i, t = g // TS, g % TS
        rows = slice((t // 32) * 32, (t // 32) * 32 + 32)
        tl = t % 32
        for d in range(2):
            tb = TB_[d][i % 2]
            ls = LS[d][g % 4]
            sp = SP[d]
            sbo = Sb[d][(g + 1) % 2]
            P.mm(sp[:, 0:64], ls[:], sbo[:], start=True, stop=False, r=[ls, sbo], w=[sp])
            P.mm(sp[:, 0:64], tb["kd2"][rows, 0, :], tb["Vd"][rows, tl, 0:64], start=False, stop=False,
                 r=[tb["kd2"], tb["Vd"]], w=[sp])
            P.mm(sp[:, 0:64], tb["kd2"][rows, 1, :], tb["Vd"][rows, tl, 64:128], start=False, stop=True,
                 r=[tb["kd2"], tb["Vd"]], w=[sp])
        for d in range(2):
            tb = TB_[d][i % 2]
            sbn = Sb[d][g % 2]
            P.stt(sbn[:], S[d][:], tb["wt"][:, t:t + 1], SP[d][:, 0:64], ALU.mult, ALU.add,
                  r=[S[d], tb["wt"], SP[d]], w=[sbn])
        for d in range(2):
            tb = TB_[d][i % 2]
            P.mm(OP[d][0:64, 2 * t:2 * t + 2], Sb[d][g % 2][:], tb["rbd"][:, t, :], r=[Sb[d][g % 2], tb["rbd"]], w=[OP[d]])
        for d in range(2):
            tb = TB_[d][i % 2]
            P.stt(S[d][:], S[d][:], tb["wt"][:, t:t + 1], SP[d][:, 0:64], ALU.mult, ALU.add,
                  r=[S[d], tb["wt"], SP[d]], w=[S[d]])

    LA = 2
    load(0)
    for g in range(LA):
        lgen(g)
    for g in range(T):
        if g % TS == 0 and g // TS + 1 < T // TS:
            load(g // TS + 1)
        if g + LA < T:
            lgen(g + LA)
        step(g)
        if g % TS == TS - 1:
            i = g // TS
            for d in range(2):
                P.cp(osb[d][:], OP[d][0:64, 0:2 * TS], r=[OP[d]], w=[osb[d]])
                P.dma("sync", O[d][:, i * TS:(i + 1) * TS, :], osb[d][:].rearrange("p (t h) -> p t h", h=2), r=[osb[d]])
    P.emit()
    return nc


def build_R3():
    nc = bass.Bass("TRN2", target_bir_lowering=False)
    P = Prog(nc)

    def din(name, shape, dt=F32):
        return nc.dram_tensor(name, list(shape), dt, kind="ExternalInput").ap()
    of_d = din("of", [T, 128])
    ob_d = din("ob", [T, 128])
    g_d = din("g", [T, 128])
    bv_d = din("bv", [T, 128])
    ln_d = din("lnrow", [128, 2, 128])
    y_d = nc.dram_tensor("ymix", [T, 128], F32, kind="ExternalOutput").ap()
    ln = P.sb("ln", [128, 2, 128], F32)
    P.dma("sync", ln[:], ln_d[:, :, :], w=[ln])
    bufs = [[P.sb("r3_%d%d" % (k, i), [128, 128], F32) for i in range(4)] for k in range(2)]
    st = P.sb("st", [128, 2, 6], F32)
    mv = P.sb("mv", [128, 2, 2], F32)
    for tt in range(NT):
        of, ob, g, bv = bufs[tt % 2]
        r0 = tt * 128
        P.dma("sync", of[:], of_d[r0:r0 + 128, :], w=[of])
        P.dma("sync", ob[:], ob_d[r0:r0 + 128, :], w=[ob])
        P.dma("sync", g[:], g_d[r0:r0 + 128, :], w=[g])
        P.dma("sync", bv[:], bv_d[r0:r0 + 128, :], w=[bv])
        P.tt(of[:], of[:], ob[:], ALU.add, r=[of, ob], w=[of])
        for h in range(2):
            P.call("vector", "bn_stats", st[:, h, :], of[:, h * 64:(h + 1) * 64], r=[of], w=[st])
            P.call("vector", "bn_aggr", mv[:, h, :], st[:, h, :], r=[st], w=[mv])
        P.act(mv[:, :, 1:2], mv[:, :, 1:2], AF.Sqrt, r=[mv], w=[mv], bias=64e-5)
        P.call("vector", "reciprocal", mv[:, :, 1:2], mv[:, :, 1:2], r=[mv], w=[mv])
        for h in range(2):
            sl = slice(h * 64, (h + 1) * 64)
            P.ts(of[:, sl], of[:, sl], mv[:, h, 0:1], mv[:, h, 1:2], ALU.subtract, ALU.mult, r=[of, mv], w=[of])
        P.tt(of[:], of[:], ln[:, 0, :], ALU.mult, r=[of, ln], w=[of])
        P.tt(of[:], of[:], ln[:, 1, :], ALU.add, r=[of, ln], w=[of])
        P.tt(of[:], of[:], bv[:], ALU.add, r=[of, bv], w=[of])
        P.tt(of[:], of[:], g[:], ALU.mult, r=[of, g], w=[of])
        P.dma("sync", y_d[r0:r0 + 128, :], of[:], r=[of])
    P.emit()
    return nc


_PERM_B = np.concatenate([np.arange(255, -1, -1), np.arange(T - 1, 255, -1)])


def prep_R2(r1res):
    maps = []
    mask32 = (np.arange(128)[:, None] % 32 == np.arange(32)[None, :]).astype(np.float32)
    for core in range(8):
        o = r1res[core]
        m = {"mask32": mask32}
        for d in range(2):
            perm = np.arange(T) if d == 0 else _PERM_B

            def tm(a):
                return np.ascontiguousarray(a.T[perm])

            def mask2(a_tm):
                out = np.zeros((T, 2, 128), np.float32)
                out[:, 0, 0:64] = a_tm[:, 0:64]
                out[:, 1, 64:128] = a_tm[:, 64:128]
                return out
            m["nk2_%d" % d] = mask2(tm(o["o_NKK"]))
            m["bt_%d" % d] = tm(o["o_B%d" % d])
            m["kd2_%d" % d] = mask2(tm(o["o_KD%d" % d]))
            m["vt_%d" % d] = tm(o["o_V"])
            m["wt_%d" % d] = np.ascontiguousarray(o["o_WT%d" % d][:, perm])
            r = o["o_R"][:, perm]
            rbd = np.zeros((128, T, 2), np.float32)
            rbd[0:64, :, 0] = r[0:64]
            rbd[64:128, :, 1] = r[64:128]
            m["rbd_%d" % d] = rbd
        maps.append(m)
    return maps


def prep_R3(inp, l, r1res, r2res):
    maps = []
    inv = np.argsort(_PERM_B)
    for core in range(8):
        hp = core % 2
        hc = slice(128 * hp, 128 * hp + 128)
        o0 = r2res[core]["O_0"]
        o1 = r2res[core]["O_1"][:, inv, :]
        m = {}
        m["of"] = np.ascontiguousarray(o0.transpose(1, 2, 0).reshape(T, 128))
        m["ob"] = np.ascontiguousarray(o1.transpose(1, 2, 0).reshape(T, 128))
        m["g"] = np.ascontiguousarray(r1res[core]["o_G"].T)
        m["bv"] = np.ascontiguousarray(r1res[core]["o_BV"].T)
        m["lnrow"] = np.ascontiguousarray(np.broadcast_to(
            np.stack([inp["rwkv_ln_w"][l][hc], inp["rwkv_ln_b"][l][hc]])[None], (128, 2, 128)))
        maps.append(m)
    return maps


def run_rwkv(inp, l, x, xc, emit_ctx=True):
    cores = list(range(8))
    r1 = run_bass_kernel_spmd(build_A("rwkv", emit_ctx), prep_A(inp, l, x, xc, "rwkv"), core_ids=cores).results
    r2 = run_bass_kernel_spmd(build_R2(), prep_R2(r1), core_ids=cores).results
    r3 = run_bass_kernel_spmd(build_R3(), prep_R3(inp, l, r1, r2), core_ids=cores).results
    return [r3[c]["ymix"] for c in cores]


def _kernel_unfused(inp, nlayers=2):
    inp = {k: np.asarray(v) for k, v in inp.items()}
    x = inp["x"].astype(np.float32)
    xc = inp["ctx"].astype(np.float32)
    cores = list(range(8))
    for l in range(nlayers):
        emit_ctx = (l == 0)
        ymix = np.zeros((4, T, 1024), np.float32)
        for kind, c0, W in (("mlstm", 0, 128), ("mla", 512, 256)):
            nc = build_A(kind, emit_ctx)
            res = run_bass_kernel_spmd(nc, prep_A(inp, l, x, xc, kind), core_ids=cores)
            for core in cores:
                b, hp = core // 2, core % 2
                ymix[b][:, c0 + hp * W:c0 + (hp + 1) * W] = res.results[core]["ymix"]
        yr = run_rwkv(inp, l, x, xc, emit_ctx)
        for core in cores:
            b, hp = core // 2, core % 2
            ymix[b][:, 256 + hp * 128:256 + (hp + 1) * 128] = yr[core]
        resB = run_bass_kernel_spmd(build_B(), prep_B(inp, l, x, xc, ymix), core_ids=cores)
        x_new = np.empty_like(x)
        xc_new = np.empty_like(xc)
        for core in cores:
            b, hf = core // 2, core % 2
            o = resB.results[core]["xout"]
            xc_new[b][128 * hf:128 * hf + 128] = o[0:128]
            x_new[b][2048 * hf:2048 * hf + 2048] = o[128:]
        x, xc = x_new, xc_new
    return x.astype(np.float32)


GROUPS = [[0, 1], [2, 3], [4, 5], [6, 7]]
DIAG_ENG = "gpsimd"
TM_NAMES = ("NKK", "B0", "B1", "KD0", "KD1", "V", "G", "BV")


def _half_rows(tt):
    if tt < 2:
        return tt, 0
    k = tt - 2
    return k // 16, 128 + (k % 16) * 128


def fused_rwkv_scan(nc, P, ps, ident, scr, sres, ymix, lnrow_d, emit_ctx):
    TS = 32
    m0 = P.mark()
    mask32 = P.sb("mask32", [64, 32], F32)
    for q in range(2):
        P.cp(mask32[q * 32:(q + 1) * 32, :], ident[q * 32:(q + 1) * 32, q * 32:(q + 1) * 32], r=[ident], w=[mask32])
    S = [P.sb("S%d" % d, [128, 64], F32) for d in range(2)]
    Sb = [[P.sb("Sb%d%d" % (d, k), [128, 64], BF16) for k in range(2)] for d in range(2)]
    for d in range(2):
        P.memset(S[d][:], 0.0, w=[S[d]])
        P.memset(Sb[d][1][:], 0.0, w=[Sb[d][1]])
    TB_ = [[dict(nkf=P.sb("nkf", [64, 128], F32), kdf=P.sb("kdf", [64, 128], F32),
                 btf=P.sb("btf", [64, 128], F32), vtf=P.sb("vtf", [64, 64], F32),
                 rf=P.sb("rf", [128, TS], F32), wt=P.sb("wt", [128, TS], F32),
                 nk=P.sb("nk", [64, 128], BF16), kd=P.sb("kd", [64, 128], BF16),
                 rbd=P.sb("rbd", [128, TS, 2], BF16),
                 Bd=P.sb("Bd", [64, 32, 128], BF16), Vd=P.sb("Vd", [64, 32, 64], BF16))
            for k in range(2)] for d in range(2)]
    for d in range(2):
        for k in range(2):
            tb = TB_[d][k]
            for n in ("nkf", "kdf", "btf"):
                P.memset(tb[n][:], 0.0, w=[tb[n]])
            P.memset(tb["rbd"][:], 0.0, w=[tb["rbd"]])
    LS = [[P.sb("LS", [128, 512], BF16) for k in range(3)] for d in range(2)]
    osb = [P.sb("osb", [64, 2 * TS], F32) for d in range(2)]
    LP = [[ps[0], ps[1]], [ps[2], ps[3]]]
    SP = [ps[4], ps[5]]
    OP = [ps[6], ps[7]]
    perm_b = _PERM_B

    def lo_of(d, i):
        return i * TS if d == 0 else int(perm_b[i * TS + TS - 1])

    def loc(d, t):
        return t if d == 0 else TS - 1 - t

    def load(i):
        for d in range(2):
            tb = TB_[d][i % 2]
            lo = lo_of(d, i)
            for (dst, name) in (("nkf", "NKK"), ("kdf", "KD%d" % d), ("btf", "B%d" % d)):
                for h in range(2):
                    P.dma("sync", tb[dst][h * 32:(h + 1) * 32, h * 64:(h + 1) * 64],
                          scr[name][lo:lo + TS, h * 64:(h + 1) * 64], r=[sres[name]], w=[tb[dst]])
            for h in range(2):
                P.dma("sync", tb["vtf"][h * 32:(h + 1) * 32, :], scr["V"][lo:lo + TS, h * 64:(h + 1) * 64],
                      r=[sres["V"]], w=[tb["vtf"]])
            P.dma("sync", tb["wt"][:], scr["WT%d" % d][:, lo:lo + TS], r=[sres["WT%d" % d]], w=[tb["wt"]])
            P.dma("sync", tb["rf"][:], scr["R"][:, lo:lo + TS], r=[sres["R"]], w=[tb["rf"]])
            P.cp(tb["nk"][:], tb["nkf"][:], r=[tb["nkf"]], w=[tb["nk"]], eng="gpsimd")
            P.cp(tb["kd"][:], tb["kdf"][:], r=[tb["kdf"]], w=[tb["kd"]], eng="gpsimd")
            P.cp(tb["rbd"][0:64, :, 0], tb["rf"][0:64, :], r=[tb["rf"]], w=[tb["rbd"]], eng="gpsimd")
            P.cp(tb["rbd"][64:128, :, 1], tb["rf"][64:128, :], r=[tb["rf"]], w=[tb["rbd"]], eng="gpsimd")
            P.tt(tb["Bd"][:], mask32[:].unsqueeze(2).to_broadcast([64, 32, 128]),
                 tb["btf"][:].unsqueeze(1).to_broadcast([64, 32, 128]), ALU.mult,
                 r=[mask32, tb["btf"]], w=[tb["Bd"]], eng=DIAG_ENG)
            P.tt(tb["Vd"][:], mask32[:].unsqueeze(2).to_broadcast([64, 32, 64]),
                 tb["vtf"][:].unsqueeze(1).to_broadcast([64, 32, 64]), ALU.mult,
                 r=[mask32, tb["vtf"]], w=[tb["Vd"]], eng=DIAG_ENG)

    def lgen(q):
        g0 = 4 * q
        i, t0 = g0 // TS, g0 % TS
        for d in range(2):
            b0 = t0 if d == 0 else TS - 4 - t0
            tb = TB_[d][i % 2]
            lp = LP[d][q % 2]
            ls = LS[d][q % 3]
            P.mm(lp[:, 0:512], tb["nk"][:], tb["Bd"][:, b0:b0 + 4, :], r=[tb["nk"], tb["Bd"]], w=[lp])
            P.cp(ls[:], lp[:, 0:512], r=[lp], w=[ls], eng="scalar")

    def mm_step(g):
        i, t = g // TS, g % TS
        for d in range(2):
            tl = loc(d, t)
            tb = TB_[d][i % 2]
            P.mm(SP[d][:, 0:64], tb["kd"][:], tb["Vd"][:, tl, :], start=True, stop=False,
                 r=[tb["kd"], tb["Vd"]], w=[SP[d]])
        for d in range(2):
            ls = LS[d][(g // 4) % 3]
            j = (g % 4) if d == 0 else 3 - (g % 4)
            sbo = Sb[d][(g + 1) % 2]
            P.mm(SP[d][:, 0:64], ls[:, j * 128:(j + 1) * 128], sbo[:], start=False, stop=True, r=[ls, sbo], w=[SP[d]])

    def out_step(g):
        i, t = g // TS, g % TS
        for d in range(2):
            tb = TB_[d][i % 2]
            tl = loc(d, t)
            P.mm(OP[d][0:64, 2 * tl:2 * tl + 2], Sb[d][g % 2][:], tb["rbd"][:, tl, :],
                 r=[Sb[d][g % 2], tb["rbd"]], w=[OP[d]])

    def dve_step(g):
        i, t = g // TS, g % TS
        for d in range(2):
            tb = TB_[d][i % 2]
            tl = loc(d, t)
            P.stt(Sb[d][g % 2][:], S[d][:], tb["wt"][:, tl:tl + 1], SP[d][:, 0:64], ALU.mult, ALU.add,
                  r=[S[d], tb["wt"], SP[d]], w=[Sb[d][g % 2]])
        for d in range(2):
            tb = TB_[d][i % 2]
            tl = loc(d, t)
            P.stt(S[d][:], S[d][:], tb["wt"][:, tl:tl + 1], SP[d][:, 0:64], ALU.mult, ALU.add,
                  r=[S[d], tb["wt"], SP[d]], w=[S[d]])

    def evac(i):
        for d in range(2):
            lo = lo_of(d, i)
            P.cp(osb[d][:], OP[d][0:64, 0:2 * TS], r=[OP[d]], w=[osb[d]])
            P.dma("sync", scr["O%d" % d][:, lo:lo + TS, :], osb[d][:].rearrange("p (t h) -> p t h", h=2),
                  r=[osb[d]], w=[sres["O%d" % d]])

    load(0)
    lgen(0)
    for g in range(T):
        flushed = False
        if g % TS == 0:
            if g > 0:
                out_step(g - 1)
                evac(g // TS - 1)
                flushed = True
            if g // TS + 1 < T // TS:
                load(g // TS + 1)
        if g % 4 == 0 and g + 4 < T:
            lgen(g // 4 + 1)
        mm_step(g)
        if g > 0 and not flushed:
            out_step(g - 1)
        dve_step(g)
    out_step(T - 1)
    evac(T // TS - 1)
    P.release(m0)
    ln = P.sb("ln", [128, 2, 128], F32)
    P.dma("sync", ln[:], lnrow_d[:, :, :], w=[ln])
    o3 = [[P.sb("o3", [64, 128, 2], F32) for i in range(2)] for k in range(2)]
    gb = [[P.sb("gb", [128, 128], F32) for i in range(2)] for k in range(2)]
    ot = [P.sb("ot", [128, 128], F32) for k in range(2)]
    st = P.sb("st", [128, 2, 6], F32)
    mv = P.sb("mv", [128, 2, 2], F32)
    for tt in range(NT):
        if tt < 2 and not emit_ctx:
            continue
        r0 = tt * 128
        of, ob = o3[tt % 2]
        g_, bv = gb[tt % 2]
        o_ = ot[tt % 2]
        P.dma("sync", of[:], scr["O0"][:, r0:r0 + 128, :], r=[sres["O0"]], w=[of])
        P.dma("sync", ob[:], scr["O1"][:, r0:r0 + 128, :], r=[sres["O1"]], w=[ob])
        P.dma("sync", g_[:], scr["G"][r0:r0 + 128, :], r=[sres["G"]], w=[g_])
        P.dma("sync", bv[:], scr["BV"][r0:r0 + 128, :], r=[sres["BV"]], w=[bv])
        P.tt(of[:], of[:], ob[:], ALU.add, r=[of, ob], w=[of])
        pt = ps[tt % 2]
        for h in range(2):
            P.tr(pt[:, h * 64:(h + 1) * 64], of[:, :, h], ident[0:64, 0:64], r=[of, ident], w=[pt])
        P.cp(o_[:], pt[:, 0:128], r=[pt], w=[o_])
        for h in range(2):
            P.call("vector", "bn_stats", st[:, h, :], o_[:, h * 64:(h + 1) * 64], r=[o_], w=[st])
            P.call("vector", "bn_aggr", mv[:, h, :], st[:, h, :], r=[st], w=[mv])
        P.act(mv[:, :, 1:2], mv[:, :, 1:2], AF.Sqrt, r=[mv], w=[mv], bias=64e-5)
        P.call("vector", "reciprocal", mv[:, :, 1:2], mv[:, :, 1:2], r=[mv], w=[mv])
        for h in range(2):
            sl = slice(h * 64, (h + 1) * 64)
            P.ts(o_[:, sl], o_[:, sl], mv[:, h, 0:1], mv[:, h, 1:2], ALU.subtract, ALU.mult, r=[o_, mv], w=[o_])
        P.tt(o_[:], o_[:], ln[:, 0, :], ALU.mult, r=[o_, ln], w=[o_])
        P.tt(o_[:], o_[:], ln[:, 1, :], ALU.add, r=[o_, ln], w=[o_])
        P.tt(o_[:], o_[:], bv[:], ALU.add, r=[o_, bv], w=[o_])
        P.tt(o_[:], o_[:], g_[:], ALU.mult, r=[o_, g_], w=[o_])
        P.dma("sync", ymix[r0:r0 + 128, :], o_[:], r=[o_])
    P.release(m0)


class _Din:
    def __init__(self, nc, pre, shared):
        self.nc, self.pre, self._shared = nc, pre, shared

    def __call__(self, name, shape, dt=F32):
        return self.nc.dram_tensor(self.pre + name, list(shape), dt, kind="ExternalInput").ap()

    def shared(self, name, shape, dt=F32):
        if name not in self._shared:
            self._shared[name] = self.nc.dram_tensor(name, list(shape), dt, kind="ExternalInput").ap()
        return self._shared[name]


def build_fused(nlayers=2):
    nc = bass.Bass("TRN2", target_bir_lowering=False)
    P = Prog(nc)
    P.scoped = True
    shared = {}
    d0 = _Din(nc, "", shared)
    xin0 = d0.shared("xin", [T, D])
    xres0 = d0.shared("xres0", [TB, D])
    sel_d = d0.shared("sel", [128, 2])
    ident_d = d0.shared("ident", [128, 128])
    out = nc.dram_tensor("xout", [TB, D], F32, kind="ExternalOutput").ap()

    ident = P.sb("ident", [128, 128], F32)
    P.dma("sync", ident[:], ident_d[:, :], w=[ident])
    identb = P.sb("identb", [128, 128], BF16)
    P.cp(identb[:], ident[:], r=[ident], w=[identb])
    onesb = P.sb("onesb", [128, 128], BF16)
    P.memset(onesb[:], 1.0, w=[onesb])
    ones32 = P.sb("ones32", [128, 128], F32)
    P.memset(ones32[:], 1.0, w=[ones32])
    sel = P.sb("sel", [128, 2], F32)
    P.dma("sync", sel[:], sel_d[:, :], w=[sel])
    ps = [P.ps("ps%d" % i, [128, 512]) for i in range(8)]

    XG = None
    XHp = None
    xg_res = Res()
    for l in range(nlayers):
        pre = "L%d_" % l
        last = (l == nlayers - 1)
        emit_ctx = not last
        din = _Din(nc, pre, shared)
        YH = nc.dram_tensor(pre + "YH", [T, 512], F32).ap()
        ych = [(0, 1024), (1024, 1024), (2048, 1024), (3072, 1024), (4096, 256)]
        YG = [nc.dram_tensor(pre + "YG%d" % k, [2 * n_, 512], F32).ap() for k, (s_, n_) in enumerate(ych)]
        yg_res = Res()
        mL = P.mark()
        if l == 0:
            def xrow_fn(tt):
                return xin0[tt * 128:(tt + 1) * 128, :], []
        else:
            def xrow_fn(tt, XG=XG):
                j, lo = _half_rows(tt)
                k, off = lo // 512, lo % 512
                n_ = 512 if k < 4 else 128
                return XG[k][j * n_ + off:j * n_ + off + 128, :], [xg_res]
        cT = din.shared("cT", [128, 8, 2])
        wmodA = din("wmodA", [D, 2048])
        bmodA = din("bmodA", [128, 16])
        hT, hres, xt = phase_ln(nc, P, ps, ident, xrow_fn, cT, wmodA, bmodA, None)
        for kind, c0 in (("mlstm", 0), ("rwkv", 128), ("mla", 256)):
            mk = P.mark()
            winA = din("winA_" + kind, [D, NCA[kind]])
            winb = P.sb("winb", [128, 8, NCA[kind]], BF16)
            for kc in range(8):
                P.dma("gpsimd", winb[:, kc, :], winA[kc * 128:(kc + 1) * 128, :], w=[winb])
            proj_fm = make_proj_fm(P, hT, hres, winb)
            ymix = YH[:, c0:c0 + YW[kind]]
            env = dict(nc=nc, P=P, din=din, ps=ps, hT=hT, hres=hres, winb=winb, proj_fm=proj_fm, ident=ident,
                       identb=identb, onesb=onesb, ymix=ymix, emit_ctx=emit_ctx, dbg=(), xt=xt)
            if kind == "mlstm":
                build_mlstm(**env)
            elif kind == "mla":
                build_mla(**env)
            else:
                scr = {}
                sres = {}
                for n in R1_OUT:
                    shp = [T, 128] if n in TM_NAMES else [128, T]
                    scr[n] = nc.dram_tensor(pre + "S_" + n, shp, F32).ap()
                    sres[n] = Res()
                for n in ("O0", "O1"):
                    scr[n] = nc.dram_tensor(pre + "S_" + n, [64, T, 2], F32).ap()
                    sres[n] = Res()
                tmos = [P.sb("tmo", [128, 512], F32) for k in range(2)]
                tmc = [0]

                def r1sink(name, src, t0, w, srcres, scr=scr, sres=sres, tmos=tmos, tmc=tmc):
                    tmo = tmos[tmc[0] % 2]
                    tmc[0] += 1
                    if name not in TM_NAMES:
                        P.dma("sync", scr[name][:, t0:t0 + w], src, r=srcres, w=[sres[name]])
                        return
                    pt = ps[7]
                    for ci in range(w // 128):
                        P.tr(pt[:, ci * 128:(ci + 1) * 128], src[:, ci * 128:(ci + 1) * 128], ident[:],
                             r=list(srcres) + [ident], w=[pt])
                    P.cp(tmo[:, 0:w], pt[:, 0:w], r=[pt], w=[tmo])
                    for ci in range(w // 128):
                        P.dma("sync", scr[name][t0 + ci * 128:t0 + (ci + 1) * 128, :], tmo[:, ci * 128:(ci + 1) * 128],
                              r=[tmo], w=[sres[name]])
                build_rwkv(r1sink=r1sink, **env)
                P.release(mk)
                rln = din("rlnrow", [128, 2, 128])
                fused_rwkv_scan(nc, P, ps, ident, scr, sres, ymix, rln, emit_ctx)
            P.release(mk)
        P.release(mL)
        for k, (s_, n_) in enumerate(ych):
            P.coll("AllGather", YH[s_:s_ + n_, :], YG[k][:, :], GROUPS, w=[yg_res])
        P.barrier()
        XH = out if last else nc.dram_tensor(pre + "XH", [TB, D], F32).ap()

        def xres_fn(tt, buf, l=l, XHp=XHp):
            srcx = xres0 if l == 0 else XHp
            P.dma("sync", buf[:], srcx[tt * 128:(tt + 1) * 128, :], w=[buf])

        def ymx_fn(tt, ym, tmp, YG=YG, yg_res=yg_res):
            ra = 0 if tt == 0 else 256 + (tt - 1) * 128
            rb = 128 if tt == 0 else 256 + 2048 + (tt - 1) * 128
            for r in range(2):
                for (rr_, buf_) in ((ra, ym), (rb, tmp)):
                    k, off = rr_ // 1024, rr_ % 1024
                    n_ = 1024 if k < 4 else 256
                    P.dma("sync", buf_[:, r * 512:(r + 1) * 512], YG[k][r * n_ + off:r * n_ + off + 128, :],
                          r=[yg_res], w=[buf_])
            P.ts(ym[:], ym[:], sel[:, 0:1], None, ALU.mult, r=[ym, sel], w=[ym])
            P.stt(ym[:], tmp[:], sel[:, 1:2], ym[:], ALU.mult, ALU.add, r=[tmp, sel, ym], w=[ym])

        def out_fn(tt, XH=XH):
            return XH[tt * 128:(tt + 1) * 128, :], []
        phase_B(nc, P, ps, ident, ones32, din, xres_fn, ymx_fn, out_fn, pre=pre)
        P.release(mL)
        if not last:
            xch = [(0, 512), (512, 512), (1024, 512), (1536, 512), (2048, 128)]
            XG = [nc.dram_tensor(pre + "XG%d" % k, [2 * n_, D], F32).ap() for k, (s_, n_) in enumerate(xch)]
            P.barrier()
            for k, (s_, n_) in enumerate(xch):
                P.coll("AllGather", XH[s_:s_ + n_, :], XG[k][:, :], GROUPS, w=[xg_res])
            P.barrier()
            XHp = XH
    P.emit()
    return nc


_WOUT_PERM = np.concatenate([np.arange(0, 128), np.arange(256, 384), np.arange(512, 768),
                             np.arange(128, 256), np.arange(384, 512), np.arange(768, 1024)])


def prep_fused(inp, nlayers=2):
    x = inp["x"]
    xc = inp["ctx"]
    maps = [dict() for _ in range(8)]
    ident = np.eye(128, dtype=np.float32)
    for core in range(8):
        b, hp = core // 2, core % 2
        m = maps[core]
        m["xin"] = np.ascontiguousarray(np.concatenate([xc[b], x[b]], 0))
        m["xres0"] = np.ascontiguousarray(np.concatenate([xc[b][128 * hp:128 * hp + 128], x[b][2048 * hp:2048 * hp + 2048]], 0))
        sv = np.zeros((128, 2), np.float32)
        sv[:, hp] = 1.0
        m["sel"] = sv
        m["ident"] = ident
    for l in range(nlayers):
        pre = "L%d_" % l
        for kind in ("mlstm", "rwkv", "mla"):
            pa = prep_A(inp, l, x, xc, kind)
            for core in range(8):
                m = maps[core]
                for k, v in pa[core].items():
                    if k in ("xin", "ident"):
                        continue
                    if k == "cT":
                        m["cT"] = v
                    elif k == "winA":
                        m[pre + "winA_" + kind] = v
                    else:
                        m[pre + k] = v
        bm = inp["b_mod"][l][2048:6144]
        shared_b = {
            "wmodB": np.ascontiguousarray(inp["w_mod"][l][:, 2048:6144]),
            "bmodrow": np.ascontiguousarray(np.broadcast_to(bm[None], (128, 4096))),
            "bmodcol": _col_layout(bm),
            "wout": np.ascontiguousarray(inp["w_out"][l][_WOUT_PERM]),
            "lnrow": np.ascontiguousarray(np.broadcast_to(
                np.stack([inp["ln1_w"][l], inp["ln1_b"][l], inp["ln2_w"][l], inp["ln2_b"][l]])[None], (128, 4, D))),
            "rw": inp["router_w"][l],
            "rbias": np.ascontiguousarray(np.broadcast_to(inp["router_bias"][l][None], (128, 64))),
            "eg": np.concatenate([inp["exp_w_gate"][l], inp["sh_w_gate"][l][None]], 0),
            "eu": np.concatenate([inp["exp_w_up"][l], inp["sh_w_up"][l][None]], 0),
            "ed": np.concatenate([inp["exp_w_down"][l], inp["sh_w_down"][l][None]], 0),
        }
        for core in range(8):
            hp = core % 2
            hc = slice(128 * hp, 128 * hp + 128)
            m = maps[core]
            for k, v in shared_b.items():
                m[pre + k] = v
            m[pre + "rlnrow"] = np.ascontiguousarray(np.broadcast_to(
                np.stack([inp["rwkv_ln_w"][l][hc], inp["rwkv_ln_b"][l][hc]])[None], (128, 2, 128)))
    return maps


def kernel_unfused(**inp):
    return _kernel_unfused(inp)


def kernel(**inp):
    inp = {k: np.asarray(v) for k, v in inp.items()}
    nc = build_fused(2)
    res = run_bass_kernel_spmd(nc, prep_fused(inp, 2), core_ids=list(range(8)))
    x = np.empty_like(inp["x"], dtype=np.float32)
    for core in range(8):
        b, hf = core // 2, core % 2
        o = res.results[core]["xout"]
        x[b][2048 * hf:2048 * hf + 2048] = o[128:]
    return x
```
